# Optimizing a Trainium2 kernel written in Bass

```python
import math
import jax
import jax.numpy as jnp
from jax import lax
import numpy as np

D_MODEL = 2048
BATCH = 2
SEQ = 4096
DEPTH = 1

GRID_W = 64
CTX_LEN = 256
NA_HEAD_DIM = 128
NA_HEADS = D_MODEL // (2 * NA_HEAD_DIM)
NA_WIN_H = 8
NA_WIN_W = 16
DIFF_HEAD_DIM = 64
DIFF_V_DIM = 2 * DIFF_HEAD_DIM
DIFF_HEADS = D_MODEL // (2 * DIFF_V_DIM)
NA_WIDTH = NA_HEADS * NA_HEAD_DIM
DIFF_QK_WIDTH = DIFF_HEADS * 2 * DIFF_HEAD_DIM
DIFF_V_WIDTH = DIFF_HEADS * DIFF_V_DIM
IN_CTX_COLS = 2 * NA_WIDTH + DIFF_QK_WIDTH + DIFF_V_WIDTH
IN_COLS = IN_CTX_COLS + NA_WIDTH + DIFF_QK_WIDTH + 2 * D_MODEL
Q_BLOCK = 128
ROPE_BASE = 10000.0
N_EXPERTS = 32
TOP_K = 4
D_FF = D_MODEL
SWIGLU_ALPHA = 1.702
SWIGLU_LIMIT = 7.0
MOE_BLOCK = 128
LN_EPS = 1e-6
RMS_EPS = 1e-5
DEEPNORM_ALPHA = (2 * DEPTH) ** 0.25
DEEPNORM_BETA = (8 * DEPTH) ** -0.25

kernel_name = 'hybrid_natten_diffattn_moe_dit_layer'


def _layernorm(x, w=None, b=None):
    xf = x.astype(jnp.float32)
    xc = xf - jnp.mean(xf, axis=-1, keepdims=True)
    y = xc * lax.rsqrt(jnp.mean(xc * xc, axis=-1, keepdims=True) + LN_EPS)
    if w is not None:
        y = y * w.astype(jnp.float32) + b.astype(jnp.float32)
    return y.astype(x.dtype)


def _modulate(x, shift, scale):
    return _layernorm(x) * (1 + scale) + shift


def _axial_rope(x, row_pos, col_pos):
    half = x.shape[-1] // 2
    n_freq = half // 2
    inv_freq = ROPE_BASE ** (-jnp.arange(n_freq, dtype=jnp.float32) / n_freq)
    xf = x.astype(jnp.float32)

    def rotate(xa, pos):
        ang = pos[:, None] * inv_freq[None, :]
        cos = jnp.cos(ang)[None, :, None, None, :]
        sin = jnp.sin(ang)[None, :, None, None, :]
        x1, x2 = xa[..., :n_freq], xa[..., n_freq:]
        return jnp.concatenate([x1 * cos - x2 * sin, x2 * cos + x1 * sin], axis=-1)

    out = jnp.concatenate([rotate(xf[..., :half], row_pos), rotate(xf[..., half:], col_pos)], axis=-1)
    return out.astype(x.dtype)


def _split_kv(p, b, n):
    o1 = NA_WIDTH
    o2 = 2 * NA_WIDTH
    o3 = o2 + DIFF_QK_WIDTH
    k_na = p[..., :o1].reshape(b, n, NA_HEADS, NA_HEAD_DIM)
    v_na = p[..., o1:o2].reshape(b, n, NA_HEADS, NA_HEAD_DIM)
    k_df = p[..., o2:o3].reshape(b, n, DIFF_HEADS, 2, DIFF_HEAD_DIM)
    v_df = p[..., o3:IN_CTX_COLS].reshape(b, n, DIFF_HEADS, DIFF_V_DIM)
    return k_na, v_na, k_df, v_df


def _split_qg(p, b, n):
    o1 = NA_WIDTH
    o2 = o1 + DIFF_QK_WIDTH
    o3 = o2 + D_MODEL
    q_na = p[..., :o1].reshape(b, n, NA_HEADS, NA_HEAD_DIM)
    q_df = p[..., o1:o2].reshape(b, n, DIFF_HEADS, 2, DIFF_HEAD_DIM)
    return q_na, q_df, p[..., o2:o3], p[..., o3:]


def _neighbourhood_attention(q, k, v, k_ctx, v_ctx, rel_bias):
    b, s, h, hd = q.shape
    rows = s // GRID_W
    win_h = min(NA_WIN_H, rows)
    n_loc = win_h * NA_WIN_W
    scale = hd ** -0.5
    qg = q.reshape(b, rows, GRID_W, h, hd)
    kg = k.reshape(b, rows, GRID_W, h, hd)
    vg = v.reshape(b, rows, GRID_W, h, hd)
    col = np.arange(GRID_W)
    col_start = np.clip(col - NA_WIN_W // 2, 0, GRID_W - NA_WIN_W)
    col_idx = col_start[:, None] + np.arange(NA_WIN_W)[None, :]
    col_off = col_idx - col[:, None] + (NA_WIN_W - 1)

    def row_block(r):
        r0 = jnp.clip(r - win_h // 2, 0, rows - win_h)
        q_r = lax.dynamic_index_in_dim(qg, r, axis=1, keepdims=False)
        k_win = lax.dynamic_slice_in_dim(kg, r0, win_h, axis=1)[:, :, col_idx]
        v_win = lax.dynamic_slice_in_dim(vg, r0, win_h, axis=1)[:, :, col_idx]
        row_off = r0 + jnp.arange(win_h) - r + (NA_WIN_H - 1)
        bias = rel_bias[:, row_off[None, :, None], col_off[:, None, :]]
        s_loc = jnp.einsum('bqhd,biqjhd->bhqij', q_r, k_win).astype(jnp.float32) * scale
        s_loc = s_loc + bias.astype(jnp.float32)[None]
        s_ctx = jnp.einsum('bqhd,bchd->bhqc', q_r, k_ctx).astype(jnp.float32) * scale
        logits = jnp.concatenate([s_loc.reshape(b, h, GRID_W, n_loc), s_ctx], axis=-1)
        p = jax.nn.softmax(logits, axis=-1).astype(v.dtype)
        p_loc = p[..., :n_loc].reshape(b, h, GRID_W, win_h, NA_WIN_W)
        p_ctx = p[..., n_loc:]
        return (jnp.einsum('bhqij,biqjhd->bqhd', p_loc, v_win)
                + jnp.einsum('bhqc,bchd->bqhd', p_ctx, v_ctx))

    out = lax.map(row_block, jnp.arange(rows))
    return jnp.moveaxis(out, 0, 1).reshape(b, s, h * hd)


def _softmax_attention(q, k, v):
    s = jnp.einsum('bqhd,bkhd->bhqk', q, k).astype(jnp.float32) * q.shape[-1] ** -0.5
    p = jax.nn.softmax(s, axis=-1).astype(v.dtype)
    return jnp.einsum('bhqk,bkhd->bqhd', p, v)


def _diff_core(q, k, v, lam):
    s = jnp.einsum('bqhmd,bkhmd->bhmqk', q, k).astype(jnp.float32) * q.shape[-1] ** -0.5
    p = jax.nn.softmax(s, axis=-1)
    w = (p[:, :, 0] - lam * p[:, :, 1]).astype(v.dtype)
    return jnp.einsum('bhqk,bkhd->bqhd', w, v)


def _differential_attention_latent(q, k, v, k_ctx, v_ctx, lam):
    b, s = q.shape[:2]
    k_all = jnp.concatenate([k, k_ctx], axis=1)
    v_all = jnp.concatenate([v, v_ctx], axis=1)
    qb = jnp.moveaxis(q.reshape(b, s // Q_BLOCK, Q_BLOCK, DIFF_HEADS, 2, DIFF_HEAD_DIM), 1, 0)
    out = lax.map(lambda qq: _diff_core(qq, k_all, v_all, lam), qb)
    return jnp.moveaxis(out, 0, 1).reshape(b, s, DIFF_HEADS, DIFF_V_DIM)


def _diff_lambda(lq1, lk1, lq2, lk2, lam_init):
    f = jnp.float32
    return (jnp.exp(jnp.sum(lq1.astype(f) * lk1.astype(f)))
            - jnp.exp(jnp.sum(lq2.astype(f) * lk2.astype(f))) + lam_init)


def _diff_head_norm(o, w, lam_init):
    b, n = o.shape[:2]
    of = o.astype(jnp.float32)
    y = of * lax.rsqrt(jnp.mean(of * of, axis=-1, keepdims=True) + RMS_EPS)
    y = y * w.astype(jnp.float32) * (1.0 - lam_init)
    return y.reshape(b, n, DIFF_V_WIDTH).astype(o.dtype)


def _merge_branches(o_na, o_df, g_na, g_df, w_proj_na, w_proj_diff, w_out):
    y = jax.nn.sigmoid(g_na) * (o_na @ w_proj_na) + jax.nn.sigmoid(g_df) * (o_df @ w_proj_diff)
    return y @ w_out


def _token_mixer(u, uc, w_in, rel_bias, lam, lam_init, subln_w, w_proj_na, w_proj_diff, w_out,
                 row_pos, col_pos, ctx_out):
    b, s, _ = u.shape
    cl = uc.shape[1]
    p = u @ w_in
    k_na, v_na, k_df, v_df = _split_kv(p[..., :IN_CTX_COLS], b, s)
    q_na, q_df, g_na, g_df = _split_qg(p[..., IN_CTX_COLS:], b, s)
    pc = uc @ (w_in if ctx_out else w_in[:, :IN_CTX_COLS])
    k_na_c, v_na_c, k_df_c, v_df_c = _split_kv(pc[..., :IN_CTX_COLS], b, cl)
    q_df = _axial_rope(q_df, row_pos, col_pos)
    k_df = _axial_rope(k_df, row_pos, col_pos)
    o_na = _neighbourhood_attention(q_na, k_na, v_na, k_na_c, v_na_c, rel_bias)
    o_df = _diff_head_norm(_differential_attention_latent(q_df, k_df, v_df, k_df_c, v_df_c, lam),
                           subln_w, lam_init)
    y = _merge_branches(o_na, o_df, g_na, g_df, w_proj_na, w_proj_diff, w_out)
    if not ctx_out:
        return y, None
    q_na_c, q_df_c, g_na_c, g_df_c = _split_qg(pc[..., IN_CTX_COLS:], b, cl)
    o_na_c = _softmax_attention(q_na_c, k_na_c, v_na_c).reshape(b, cl, NA_WIDTH)
    o_df_c = _diff_head_norm(_diff_core(q_df_c, k_df_c, v_df_c, lam), subln_w, lam_init)
    yc = _merge_branches(o_na_c, o_df_c, g_na_c, g_df_c, w_proj_na, w_proj_diff, w_out)
    return y, yc


def _moe_ffn(u, w_router, b_router, w_gate, b_gate, w_up, b_up, w_down, b_down):
    t, d = u.shape
    logits = (u @ w_router + b_router).astype(jnp.float32)
    top_logit, top_e = lax.top_k(logits, TOP_K)
    top_w = jax.nn.softmax(top_logit, axis=-1)
    n_assign = t * TOP_K
    flat_e = top_e.reshape(n_assign)
    order = jnp.argsort(flat_e)
    sorted_e = flat_e[order]
    counts = jnp.zeros((N_EXPERTS,), jnp.int32).at[flat_e].add(1)
    padded = (counts + MOE_BLOCK - 1) // MOE_BLOCK * MOE_BLOCK
    start = jnp.cumsum(counts) - counts
    pad_end = jnp.cumsum(padded)
    pad_start = pad_end - padded
    dest = pad_start[sorted_e] + jnp.arange(n_assign) - start[sorted_e]
    n_blocks = -(-(n_assign + N_EXPERTS * (MOE_BLOCK - 1)) // MOE_BLOCK)
    n_rows = n_blocks * MOE_BLOCK
    src_token = order // TOP_K
    row_token = jnp.full((n_rows,), t, jnp.int32).at[dest].set(src_token)
    u_pad = jnp.concatenate([u, jnp.zeros((1, d), u.dtype)], axis=0)
    x_rows = u_pad[row_token].reshape(n_blocks, MOE_BLOCK, d)
    block_e = jnp.minimum(jnp.searchsorted(pad_end, jnp.arange(n_blocks) * MOE_BLOCK, side='right'),
                          N_EXPERTS - 1)

    def expert_block(args):
        xb, e = args
        g = jnp.minimum(xb @ w_gate[e] + b_gate[e], SWIGLU_LIMIT)
        lin = jnp.clip(xb @ w_up[e] + b_up[e], -SWIGLU_LIMIT, SWIGLU_LIMIT)
        h = g * jax.nn.sigmoid(SWIGLU_ALPHA * g) * (lin + 1)
        return h @ w_down[e] + b_down[e]

    y_rows = lax.map(expert_block, (x_rows, block_e)).reshape(n_rows, d)
    y_assign = y_rows[dest] * top_w.reshape(n_assign)[order][:, None].astype(y_rows.dtype)
    return jnp.zeros((t, d), y_rows.dtype).at[src_token].add(y_assign)


def setup_inputs(seed: int = 0) -> dict:
    key = jax.random.key(seed)
    ks = jax.random.split(key, 28)
    f32 = jnp.float32
    L, D, E, F = DEPTH, D_MODEL, N_EXPERTS, D_FF

    def nrm(k, shape, scale):
        return jax.random.normal(k, shape, f32) * scale

    return {
        'x': nrm(ks[0], (BATCH, SEQ, D), 1.0),
        'c': nrm(ks[1], (BATCH, D), 1.0),
        'ctx': nrm(ks[2], (BATCH, CTX_LEN, D), 1.0),
        'c_ctx': nrm(ks[3], (D,), 1.0),
        'w_ada': nrm(ks[4], (L, D, 6 * D), D ** -0.5),
        'b_ada': nrm(ks[5], (L, 6 * D), 0.02),
        'w_in': nrm(ks[6], (L, D, IN_COLS), D ** -0.5),
        'na_rel_bias': nrm(ks[7], (L, NA_HEADS, 2 * NA_WIN_H - 1, 2 * NA_WIN_W - 1), 0.2),
        'lam_q1': nrm(ks[8], (L, DIFF_HEAD_DIM), 0.1),
        'lam_k1': nrm(ks[9], (L, DIFF_HEAD_DIM), 0.1),
        'lam_q2': nrm(ks[10], (L, DIFF_HEAD_DIM), 0.1),
        'lam_k2': nrm(ks[11], (L, DIFF_HEAD_DIM), 0.1),
        'diff_subln_w': 1.0 + nrm(ks[12], (L, DIFF_V_DIM), 0.02),
        'w_proj_na': nrm(ks[13], (L, NA_WIDTH, D), NA_WIDTH ** -0.5),
        'w_proj_diff': nrm(ks[14], (L, DIFF_V_WIDTH, D), DIFF_V_WIDTH ** -0.5),
        'w_out': nrm(ks[15], (L, D, D), D ** -0.5 * DEEPNORM_BETA),
        'ln1_w': 1.0 + nrm(ks[16], (L, D), 0.02),
        'ln1_b': nrm(ks[17], (L, D), 0.02),
        'w_router': nrm(ks[18], (L, D, E), D ** -0.5),
        'b_router': nrm(ks[19], (L, E), 0.01),
        'w_gate': nrm(ks[20], (L, E, D, F), D ** -0.5),
        'b_gate': nrm(ks[21], (L, E, F), 0.02),
        'w_up': nrm(ks[22], (L, E, D, F), D ** -0.5),
        'b_up': nrm(ks[23], (L, E, F), 0.02),
        'w_down': nrm(ks[24], (L, E, F, D), F ** -0.5 * DEEPNORM_BETA),
        'b_down': nrm(ks[25], (L, E, D), 0.02),
        'ln2_w': 1.0 + nrm(ks[26], (L, D), 0.02),
        'ln2_b': nrm(ks[27], (L, D), 0.02),
    }


def reference(x, c, ctx, c_ctx, w_ada, b_ada, w_in, na_rel_bias, lam_q1, lam_k1, lam_q2, lam_k2,
              diff_subln_w, w_proj_na, w_proj_diff, w_out, ln1_w, ln1_b, w_router, b_router,
              w_gate, b_gate, w_up, b_up, w_down, b_down, ln2_w, ln2_b):
    b, s, d = x.shape
    tok = jnp.arange(s)
    row_pos = (tok // GRID_W).astype(jnp.float32)
    col_pos = (tok % GRID_W).astype(jnp.float32)
    for l in range(DEPTH):
        last = l == DEPTH - 1
        lam_init = 0.8 - 0.6 * math.exp(-0.3 * l)
        mod = jax.nn.silu(c) @ w_ada[l] + b_ada[l]
        mod_c = jax.nn.silu(c_ctx) @ w_ada[l] + b_ada[l]
        sh_m, sc_m, g_m, sh_f, sc_f, g_f = [m[:, None, :] for m in jnp.split(mod, 6, axis=-1)]
        sh_mc, sc_mc, g_mc, sh_fc, sc_fc, g_fc = jnp.split(mod_c, 6, axis=-1)
        lam = _diff_lambda(lam_q1[l], lam_k1[l], lam_q2[l], lam_k2[l], lam_init)

        u = _modulate(x, sh_m, sc_m)
        uc = _modulate(ctx, sh_mc, sc_mc)
        y, yc = _token_mixer(u, uc, w_in[l], na_rel_bias[l], lam, lam_init, diff_subln_w[l],
                             w_proj_na[l], w_proj_diff[l], w_out[l], row_pos, col_pos, not last)
        x = _layernorm(DEEPNORM_ALPHA * x + g_m * y, ln1_w[l], ln1_b[l])

        u2 = _modulate(x, sh_f, sc_f)
        y2 = _moe_ffn(u2.reshape(b * s, d), w_router[l], b_router[l], w_gate[l], b_gate[l],
                      w_up[l], b_up[l], w_down[l], b_down[l]).reshape(b, s, d)
        x = _layernorm(DEEPNORM_ALPHA * x + g_f * y2, ln2_w[l], ln2_b[l])

        if not last:
            ctx = _layernorm(DEEPNORM_ALPHA * ctx + g_mc * yc, ln1_w[l], ln1_b[l])
            uc2 = _modulate(ctx, sh_fc, sc_fc)
            yc2 = _moe_ffn(uc2.reshape(-1, d), w_router[l], b_router[l], w_gate[l], b_gate[l],
                           w_up[l], b_up[l], w_down[l], b_down[l]).reshape(ctx.shape)
            ctx = _layernorm(DEEPNORM_ALPHA * ctx + g_fc * yc2, ln2_w[l], ln2_b[l])
    return x
```

```python
import math
from contextlib import ExitStack
import numpy as np
import concourse.bass as bass
import concourse.mybir as mybir
from concourse.bass_utils import run_bass_kernel_spmd

F32 = mybir.dt.float32
BF16 = mybir.dt.bfloat16
AF = mybir.ActivationFunctionType
ALU = mybir.AluOpType
AX = mybir.AxisListType

D = 2048
S = 4096
NCORE = 8
TOK = 1024
CTX = 256
NH = 8
NE_FULL = 32
LN_EPS = 1e-6
RMS_EPS = 1e-5
ALPHA = 2.0 ** 0.25
LAM_INIT = 0.8 - 0.6 * math.exp(0.0)
NKEY_DF = S + CTX
NKEY_NA = 24 * 64 + CTX
NSLOT = 6
NEG = -30000.0


class Res:
    __slots__ = ("name", "w", "r", "psum")

    def __init__(self, name):
        self.name = name
        self.w = None
        self.r = {}
        self.psum = False


class Tile:
    def __init__(self, t, name):
        self.t = t
        self.res = Res(name)

    def __getitem__(self, k):
        return self.t[k]


class Eng:
    def __init__(self, name, h, is_pe=False):
        self.name = name
        self.h = h
        self.sem = "e_" + name
        self.cnt = 0
        self.seen = {}
        self.dk = 0
        self.dvals = [0] * NSLOT
        self.is_pe = is_pe


class Ctx:
    def __init__(self, nc, stack):
        self.nc = nc
        self.stack = stack
        self.E = {
            "pe": Eng("pe", nc.tensor, True),
            "act": Eng("act", nc.scalar),
            "dve": Eng("dve", nc.vector),
            "pool": Eng("pool", nc.gpsimd),
            "sp": Eng("sp", nc.sync),
        }
        self.sems = {}
        for e in self.E.values():
            self.sems[e.sem] = stack.enter_context(nc.semaphore(e.sem))
        for q in ("sp", "pool", "act"):
            for k in range(NSLOT):
                n = "d_%s%d" % (q, k)
                self.sems[n] = stack.enter_context(nc.semaphore(n))
        self.ninstr = 0
        import os
        self.limit = int(os.environ.get("K_LIMIT", "0"))
        self.trace = int(os.environ.get("K_TRACE", "0"))

    def skip(self):
        return self.limit and self.ninstr >= self.limit

    def _waits(self, E, reads, writes, extra=()):
        if self.skip():
            return
        waits = {}

        def need(tok):
            if tok is None:
                return
            sem, val = tok
            if E.is_pe and sem == E.sem:
                return
            if E.seen.get(sem, 0) >= val:
                return
            if waits.get(sem, 0) < val:
                waits[sem] = val

        for R in reads:
            need(R.w)
        for R in writes:
            need(R.w)
            for s_, v_ in R.r.items():
                need((s_, v_))
        for t in extra:
            need(t)
        for sem, val in waits.items():
            E.seen[sem] = val
            E.h.wait_ge(self.sems[sem], val)
            self.ninstr += 1

    @staticmethod
    def _res(xs):
        return [x.res if isinstance(x, Tile) else x for x in xs]

    def op(self, eng, fn, r=(), w=(), sig=True):
        E = self.E[eng]
        if self.skip():
            return None
        r = self._res(r)
        w = self._res(w)
        w = w + [R for R in r if R.psum and R not in w]
        r = [R for R in r if not R.psum]
        self._waits(E, r, w)
        inst = fn(E.h)
        self.ninstr += 1
        if self.trace:
            print("OP", self.ninstr, eng, fn.__code__.co_firstlineno)
        if sig:
            E.cnt += 1
            inst.then_inc(self.sems[E.sem], 1)
            tok = (E.sem, E.cnt)
        else:
            tok = (E.sem, E.cnt + 1)
        for R in r:
            if R.r.get(tok[0], 0) < tok[1]:
                R.r[tok[0]] = tok[1]
        for R in w:
            R.w = tok
            R.r = {}
        return inst

    def dma(self, q, out, in_, r=(), w=(), **kw):
        E = self.E[q]
        if self.skip():
            return None
        r = self._res(r)
        w = self._res(w)
        slot = E.dk % NSLOT
        E.dk += 1
        sem = "d_%s%d" % (q, slot)
        prev = E.dvals[slot]
        extra = [(sem, prev)] if prev > 0 else []
        self._waits(E, r, w, extra)
        inst = E.h.dma_start(out=out, in_=in_, **kw)
        self.ninstr += 1
        if self.trace:
            import sys as _s
            print("DMA", self.ninstr, q, _s._getframe(1).f_lineno)
        E.dvals[slot] = prev + 16
        inst.then_inc(self.sems[sem], 16)
        tok = (sem, prev + 16)
        for R in r:
            if R.r.get(tok[0], 0) < tok[1]:
                R.r[tok[0]] = tok[1]
        for R in w:
            R.w = tok
            R.r = {}
        return inst

    def barrier(self):
        lim, self.limit = self.limit, 0
        self._barrier()
        self.limit = lim

    def _barrier(self):
        toks = []
        for e in self.E.values():
            if e.cnt > 0:
                toks.append((e.sem, e.cnt))
            for k in range(NSLOT):
                if e.dvals[k] > 0:
                    toks.append(("d_%s%d" % (e.name, k), e.dvals[k]))
        for e in self.E.values():
            self._waits(e, [], [], toks)


class Phase:
    def __init__(self, cx):
        self.cx = cx
        self.st = ExitStack()
        self.n = 0

    def __enter__(self):
        self.st.__enter__()
        return self

    def __exit__(self, *a):
        self.cx.barrier()
        return self.st.__exit__(*a)

    def sb(self, shape, dt, name=None):
        self.n += 1
        name = (name or "t") + "_%d_%d" % (id(self) % 100000, self.n)
        return Tile(self.st.enter_context(self.cx.nc.sbuf_tensor(name, list(shape), dt)), name)

    def ps(self, shape, dt, name=None):
        self.n += 1
        name = (name or "p") + "_%d_%d" % (id(self) % 100000, self.n)
        t = Tile(self.st.enter_context(self.cx.nc.psum_tensor(name, list(shape), dt)), name)
        t.res.psum = True
        return t


class RR:
    def __init__(self, items):
        self.items = items
        self.i = 0

    def next(self):
        x = self.items[self.i % len(self.items)]
        self.i += 1
        return x


def build(NE=NE_FULL, dbg=False, phases=("mod", "proj", "na", "df", "merge", "out", "moe", "fin")):
    nc = bass.Bass("TRN2", target_bir_lowering=False)
    st = ExitStack()
    with st:
        cx = Ctx(nc, st)

        def din(name, shape, dt=F32):
            return nc.dram_tensor(name, list(shape), dt, kind="ExternalInput").ap()

        def dscr(name, shape, dt=BF16):
            return nc.dram_tensor(name, list(shape), dt, kind="ExternalOutput" if dbg else "Internal").ap()

        xb = din("xb", [S, D])
        xo = din("xo", [TOK, D])
        xh = din("xh", [512, D])
        ctxi = din("ctx", [CTX, D])
        cc = din("cc", [128, 16, 2])
        w_ada = din("w_ada", [D, 6 * D])
        b_ada = din("b_ada", [1, 6 * D])
        w_in = din("w_in", [D, 10240])
        nab = din("nab", [NH, 128, 16, 64])
        navalid = din("navalid", [128, 16, 6])
        lamv = din("lamv", [1, 256])
        wsub = din("wsub", [128, 1])
        w_pn = din("w_proj_na", [1024, D])
        w_pd = din("w_proj_diff", [1024, D])
        w_out = din("w_out", [D, D])
        lnp = din("lnp", [4, D])
        w_r = din("w_router", [D, 32])
        b_r = din("b_router", [1, 32])
        w_g = din("w_gate", [NE, D, D])
        w_u = din("w_up", [NE, D, D])
        w_d = din("w_down", [NE, D, D])
        bgu = din("bgu", [NE, 128, 32])
        b_d = din("b_down", [NE, D])
        ropeK = din("ropeK", [2, 128, S])
        ropeQ = din("ropeQ", [2, 128, TOK])
        ident_in = din("ident", [128, 128])
        perm_in = din("perm", [128, 128])
        out = nc.dram_tensor("out", [TOK, D], F32, kind="ExternalOutput").ap()

        modv = dscr("modv", [2, 6 * D], F32)
        kdfT = dscr("kdfT", [NH, 128, NKEY_DF])
        vdf = dscr("vdf", [NKEY_DF, 1024])
        knaT = dscr("knaT", [NH, 128, NKEY_NA])
        vna = dscr("vna", [NKEY_NA, 1024])
        qnaT = dscr("qnaT", [NH, 128, TOK])
        qdfT = dscr("qdfT", [NH, 128, TOK])
        gnaT = dscr("gnaT", [16, 128, TOK])
        gdfT = dscr("gdfT", [16, 128, TOK])
        onaT = dscr("onaT", [NH, 128, TOK])
        odfT = dscr("odfT", [NH, 128, TOK])
        mrgT = dscr("mrgT", [16, 128, TOK])
        x1s = dscr("x1s", [TOK, D], F32)
        u2Ts = dscr("u2Ts", [16, 128, TOK])
        Gs = dscr("Gs", [TOK, 32], F32)
        y2s = dscr("y2s", [TOK, D], F32)

        PP = Phase(cx)
        PP.__enter__()
        ident_f = PP.sb([128, 128], F32, "identf")
        ident_b = PP.sb([128, 128], BF16, "identb")
        perm_b = PP.sb([128, 128], BF16, "permb")
        ones_b = PP.sb([128, 128], BF16, "onesb")
        ones_f = PP.sb([128, 128], F32, "onesf")
        cx.dma("sp", ident_f[:], ident_in[:, :], w=[ident_f])
        cx.dma("pool", ident_b[:], ident_in[:, :], w=[ident_b])
        cx.dma("pool", perm_b[:], perm_in[:, :], w=[perm_b])
        cx.op("dve", lambda e: e.memset(ones_b[:], 1.0), w=[ones_b])
        cx.op("dve", lambda e: e.memset(ones_f[:], 1.0), w=[ones_f])
        eps_ln = PP.sb([128, 1], F32, "epsln")
        eps_rms = PP.sb([128, 1], F32, "epsrms")
        cx.op("dve", lambda e: e.memset(eps_ln[:], LN_EPS), w=[eps_ln])
        cx.op("dve", lambda e: e.memset(eps_rms[:], RMS_EPS), w=[eps_rms])

        w_in_v = w_in.rearrange("(kc p) n -> p kc n", p=128)

        if "mod" in phases:
            with Phase(cx) as P:
                ccs = P.sb([128, 32], F32, "ccs")
                sg = P.sb([128, 32], F32, "sg")
                sil = P.sb([128, 16, 2], BF16, "sil")
                bsb = P.sb([2, 6 * D], F32, "bsb")
                msb = P.sb([2, 6 * D], F32, "msb")
                wt = [P.sb([128, 16, 512], BF16, "wada") for _ in range(3)]
                pb = [P.ps([128, 512], F32, "pm") for _ in range(2)]
                cx.dma("sp", ccs[:], cc.rearrange("p k t -> p (k t)"), w=[ccs])
                cx.dma("sp", bsb[0:1, :], b_ada[:, :], w=[bsb])
                cx.dma("sp", bsb[1:2, :], b_ada[:, :], w=[bsb])
                cx.op("act", lambda e: e.activation(out=sg[:], in_=ccs[:], func=AF.Sigmoid), r=[ccs], w=[sg])
                cx.op("dve", lambda e: e.tensor_tensor(out=sil[:].rearrange("p k t -> p (k t)"), in0=ccs[:], in1=sg[:], op=ALU.mult),
                      r=[ccs, sg], w=[sil])
                w_ada_v = w_ada.rearrange("(kc p) n -> p kc n", p=128)
                for n in range(24):
                    W = wt[n % 3]
                    cx.dma("pool", W[:], w_ada_v[:, :, n * 512:(n + 1) * 512], w=[W])
                    pp = pb[n % 2]
                    for kc in range(16):
                        cx.op("pe", lambda e, kc=kc: e.matmul(pp[0:2, :], sil[:, kc, :], W[:, kc, :], start=(kc == 0), stop=(kc == 15)),
                              r=[sil, W], w=[pp], sig=(kc == 15))
                    cx.op("dve", lambda e: e.tensor_tensor(out=msb[0:2, n * 512:(n + 1) * 512], in0=pp[0:2, :],
                                                          in1=bsb[0:2, n * 512:(n + 1) * 512], op=ALU.add),
                          r=[pp, bsb], w=[msb])
                cx.dma("sp", modv[:, :], msb[0:2, :], r=[msb])

        def mod_bc(row, idx):
            return modv[row:row + 1, idx * D:(idx + 1) * D].partition_broadcast(128)

        def load_bc(P, src_ap, name, plus1=False):
            t = P.sb([128, D], F32, name)
            cx.dma("sp", t[:], src_ap, w=[t])
            if plus1:
                cx.op("dve", lambda e: e.tensor_scalar(out=t[:], in0=t[:], scalar1=1.0, scalar2=None, op0=ALU.add), r=[t], w=[t])
            return t

        def ln_stats(P, src, tmp):
            stt, mv, rstd, nmr = tmp
            for c in range(4):
                cx.op("dve", lambda e, c=c: e.bn_stats(out=stt[:, c, :], in_=src[:, c * 512:(c + 1) * 512]), r=[src], w=[stt])
            cx.op("dve", lambda e: e.bn_aggr(out=mv[:], in_=stt[:].rearrange("p a b -> p (a b)")), r=[stt], w=[mv])
            cx.op("act", lambda e: e.activation(out=rstd[:], in_=mv[:, 1:2], func=AF.Sqrt, bias=eps_ln[:], scale=1.0), r=[mv, eps_ln], w=[rstd])
            cx.op("dve", lambda e: e.reciprocal(out=rstd[:], in_=rstd[:]), r=[rstd], w=[rstd])
            cx.op("dve", lambda e: e.tensor_scalar(out=nmr[:], in0=mv[:, 0:1], scalar1=rstd[:], scalar2=-1.0, op0=ALU.mult, op1=ALU.mult),
                  r=[mv, rstd], w=[nmr])

        def ln_tmp(P):
            return (P.sb([128, 4, 6], F32, "stt"), P.sb([128, 2], F32, "mv"), P.sb([128, 1], F32, "rstd"), P.sb([128, 1], F32, "nmr"))

        if "proj" in phases:
            with Phase(cx) as P:
                sc1_m = load_bc(P, mod_bc(0, 1), "sc1m", True)
                sh_m = load_bc(P, mod_bc(0, 0), "shm")
                sc1_c = load_bc(P, mod_bc(1, 1), "sc1c", True)
                sh_c = load_bc(P, mod_bc(1, 0), "shc")
                xt = [P.sb([128, D], F32, "xt") for _ in range(2)]
                zt = [P.sb([128, D], F32, "zt") for _ in range(2)]
                ut = [P.sb([128, D], BF16, "ut") for _ in range(2)]
                tmps = [ln_tmp(P) for _ in range(2)]
                uT = [P.sb([128, 16, 512], BF16, "uT") for _ in range(2)]
                wts = RR([P.sb([128, 16, 512], BF16, "win") for _ in range(3)])
                ptr = RR([P.ps([128, 1024], BF16, "ptr") for _ in range(2)])
                pmm = RR([P.ps([128, 512], F32, "pmm") for _ in range(4)])
                prp = pmm
                osb = RR([P.sb([128, 512], BF16, "osb") for _ in range(4)])
                xsb = RR([P.sb([128, 512], BF16, "xsb") for _ in range(2)])
                t1s = RR([P.sb([128, 512], F32, "t1s") for _ in range(2)])
                rC = RR([P.sb([128, 512], F32, "rC") for _ in range(2)])
                rS = RR([P.sb([128, 512], F32, "rS") for _ in range(2)])
                tcount = [0]

                def ln_block(src_rows, ntok, sc1, sh, U):
                    for t in range(ntok // 128):
                        k = tcount[0] % 2
                        tcount[0] += 1
                        X, Z, Ub, tmp = xt[k], zt[k], ut[k], tmps[k]
                        cx.dma("sp", X[:], src_rows[t * 128:(t + 1) * 128, :], w=[X])
                        ln_stats(P, X, tmp)
                        cx.op("act", lambda e: e.activation(out=Z[:], in_=X[:], func=AF.Identity, scale=tmp[2][:], bias=tmp[3][:]),
                              r=[X, tmp[2], tmp[3]], w=[Z])
                        cx.op("dve", lambda e: e.tensor_tensor(out=Z[:], in0=Z[:], in1=sc1[:], op=ALU.mult), r=[Z, sc1], w=[Z])
                        cx.op("dve", lambda e: e.tensor_tensor(out=Ub[:], in0=Z[:], in1=sh[:], op=ALU.add), r=[Z, sh], w=[Ub])
                        for g in range(2):
                            pt = ptr.next()
                            for q in range(8):
                                kc = g * 8 + q
                                cx.op("pe", lambda e, kc=kc, q=q: e.transpose(out=pt[:, q * 128:(q + 1) * 128], in_=Ub[:, kc * 128:(kc + 1) * 128],
                                                                            identity=ident_b[:]),
                                      r=[Ub, ident_b], w=[pt], sig=(q == 7))
                            cx.op("act", lambda e, g=g: e.copy(out=U[:, g * 8:(g + 1) * 8, t * 128:(t + 1) * 128],
                                                              in_=pt[:].rearrange("p (q n) -> p q n", q=8)),
                                  r=[pt], w=[U])

                def proj(U, ntok, col0, ncols, mode, dest, rope=None):
                    for cb in range(ncols // 512):
                        W = wts.next()
                        c0 = col0 + cb * 512
                        cx.dma("pool", W[:], w_in_v[:, :, c0:c0 + 512], w=[W])
                        if mode == "FM":
                            for sub in range(4):
                                pm = pmm.next()
                                for kc in range(16):
                                    cx.op("pe", lambda e, kc=kc: e.matmul(pm[:, 0:ntok], W[:, kc, sub * 128:(sub + 1) * 128], U[:, kc, 0:ntok],
                                                                         start=(kc == 0), stop=(kc == 15)),
                                          r=[W, U], w=[pm], sig=(kc == 15))
                                ci = cb * 4 + sub
                                if rope is None:
                                    O = osb.next()
                                    cx.op("act", lambda e: e.copy(out=O[:, 0:ntok], in_=pm[:, 0:ntok]), r=[pm], w=[O])
                                    dest(ci, O)
                                else:
                                    Cc, Ss = rope
                                    Xs = xsb.next()
                                    cx.op("act", lambda e: e.copy(out=Xs[:, 0:ntok], in_=pm[:, 0:ntok]), r=[pm], w=[Xs])
                                    pr = prp.next()
                                    cx.op("pe", lambda e: e.matmul(pr[:, 0:ntok], perm_b[:], Xs[:, 0:ntok], start=True, stop=True),
                                          r=[perm_b, Xs], w=[pr])
                                    T1 = t1s.next()
                                    cx.op("dve", lambda e: e.tensor_tensor(out=T1[:, 0:ntok], in0=Xs[:, 0:ntok], in1=Cc[:, 0:ntok], op=ALU.mult),
                                          r=[Xs, Cc], w=[T1])
                                    T2 = t1s.next()
                                    cx.op("dve", lambda e: e.tensor_tensor(out=T2[:, 0:ntok], in0=pr[:, 0:ntok], in1=Ss[:, 0:ntok], op=ALU.mult),
                                          r=[pr, Ss], w=[T2])
                                    O = osb.next()
                                    cx.op("dve", lambda e: e.tensor_tensor(out=O[:, 0:ntok], in0=T1[:, 0:ntok], in1=T2[:, 0:ntok], op=ALU.add),
                                          r=[T1, T2], w=[O])
                                    dest(ci, O)
                        else:
                            for t in range(ntok // 128):
                                pm = pmm.next()
                                for kc in range(16):
                                    cx.op("pe", lambda e, kc=kc: e.matmul(pm[:, :], U[:, kc, t * 128:(t + 1) * 128], W[:, kc, :],
                                                                         start=(kc == 0), stop=(kc == 15)),
                                          r=[W, U], w=[pm], sig=(kc == 15))
                                O = osb.next()
                                cx.op("act", lambda e: e.copy(out=O[:], in_=pm[:]), r=[pm], w=[O])
                                dest(t, cb, O)

                def st_fm(dst, tok0, ntok):
                    return lambda ci, O: cx.dma("sp", dst[ci, :, tok0:tok0 + ntok], O[:, 0:ntok], r=[O])

                def st_tm(dst, row0):
                    return lambda t, cb, O: cx.dma("sp", dst[row0 + t * 128:row0 + (t + 1) * 128, cb * 512:(cb + 1) * 512], O[:], r=[O])

                ub = [0]

                def nextU():
                    ub[0] += 1
                    return uT[ub[0] % 2]

                for blk in range(S // 512):
                    U = nextU()
                    ln_block(xb[blk * 512:(blk + 1) * 512, :], 512, sc1_m, sh_m, U)
                    Cc, Ss = rC.next(), rS.next()
                    cx.dma("sp", Cc[:], ropeK[0, :, blk * 512:(blk + 1) * 512], w=[Cc])
                    cx.dma("sp", Ss[:], ropeK[1, :, blk * 512:(blk + 1) * 512], w=[Ss])
                    proj(U, 512, 2048, 1024, "FM", st_fm(kdfT, blk * 512, 512), rope=(Cc, Ss))
                    proj(U, 512, 3072, 1024, "TM", st_tm(vdf, blk * 512))
                U = nextU()
                ln_block(ctxi, CTX, sc1_c, sh_c, U)
                proj(U, CTX, 0, 1024, "FM", st_fm(knaT, 1536, CTX))
                proj(U, CTX, 1024, 1024, "TM", st_tm(vna, 1536))
                proj(U, CTX, 2048, 1024, "FM", st_fm(kdfT, S, CTX))
                proj(U, CTX, 3072, 1024, "TM", st_tm(vdf, S))
                U = nextU()
                ln_block(xh, 512, sc1_m, sh_m, U)

                def st_halo_fm(ci, O):
                    cx.dma("sp", knaT[ci, :, 0:256], O[:, 0:256], r=[O])
                    cx.dma("sp", knaT[ci, :, 1280:1536], O[:, 256:512], r=[O])

                def st_halo_tm(t, cb, O):
                    row0 = t * 128 if t < 2 else 1280 + (t - 2) * 128
                    cx.dma("sp", vna[row0:row0 + 128, cb * 512:(cb + 1) * 512], O[:], r=[O])

                proj(U, 512, 0, 1024, "FM", st_halo_fm)
                proj(U, 512, 1024, 1024, "TM", st_halo_tm)
                for blk in range(2):
                    U = nextU()
                    ln_block(xo[blk * 512:(blk + 1) * 512, :], 512, sc1_m, sh_m, U)
                    Cc, Ss = rC.next(), rS.next()
                    cx.dma("sp", Cc[:], ropeQ[0, :, blk * 512:(blk + 1) * 512], w=[Cc])
                    cx.dma("sp", Ss[:], ropeQ[1, :, blk * 512:(blk + 1) * 512], w=[Ss])
                    proj(U, 512, 0, 1024, "FM", st_fm(knaT, 256 + blk * 512, 512))
                    proj(U, 512, 1024, 1024, "TM", st_tm(vna, 256 + blk * 512))
                    proj(U, 512, 4096, 1024, "FM", st_fm(qnaT, blk * 512, 512))
                    proj(U, 512, 5120, 1024, "FM", st_fm(qdfT, blk * 512, 512), rope=(Cc, Ss))
                    proj(U, 512, 6144, 2048, "FM", st_fm(gnaT, blk * 512, 512))
                    proj(U, 512, 8192, 2048, "FM", st_fm(gdfT, blk * 512, 512))

        if "na" in phases:
            with Phase(cx) as P:
                val = P.sb([128, 16, 6], F32, "val")
                cx.dma("sp", val[:], navalid[:, :, :], w=[val])
                kT = [P.sb([128, NKEY_NA], BF16, "kT") for _ in range(2)]
                vv = [P.sb([128, 14, 128], BF16, "vv") for _ in range(2)]
                qT = [P.sb([128, TOK], BF16, "qT") for _ in range(2)]
                nbf = [P.sb([128, 16, 64], F32, "nbf") for _ in range(2)]
                EB = [P.sb([128, 16, 64], BF16, "EB") for _ in range(2)]
                oT = [P.sb([128, TOK], BF16, "oT") for _ in range(2)]
                pS = RR([P.ps([128, 512], F32, "pS") for _ in range(2)])
                pO = RR([P.ps([128, 512], F32, "pO") for _ in range(2)])
                pZ = RR([P.ps([128, 512], F32, "pZ") for _ in range(2)])
                esb = RR([P.sb([128, 512], BF16, "esb") for _ in range(3)])
                e2 = RR([P.sb([128, 512], BF16, "e2") for _ in range(3)])
                rsb = RR([P.sb([128, 64], F32, "rsb") for _ in range(2)])
                scale = 128 ** -0.5
                for h in range(NH):
                    K_, V_, Q_, NB_, EB_, O_ = kT[h % 2], vv[h % 2], qT[h % 2], nbf[h % 2], EB[h % 2], oT[h % 2]
                    cx.dma("sp", K_[:], knaT[h, :, :], w=[K_])
                    cx.dma("sp", V_[:], vna.rearrange("(c p) d -> p c d", p=128)[:, :, h * 128:(h + 1) * 128], w=[V_])
                    cx.dma("sp", Q_[:], qnaT[h, :, :], w=[Q_])
                    cx.dma("sp", NB_[:], nab[h, :, :, :], w=[NB_])
                    cx.op("act", lambda e: e.activation(out=EB_[:], in_=NB_[:], func=AF.Exp), r=[NB_], w=[EB_])
                    for i in range(16):
                        lo, hi = min(i, 12), max(i + 8, 12)
                        ms = list(range(lo // 2, (hi + 1) // 2))
                        nl = len(ms)
                        chunks = ms + [12, 13]
                        ps = pS.next()
                        q = Q_[:, i * 64:(i + 1) * 64]
                        for ci, m in enumerate(chunks):
                            cx.op("pe", lambda e, ci=ci, m=m: e.matmul(ps[:, ci * 64:(ci + 1) * 64], K_[:, m * 128:(m + 1) * 128], q,
                                                                       start=True, stop=True),
                                  r=[K_, Q_], w=[ps], sig=(ci == len(chunks) - 1))
                        nc_ = len(chunks)
                        E1 = esb.next()
                        cx.op("act", lambda e: e.activation(out=E1[:, 0:nc_ * 64], in_=ps[:, 0:nc_ * 64], func=AF.Exp, scale=scale), r=[ps], w=[E1])
                        idx0 = 2 * ms[0] - i - 4 + 8
                        E2 = e2.next()
                        cx.op("dve", lambda e: e.tensor_tensor(out=E2[:, 0:nl * 64].rearrange("p (c n) -> p c n", n=64),
                                                              in0=E1[:, 0:nl * 64].rearrange("p (c n) -> p c n", n=64),
                                                              in1=EB_[:, idx0:idx0 + 2 * nl - 1:2, :], op=ALU.mult),
                              r=[E1, EB_], w=[E2])
                        cx.op("dve", lambda e: e.tensor_tensor(out=E1[:, 0:nl * 64].rearrange("p (c n) -> p c n", n=64),
                                                              in0=E2[:, 0:nl * 64].rearrange("p (c n) -> p c n", n=64),
                                                              in1=val[:, i, 0:nl].unsqueeze(2).to_broadcast([128, nl, 64]), op=ALU.mult),
                              r=[E2, val], w=[E1])
                        po, pz = pO.next(), pZ.next()
                        for ci, m in enumerate(chunks):
                            last = ci == len(chunks) - 1
                            cx.op("pe", lambda e, ci=ci, m=m: e.matmul(po[:, 0:64], V_[:, m, :], E1[:, ci * 64:(ci + 1) * 64],
                                                                       start=(ci == 0), stop=last), r=[V_, E1], w=[po], sig=False)
                            cx.op("pe", lambda e, ci=ci: e.matmul(pz[:, 0:64], ones_b[:], E1[:, ci * 64:(ci + 1) * 64],
                                                                  start=(ci == 0), stop=last), r=[ones_b, E1], w=[pz], sig=last)
                        R_ = rsb.next()
                        cx.op("dve", lambda e: e.reciprocal(out=R_[:], in_=pz[:, 0:64]), r=[pz, po], w=[R_])
                        cx.op("dve", lambda e: e.tensor_tensor(out=O_[:, i * 64:(i + 1) * 64], in0=po[:, 0:64], in1=R_[:], op=ALU.mult),
                              r=[po, R_], w=[O_])
                    cx.dma("sp", onaT[h, :, :], O_[:], r=[O_])

        if "df" in phases:
            with Phase(cx) as P:
                lv = P.sb([128, 4, 64], F32, "lv")
                lp = P.sb([128, 2, 64], F32, "lp")
                ls = P.sb([128, 2], F32, "ls")
                le = P.sb([128, 2], F32, "le")
                nlam = P.sb([128, 1], F32, "nlam")
                wsc = P.sb([128, 1], F32, "wsc")
                cx.dma("sp", lv[:].rearrange("p a d -> p (a d)"), lamv[0:1, :].partition_broadcast(128), w=[lv])
                cx.dma("sp", wsc[:], wsub[:, :], w=[wsc])
                cx.op("dve", lambda e: e.tensor_tensor(out=lp[:, 0, :], in0=lv[:, 0, :], in1=lv[:, 1, :], op=ALU.mult), r=[lv], w=[lp])
                cx.op("dve", lambda e: e.tensor_tensor(out=lp[:, 1, :], in0=lv[:, 2, :], in1=lv[:, 3, :], op=ALU.mult), r=[lv], w=[lp])
                cx.op("dve", lambda e: e.reduce_sum(out=ls[:], in_=lp[:], axis=AX.X), r=[lp], w=[ls])
                cx.op("act", lambda e: e.activation(out=le[:], in_=ls[:], func=AF.Exp), r=[ls], w=[le])
                cx.op("dve", lambda e: e.tensor_scalar(out=nlam[:], in0=le[:, 1:2], scalar1=le[:, 0:1], scalar2=-LAM_INIT, op0=ALU.subtract, op1=ALU.add),
                      r=[le], w=[nlam])
                kT = [P.sb([128, NKEY_DF], BF16, "kT") for _ in range(2)]
                vv = [P.sb([128, 34, 128], BF16, "vv") for _ in range(2)]
                qT = [P.sb([128, TOK], BF16, "qT") for _ in range(2)]
                oT = [P.sb([128, TOK], BF16, "oT") for _ in range(2)]
                pS = RR([P.ps([128, 512], F32, "pS") for _ in range(3)])
                pO = [P.ps([128, 512], F32, "pO") for _ in range(2)]
                pZ = [P.ps([128, 512], F32, "pZ") for _ in range(2)]
                esb = RR([P.sb([128, 512], BF16, "esb") for _ in range(4)])
                r0 = P.sb([128, 512], F32, "r0")
                r1 = P.sb([128, 512], F32, "r1")
                t0 = P.sb([128, 512], F32, "t0")
                t1 = P.sb([128, 512], F32, "t1")
                of = P.sb([128, 512], F32, "of")
                sq = P.sb([128, 512], F32, "sq")
                rs = P.sb([128, 512], F32, "rs")
                NKC = NKEY_DF // 128
                for h in range(NH):
                    K_, V_, Q_, O_ = kT[h % 2], vv[h % 2], qT[h % 2], oT[h % 2]
                    cx.dma("sp", K_[:], kdfT[h, :, :], w=[K_])
                    cx.dma("sp", V_[:], vdf.rearrange("(c p) d -> p c d", p=128)[:, :, h * 128:(h + 1) * 128], w=[V_])
                    cx.dma("sp", Q_[:], qdfT[h, :, :], w=[Q_])
                    for qb in range(2):
                        for kc in range(NKC):
                            for m in range(2):
                                ps = pS.next()
                                cx.op("pe", lambda e, m=m, kc=kc: e.matmul(ps[:, :], K_[m * 64:(m + 1) * 64, kc * 128:(kc + 1) * 128],
                                                                           Q_[m * 64:(m + 1) * 64, qb * 512:(qb + 1) * 512], start=True, stop=True),
                                      r=[K_, Q_], w=[ps])
                                E1 = esb.next()
                                cx.op("act", lambda e: e.activation(out=E1[:], in_=ps[:], func=AF.Exp, scale=0.125), r=[ps], w=[E1])
                                last = kc == NKC - 1
                                cx.op("pe", lambda e, m=m, kc=kc: e.matmul(pO[m][:, :], V_[:, kc, :], E1[:], start=(kc == 0), stop=last),
                                      r=[V_, E1], w=[pO[m]], sig=False)
                                cx.op("pe", lambda e, m=m, kc=kc: e.matmul(pZ[m][:, :], ones_b[:], E1[:], start=(kc == 0), stop=last),
                                      r=[ones_b, E1], w=[pZ[m]], sig=last)
                        cx.op("dve", lambda e: e.reciprocal(out=r0[:], in_=pZ[0][:]), r=[pZ[0]], w=[r0])
                        cx.op("dve", lambda e: e.reciprocal(out=r1[:], in_=pZ[1][:]), r=[pZ[1]], w=[r1])
                        cx.op("dve", lambda e: e.tensor_scalar(out=r1[:], in0=r1[:], scalar1=nlam[:], scalar2=None, op0=ALU.mult), r=[r1, nlam], w=[r1])
                        cx.op("dve", lambda e: e.tensor_tensor(out=t0[:], in0=pO[0][:], in1=r0[:], op=ALU.mult), r=[pO[0], r0], w=[t0])
                        cx.op("dve", lambda e: e.tensor_tensor(out=t1[:], in0=pO[1][:], in1=r1[:], op=ALU.mult), r=[pO[1], r1], w=[t1])
                        cx.op("dve", lambda e: e.tensor_tensor(out=of[:], in0=t0[:], in1=t1[:], op=ALU.add), r=[t0, t1], w=[of])
                        cx.op("act", lambda e: e.activation(out=sq[:], in_=of[:], func=AF.Square), r=[of], w=[sq])
                        ps = pS.next()
                        cx.op("pe", lambda e: e.matmul(ps[:, :], ones_f[:], sq[:], start=True, stop=True), r=[ones_f, sq], w=[ps])
                        cx.op("act", lambda e: e.activation(out=rs[:], in_=ps[:], func=AF.Sqrt, bias=eps_rms[:], scale=1.0 / 128), r=[ps, eps_rms], w=[rs])
                        cx.op("dve", lambda e: e.reciprocal(out=rs[:], in_=rs[:]), r=[rs], w=[rs])
                        cx.op("dve", lambda e: e.tensor_tensor(out=of[:], in0=of[:], in1=rs[:], op=ALU.mult), r=[of, rs], w=[of])
                        cx.op("dve", lambda e: e.tensor_scalar(out=O_[:, qb * 512:(qb + 1) * 512], in0=of[:], scalar1=wsc[:], scalar2=1.0 - LAM_INIT,
                                                              op0=ALU.mult, op1=ALU.mult), r=[of, wsc], w=[O_])
                    cx.dma("sp", odfT[h, :, :], O_[:], r=[O_])

        if "merge" in phases:
            with Phase(cx) as P:
                wpn = P.sb([128, 8, D], BF16, "wpn")
                wpd = P.sb([128, 8, D], BF16, "wpd")
                on = P.sb([128, 8, TOK], BF16, "on")
                od = P.sb([128, 8, TOK], BF16, "od")
                cx.dma("pool", wpn[:], w_pn.rearrange("(h p) n -> p h n", p=128), w=[wpn])
                cx.dma("pool", wpd[:], w_pd.rearrange("(h p) n -> p h n", p=128), w=[wpd])
                cx.dma("sp", on[:], onaT.rearrange("h p t -> p h t"), w=[on])
                cx.dma("sp", od[:], odfT.rearrange("h p t -> p h t"), w=[od])
                gn = RR([P.sb([128, TOK], BF16, "gn") for _ in range(2)])
                gd = RR([P.sb([128, TOK], BF16, "gd") for _ in range(2)])
                pA = RR([P.ps([128, 512], F32, "pA") for _ in range(2)])
                pD = RR([P.ps([128, 512], F32, "pD") for _ in range(2)])
                ta = RR([P.sb([128, 512], F32, "ta") for _ in range(2)])
                tb = RR([P.sb([128, 512], F32, "tb") for _ in range(2)])
                mo = RR([P.sb([128, TOK], BF16, "mo") for _ in range(2)])
                for fc in range(16):
                    Gn, Gd, Mo = gn.next(), gd.next(), mo.next()
                    cx.dma("sp", Gn[:], gnaT[fc, :, :], w=[Gn])
                    cx.dma("sp", Gd[:], gdfT[fc, :, :], w=[Gd])
                    cx.op("act", lambda e: e.activation(out=Gn[:], in_=Gn[:], func=AF.Sigmoid), r=[Gn], w=[Gn])
                    cx.op("act", lambda e: e.activation(out=Gd[:], in_=Gd[:], func=AF.Sigmoid), r=[Gd], w=[Gd])
                    for hf in range(2):
                        pa, pd = pA.next(), pD.next()
                        for h in range(8):
                            cx.op("pe", lambda e, h=h: e.matmul(pa[:, :], wpn[:, h, fc * 128:(fc + 1) * 128], on[:, h, hf * 512:(hf + 1) * 512],
                                                                start=(h == 0), stop=(h == 7)), r=[wpn, on], w=[pa], sig=(h == 7))
                        for h in range(8):
                            cx.op("pe", lambda e, h=h: e.matmul(pd[:, :], wpd[:, h, fc * 128:(fc + 1) * 128], od[:, h, hf * 512:(hf + 1) * 512],
                                                                start=(h == 0), stop=(h == 7)), r=[wpd, od], w=[pd], sig=(h == 7))
                        Ta, Tb = ta.next(), tb.next()
                        cx.op("dve", lambda e: e.tensor_tensor(out=Ta[:], in0=pa[:], in1=Gn[:, hf * 512:(hf + 1) * 512], op=ALU.mult), r=[pa, Gn], w=[Ta])
                        cx.op("dve", lambda e: e.tensor_tensor(out=Tb[:], in0=pd[:], in1=Gd[:, hf * 512:(hf + 1) * 512], op=ALU.mult), r=[pd, Gd], w=[Tb])
                        cx.op("dve", lambda e: e.tensor_tensor(out=Mo[:, hf * 512:(hf + 1) * 512], in0=Ta[:], in1=Tb[:], op=ALU.add), r=[Ta, Tb], w=[Mo])
                    cx.dma("sp", mrgT[fc, :, :], Mo[:], r=[Mo])

        if "out" in phases:
            with Phase(cx) as P:
                wo = P.sb([128, 16, D], BF16, "wo")
                cx.dma("pool", wo[:], w_out.rearrange("(kc p) n -> p kc n", p=128), w=[wo])
                gm = load_bc(P, mod_bc(0, 2), "gm")
                l1w = load_bc(P, lnp[0:1, :].partition_broadcast(128), "l1w")
                l1b = load_bc(P, lnp[1:2, :].partition_broadcast(128), "l1b")
                sc1f = load_bc(P, mod_bc(0, 4), "sc1f", True)
                shf = load_bc(P, mod_bc(0, 3), "shf")
                wr = P.sb([128, 16, 32], F32, "wr")
                cx.dma("sp", wr[:], w_r.rearrange("(kc p) e -> p kc e", p=128), w=[wr])
                brb = P.sb([128, 32], F32, "brb")
                cx.dma("sp", brb[:], b_r[0:1, :].partition_broadcast(128), w=[brb])
                mT = RR([P.sb([128, 16, 128], BF16, "mT") for _ in range(2)])
                xt = RR([P.sb([128, D], F32, "xt") for _ in range(2)])
                vt = RR([P.sb([128, D], F32, "vt") for _ in range(1)])
                ut = RR([P.sb([128, D], F32, "ut") for _ in range(1)])
                tmps = RR([ln_tmp(P) for _ in range(2)])
                py = RR([P.ps([128, 512], F32, "py") for _ in range(4)])
                ptr = RR([P.ps([128, 512], F32, "ptr") for _ in range(2)])
                plg = RR([P.ps([128, 512], F32, "plg") for _ in range(2)])
                uTf = RR([P.sb([128, 16, 128], F32, "uTf") for _ in range(2)])
                uTb = RR([P.sb([128, 16, 128], BF16, "uTb") for _ in range(2)])
                lg = RR([P.sb([128, 32], F32, "lg") for _ in range(2)])
                m8 = RR([P.sb([128, 8], F32, "m8") for _ in range(2)])
                nmx = RR([P.sb([128, 1], F32, "nmx") for _ in range(2)])
                msk = RR([P.sb([128, 32], F32, "msk") for _ in range(2)])
                ex = RR([P.sb([128, 32], F32, "ex") for _ in range(2)])
                sm = RR([P.sb([128, 1], F32, "sm") for _ in range(2)])
                gt = RR([P.sb([128, 32], F32, "gt") for _ in range(2)])
                for t in range(8):
                    M_ = mT.next()
                    cx.dma("sp", M_[:], mrgT.rearrange("k p t -> p k t")[:, :, t * 128:(t + 1) * 128], w=[M_])
                    X = xt.next()
                    cx.dma("sp", X[:], xo[t * 128:(t + 1) * 128, :], w=[X])
                    V = vt.next()
                    for cb in range(4):
                        pp = py.next()
                        for kc in range(16):
                            cx.op("pe", lambda e, kc=kc: e.matmul(pp[:, :], M_[:, kc, :], wo[:, kc, cb * 512:(cb + 1) * 512], start=(kc == 0), stop=(kc == 15)),
                                  r=[M_, wo], w=[pp], sig=(kc == 15))
                        cx.op("dve", lambda e: e.tensor_tensor(out=V[:, cb * 512:(cb + 1) * 512], in0=pp[:], in1=gm[:, cb * 512:(cb + 1) * 512], op=ALU.mult),
                              r=[pp, gm], w=[V])
                    cx.op("dve", lambda e: e.scalar_tensor_tensor(out=V[:], in0=X[:], scalar=ALPHA, in1=V[:], op0=ALU.mult, op1=ALU.add), r=[X, V], w=[V])
                    tmp = tmps.next()
                    ln_stats(P, V, tmp)
                    cx.op("act", lambda e: e.activation(out=V[:], in_=V[:], func=AF.Identity, scale=tmp[2][:], bias=tmp[3][:]), r=[V, tmp[2], tmp[3]], w=[V])
                    cx.op("dve", lambda e: e.tensor_tensor(out=V[:], in0=V[:], in1=l1w[:], op=ALU.mult), r=[V, l1w], w=[V])
                    cx.op("dve", lambda e: e.tensor_tensor(out=X[:], in0=V[:], in1=l1b[:], op=ALU.add), r=[V, l1b], w=[X])
                    cx.dma("sp", x1s[t * 128:(t + 1) * 128, :], X[:], r=[X])
                    ln_stats(P, X, tmp)
                    U = ut.next()
                    cx.op("act", lambda e: e.activation(out=U[:], in_=X[:], func=AF.Identity, scale=tmp[2][:], bias=tmp[3][:]), r=[X, tmp[2], tmp[3]], w=[U])
                    cx.op("dve", lambda e: e.tensor_tensor(out=U[:], in0=U[:], in1=sc1f[:], op=ALU.mult), r=[U, sc1f], w=[U])
                    cx.op("dve", lambda e: e.tensor_tensor(out=U[:], in0=U[:], in1=shf[:], op=ALU.add), r=[U, shf], w=[U])
                    UF, UB = uTf.next(), uTb.next()
                    for g in range(4):
                        pt = ptr.next()
                        for q in range(4):
                            kc = g * 4 + q
                            cx.op("pe", lambda e, kc=kc, q=q: e.transpose(out=pt[:, q * 128:(q + 1) * 128], in_=U[:, kc * 128:(kc + 1) * 128], identity=ident_f[:]),
                                  r=[U, ident_f], w=[pt], sig=(q == 3))
                        cx.op("act", lambda e, g=g: e.copy(out=UF[:, g * 4:(g + 1) * 4, :], in_=pt[:].rearrange("p (q n) -> p q n", q=4)), r=[pt], w=[UF])
                        cx.op("dve", lambda e, g=g: e.tensor_copy(out=UB[:, g * 4:(g + 1) * 4, :], in_=pt[:].rearrange("p (q n) -> p q n", q=4)), r=[pt], w=[UB])
                    cx.dma("sp", u2Ts.rearrange("k p t -> p k t")[:, :, t * 128:(t + 1) * 128], UB[:], r=[UB])
                    pl = plg.next()
                    for kc in range(16):
                        cx.op("pe", lambda e, kc=kc: e.matmul(pl[:, 0:32], UF[:, kc, :], wr[:, kc, :], start=(kc == 0), stop=(kc == 15)),
                              r=[UF, wr], w=[pl], sig=(kc == 15))
                    L, M8, NM, MK, EX, SM, GT = lg.next(), m8.next(), nmx.next(), msk.next(), ex.next(), sm.next(), gt.next()
                    cx.op("dve", lambda e: e.tensor_tensor(out=L[:], in0=pl[:, 0:32], in1=brb[:], op=ALU.add), r=[pl, brb], w=[L])
                    cx.op("dve", lambda e: e.max(out=M8[:], in_=L[:]), r=[L], w=[M8])
                    cx.op("dve", lambda e: e.tensor_scalar(out=NM[:], in0=M8[:, 0:1], scalar1=-1.0, scalar2=None, op0=ALU.mult), r=[M8], w=[NM])
                    cx.op("dve", lambda e: e.tensor_scalar(out=MK[:], in0=L[:], scalar1=M8[:, 3:4], scalar2=None, op0=ALU.is_ge), r=[L, M8], w=[MK])
                    cx.op("act", lambda e: e.activation(out=EX[:], in_=L[:], func=AF.Exp, bias=NM[:], scale=1.0), r=[L, NM], w=[EX])
                    cx.op("dve", lambda e: e.tensor_tensor(out=EX[:], in0=EX[:], in1=MK[:], op=ALU.mult), r=[EX, MK], w=[EX])
                    cx.op("dve", lambda e: e.reduce_sum(out=SM[:], in_=EX[:], axis=AX.X), r=[EX], w=[SM])
                    cx.op("dve", lambda e: e.reciprocal(out=SM[:], in_=SM[:]), r=[SM], w=[SM])
                    cx.op("dve", lambda e: e.tensor_scalar(out=GT[:], in0=EX[:], scalar1=SM[:], scalar2=None, op0=ALU.mult), r=[EX, SM], w=[GT])
                    cx.dma("sp", Gs[t * 128:(t + 1) * 128, :], GT[:], r=[GT])

        if "moe" in phases:
            with Phase(cx) as P:
                u2 = P.sb([128, 16, 512], BF16, "u2")
                G = P.sb([128, 4, 32], F32, "G")
                acc = P.sb([128, 4, D], F32, "acc")
                hT = [P.sb([128, 16, 512], BF16, "hT") for _ in range(1)]
                wgt = RR([P.sb([128, 16, 512], BF16, "wg") for _ in range(2)])
                wut = RR([P.sb([128, 16, 512], BF16, "wu") for _ in range(2)])
                wdt = RR([P.sb([128, 16, 512], BF16, "wd") for _ in range(2)])
                bg = RR([P.sb([128, 32], F32, "bg") for _ in range(2)])
                bd = RR([P.sb([1, D], BF16, "bd") for _ in range(2)])
                pg = RR([P.ps([128, 512], F32, "pg") for _ in range(2)])
                pu = RR([P.ps([128, 512], F32, "pu") for _ in range(2)])
                pyy = RR([P.ps([128, 512], F32, "pyy") for _ in range(2)])
                gs = RR([P.sb([128, 512], F32, "gs") for _ in range(2)])
                sgs = RR([P.sb([128, 512], F32, "sgs") for _ in range(2)])
                ls_ = RR([P.sb([128, 512], F32, "ls") for _ in range(2)])
                for hf in range(2):
                    cx.dma("sp", u2[:], u2Ts.rearrange("k p t -> p k t")[:, :, hf * 512:(hf + 1) * 512], w=[u2])
                    cx.dma("sp", G[:], Gs.rearrange("(t p) e -> p t e", p=128)[:, hf * 4:(hf + 1) * 4, :], w=[G])
                    cx.op("dve", lambda e: e.memset(acc[:], 0.0), w=[acc])
                    for ex_ in range(NE):
                        H = hT[0]
                        BG, BD = bg.next(), bd.next()
                        cx.dma("sp", BG[:], bgu[ex_, :, :], w=[BG])
                        cx.dma("pool", BD[:], b_d[ex_:ex_ + 1, :], w=[BD])
                        wg_v = w_g[ex_].rearrange("(kc p) f -> p kc f", p=128)
                        wu_v = w_u[ex_].rearrange("(kc p) f -> p kc f", p=128)
                        wd_v = w_d[ex_].rearrange("(kc p) f -> p kc f", p=128)
                        for fb in range(4):
                            WG, WU = wgt.next(), wut.next()
                            cx.dma("pool", WG[:], wg_v[:, :, fb * 512:(fb + 1) * 512], w=[WG])
                            cx.dma("pool", WU[:], wu_v[:, :, fb * 512:(fb + 1) * 512], w=[WU])
                            for sub in range(4):
                                fc = fb * 4 + sub
                                p1, p2 = pg.next(), pu.next()
                                for kc in range(16):
                                    cx.op("pe", lambda e, kc=kc: e.matmul(p1[:, :], WG[:, kc, sub * 128:(sub + 1) * 128], u2[:, kc, :], start=(kc == 0), stop=(kc == 15)),
                                          r=[WG, u2], w=[p1], sig=(kc == 15))
                                for kc in range(16):
                                    cx.op("pe", lambda e, kc=kc: e.matmul(p2[:, :], WU[:, kc, sub * 128:(sub + 1) * 128], u2[:, kc, :], start=(kc == 0), stop=(kc == 15)),
                                          r=[WU, u2], w=[p2], sig=(kc == 15))
                                GS, SG, LS = gs.next(), sgs.next(), ls_.next()
                                cx.op("dve", lambda e: e.tensor_scalar(out=GS[:], in0=p1[:], scalar1=BG[:, fc:fc + 1], scalar2=7.0, op0=ALU.add, op1=ALU.min),
                                      r=[p1, BG], w=[GS])
                                cx.op("act", lambda e: e.activation(out=SG[:], in_=GS[:], func=AF.Sigmoid, scale=1.702), r=[GS], w=[SG])
                                cx.op("dve", lambda e: e.tensor_scalar(out=LS[:], in0=p2[:], scalar1=BG[:, 16 + fc:17 + fc], scalar2=7.0, op0=ALU.add, op1=ALU.min),
                                      r=[p2, BG], w=[LS])
                                cx.op("dve", lambda e: e.tensor_scalar(out=LS[:], in0=LS[:], scalar1=-7.0, scalar2=1.0, op0=ALU.max, op1=ALU.add), r=[LS], w=[LS])
                                cx.op("dve", lambda e: e.tensor_tensor(out=GS[:], in0=GS[:], in1=SG[:], op=ALU.mult), r=[GS, SG], w=[GS])
                                cx.op("dve", lambda e: e.tensor_tensor(out=H[:, fc, :], in0=GS[:], in1=LS[:], op=ALU.mult), r=[GS, LS], w=[H])
                        for db in range(4):
                            WD = wdt.next()
                            cx.dma("pool", WD[:], wd_v[:, :, db * 512:(db + 1) * 512], w=[WD])
                            for t in range(4):
                                pp = pyy.next()
                                for fc in range(16):
                                    cx.op("pe", lambda e, fc=fc: e.matmul(pp[:, :], H[:, fc, t * 128:(t + 1) * 128], WD[:, fc, :], start=(fc == 0), stop=False),
                                          r=[H, WD], w=[pp], sig=False)
                                cx.op("pe", lambda e: e.matmul(pp[:, :], ones_b[0:1, :], BD[0:1, db * 512:(db + 1) * 512], start=False, stop=True),
                                      r=[ones_b, BD], w=[pp])
                                cx.op("dve", lambda e: e.scalar_tensor_tensor(out=acc[:, t, db * 512:(db + 1) * 512], in0=pp[:], scalar=G[:, t, ex_:ex_ + 1],
                                                                             in1=acc[:, t, db * 512:(db + 1) * 512], op0=ALU.mult, op1=ALU.add),
                                      r=[pp, G, acc], w=[acc])
                    for t in range(4):
                        row0 = hf * 512 + t * 128
                        cx.dma("sp", y2s[row0:row0 + 128, :], acc[:, t, :], r=[acc])

        if "fin" in phases:
            with Phase(cx) as P:
                gf = load_bc(P, mod_bc(0, 5), "gf")
                l2w = load_bc(P, lnp[2:3, :].partition_broadcast(128), "l2w")
                l2b = load_bc(P, lnp[3:4, :].partition_broadcast(128), "l2b")
                xt = RR([P.sb([128, D], F32, "xt") for _ in range(2)])
                yt = RR([P.sb([128, D], F32, "yt") for _ in range(2)])
                tmps = RR([ln_tmp(P) for _ in range(2)])
                for t in range(8):
                    X, Y = xt.next(), yt.next()
                    row0 = t * 128
                    cx.dma("sp", X[:], x1s[row0:row0 + 128, :], w=[X])
                    cx.dma("sp", Y[:], y2s[row0:row0 + 128, :], w=[Y])
                    cx.op("dve", lambda e: e.tensor_tensor(out=Y[:], in0=Y[:], in1=gf[:], op=ALU.mult), r=[Y, gf], w=[Y])
                    cx.op("dve", lambda e: e.scalar_tensor_tensor(out=X[:], in0=X[:], scalar=ALPHA, in1=Y[:], op0=ALU.mult, op1=ALU.add),
                          r=[X, Y], w=[X])
                    tmp = tmps.next()
                    ln_stats(P, X, tmp)
                    cx.op("act", lambda e: e.activation(out=X[:], in_=X[:], func=AF.Identity, scale=tmp[2][:], bias=tmp[3][:]), r=[X, tmp[2], tmp[3]], w=[X])
                    cx.op("dve", lambda e: e.tensor_tensor(out=X[:], in0=X[:], in1=l2w[:], op=ALU.mult), r=[X, l2w], w=[X])
                    cx.op("dve", lambda e: e.tensor_tensor(out=X[:], in0=X[:], in1=l2b[:], op=ALU.add), r=[X, l2b], w=[X])
                    cx.dma("sp", out[row0:row0 + 128, :], X[:], r=[X])

        cx.barrier()
        PP.__exit__(None, None, None)
        build.ninstr = cx.ninstr
    return nc


def _consts():
    f32 = np.float32
    ident = np.eye(128, dtype=f32)
    perm = np.zeros((128, 128), f32)
    f = np.arange(128)
    j = f % 32
    partner = f - j + (j + 16) % 32
    perm[partner, f] = 1.0
    inv_freq = (10000.0 ** (-np.arange(16, dtype=np.float32) / 16)).astype(f32)
    tok = np.arange(S)
    rowp = (tok // 64).astype(f32)
    colp = (tok % 64).astype(f32)
    i64 = f % 64
    half = i64 // 32
    pos = np.where(half[:, None] == 0, rowp[None, :], colp[None, :]).astype(f32)
    ang = (pos * inv_freq[j % 16][:, None]).astype(f32)
    C = np.cos(ang).astype(f32)
    Sn = np.sin(ang).astype(f32)
    Sn = np.where((j < 16)[:, None], -Sn, Sn).astype(f32)
    return ident, perm, np.stack([C, Sn]).astype(f32)


def _nab(rel_bias):
    p = np.arange(128)
    dr = p // 64
    ck = p % 64
    cq = np.arange(64)
    cstart = np.clip(cq - 8, 0, 48)
    colok = (ck[:, None] >= cstart[None, :]) & (ck[:, None] < cstart[None, :] + 16)
    coff = np.clip(ck[:, None] - cq[None, :] + 15, 0, 30)
    out = np.full((NH, 128, 16, 64), NEG, np.float32)
    for idx in range(16):
        d = idx - 8 + dr
        rowok = np.abs(d) <= 7
        ok = colok & rowok[:, None]
        vals = rel_bias[:, np.clip(d + 7, 0, 14)[:, None], coff]
        out[:, :, idx, :] = np.where(ok[None], vals, NEG)
    return out


def _navalid(j):
    v = np.zeros((128, 16, 6), np.float32)
    p = np.arange(128)
    for i in range(16):
        lo, hi = min(i, 12), max(i + 8, 12)
        ms = list(range(lo // 2, (hi + 1) // 2))
        r = 16 * j + i
        r0 = min(max(r - 4, 0), 56)
        for s_, m in enumerate(ms):
            g = 16 * j - 4 + 2 * m + p // 64
            v[:, i, s_] = ((g >= r0) & (g < r0 + 8)).astype(np.float32)
    return v


def prep_inputs(inp, NE=NE_FULL):
    ident, perm, rope = _consts()
    f32 = np.float32
    A = lambda a: np.ascontiguousarray(a, dtype=f32)
    nab = _nab(np.asarray(inp["na_rel_bias"][0], f32))
    lamv = A(np.concatenate([inp["lam_q1"], inp["lam_k1"], inp["lam_q2"], inp["lam_k2"]], axis=0)).reshape(1, 256)
    lnp = A(np.concatenate([inp["ln1_w"], inp["ln1_b"], inp["ln2_w"], inp["ln2_b"]], axis=0))
    bgu = A(np.concatenate([inp["b_gate"][0].reshape(32, 16, 128).transpose(0, 2, 1),
                            inp["b_up"][0].reshape(32, 16, 128).transpose(0, 2, 1)], axis=2))[:NE]
    shared = dict(
        w_ada=A(inp["w_ada"][0]), b_ada=A(inp["b_ada"]), w_in=A(inp["w_in"][0]), nab=A(nab), lamv=lamv,
        wsub=A(inp["diff_subln_w"][0].reshape(128, 1)), w_proj_na=A(inp["w_proj_na"][0]), w_proj_diff=A(inp["w_proj_diff"][0]),
        w_out=A(inp["w_out"][0]), lnp=lnp, w_router=A(inp["w_router"][0]), b_router=A(inp["b_router"]),
        w_gate=A(inp["w_gate"][0][:NE]), w_up=A(inp["w_up"][0][:NE]), w_down=A(inp["w_down"][0][:NE]), bgu=bgu,
        b_down=A(inp["b_down"][0][:NE]), ropeK=A(rope), ident=ident, perm=perm,
    )
    maps = []
    for i in range(NCORE):
        b, j = i // 4, i % 4
        x = np.asarray(inp["x"][b], f32)
        xh = np.zeros((512, D), f32)
        top0 = 1024 * j - 256
        if top0 >= 0:
            xh[0:256] = x[top0:top0 + 256]
        bot0 = 1024 * j + 1024
        if bot0 + 256 <= S:
            xh[256:512] = x[bot0:bot0 + 256]
        cc = np.stack([np.asarray(inp["c"][b], f32).reshape(16, 128).T, np.asarray(inp["c_ctx"], f32).reshape(16, 128).T], axis=2)
        m = dict(shared)
        m.update(xb=A(x), xo=A(x[1024 * j:1024 * j + 1024]), xh=xh, ctx=A(inp["ctx"][b]), cc=A(cc),
                 navalid=_navalid(j), ropeQ=A(rope[:, :, 1024 * j:1024 * j + 1024]))
        maps.append(m)
    return maps


def kernel(**inputs):
    nc = build()
    maps = prep_inputs(inputs)
    res = run_bass_kernel_spmd(nc, maps, core_ids=list(range(NCORE)))
    outp = np.empty((2, S, D), np.float32)
    for i in range(NCORE):
        b, j = i // 4, i % 4
        outp[b, 1024 * j:1024 * j + 1024] = res.results[i]["out"]
    return outp
```

```python
import math
from contextlib import ExitStack
import numpy as np
import concourse.bass as bass
import concourse.mybir as mybir
from concourse.bass_utils import run_bass_kernel_spmd

F32 = mybir.dt.float32
BF16 = mybir.dt.bfloat16
AF = mybir.ActivationFunctionType
ALU = mybir.AluOpType
AX = mybir.AxisListType

D = 2048
S = 4096
NCORE = 8
TOK = 1024
CTX = 256
NH = 8
NE_FULL = 32
LN_EPS = 1e-6
RMS_EPS = 1e-5
ALPHA = 2.0 ** 0.25
LAM_INIT = 0.8 - 0.6 * math.exp(0.0)
NKEY_DF = S + CTX
NKEY_NA = 24 * 64 + CTX
NSLOT = 6
NEG = -30000.0


class Res:
    __slots__ = ("name", "w", "r", "psum")

    def __init__(self, name):
        self.name = name
        self.w = None
        self.r = {}
        self.psum = False


class Tile:
    def __init__(self, t, name):
        self.t = t
        self.res = Res(name)

    def __getitem__(self, k):
        return self.t[k]


class Eng:
    def __init__(self, name, h, is_pe=False):
        self.name = name
        self.h = h
        self.sem = "e_" + name
        self.cnt = 0
        self.seen = {}
        self.dk = 0
        self.dvals = [0] * NSLOT
        self.is_pe = is_pe


class Ctx:
    def __init__(self, nc, stack):
        self.nc = nc
        self.stack = stack
        self.E = {
            "pe": Eng("pe", nc.tensor, True),
            "act": Eng("act", nc.scalar),
            "dve": Eng("dve", nc.vector),
            "pool": Eng("pool", nc.gpsimd),
            "sp": Eng("sp", nc.sync),
        }
        self.sems = {}
        for e in self.E.values():
            self.sems[e.sem] = stack.enter_context(nc.semaphore(e.sem))
        for q in ("sp", "pool", "act"):
            for k in range(NSLOT):
                n = "d_%s%d" % (q, k)
                self.sems[n] = stack.enter_context(nc.semaphore(n))
        self.ninstr = 0
        import os
        self.limit = int(os.environ.get("K_LIMIT", "0"))
        self.trace = int(os.environ.get("K_TRACE", "0"))

    def skip(self):
        return self.limit and self.ninstr >= self.limit

    def _waits(self, E, reads, writes, extra=()):
        if self.skip():
            return
        waits = {}

        def need(tok):
            if tok is None:
                return
            sem, val = tok
            if E.is_pe and sem == E.sem:
                return
            if E.seen.get(sem, 0) >= val:
                return
            if waits.get(sem, 0) < val:
                waits[sem] = val

        for R in reads:
            need(R.w)
        for R in writes:
            need(R.w)
            for s_, v_ in R.r.items():
                need((s_, v_))
        for t in extra:
            need(t)
        for sem, val in waits.items():
            E.seen[sem] = val
            E.h.wait_ge(self.sems[sem], val)
            self.ninstr += 1

    @staticmethod
    def _res(xs):
        return [x.res if isinstance(x, Tile) else x for x in xs]

    def op(self, eng, fn, r=(), w=(), sig=True):
        E = self.E[eng]
        if self.skip():
            return None
        r = self._res(r)
        w = self._res(w)
        w = w + [R for R in r if R.psum and R not in w]
        r = [R for R in r if not R.psum]
        self._waits(E, r, w)
        inst = fn(E.h)
        self.ninstr += 1
        if self.trace:
            print("OP", self.ninstr, eng, fn.__code__.co_firstlineno)
        if sig:
            E.cnt += 1
            inst.then_inc(self.sems[E.sem], 1)
            tok = (E.sem, E.cnt)
        else:
            tok = (E.sem, E.cnt + 1)
        for R in r:
            if R.r.get(tok[0], 0) < tok[1]:
                R.r[tok[0]] = tok[1]
        for R in w:
            R.w = tok
            R.r = {}
        return inst

    def dma(self, q, out, in_, r=(), w=(), **kw):
        E = self.E[q]
        if self.skip():
            return None
        r = self._res(r)
        w = self._res(w)
        slot = E.dk % NSLOT
        E.dk += 1
        sem = "d_%s%d" % (q, slot)
        prev = E.dvals[slot]
        extra = [(sem, prev)] if prev > 0 else []
        self._waits(E, r, w, extra)
        inst = E.h.dma_start(out=out, in_=in_, **kw)
        self.ninstr += 1
        if self.trace:
            import sys as _s
            print("DMA", self.ninstr, q, _s._getframe(1).f_lineno)
        E.dvals[slot] = prev + 16
        inst.then_inc(self.sems[sem], 16)
        tok = (sem, prev + 16)
        for R in r:
            if R.r.get(tok[0], 0) < tok[1]:
                R.r[tok[0]] = tok[1]
        for R in w:
            R.w = tok
            R.r = {}
        return inst

    def barrier(self):
        lim, self.limit = self.limit, 0
        self._barrier()
        self.limit = lim

    def _barrier(self):
        toks = []
        for e in self.E.values():
            if e.cnt > 0:
                toks.append((e.sem, e.cnt))
            for k in range(NSLOT):
                if e.dvals[k] > 0:
                    toks.append(("d_%s%d" % (e.name, k), e.dvals[k]))
        for e in self.E.values():
            self._waits(e, [], [], toks)


class Phase:
    def __init__(self, cx):
        self.cx = cx
        self.st = ExitStack()
        self.n = 0

    def __enter__(self):
        self.st.__enter__()
        return self

    def __exit__(self, *a):
        self.cx.barrier()
        return self.st.__exit__(*a)

    def sb(self, shape, dt, name=None):
        self.n += 1
        name = (name or "t") + "_%d_%d" % (id(self) % 100000, self.n)
        return Tile(self.st.enter_context(self.cx.nc.sbuf_tensor(name, list(shape), dt)), name)

    def ps(self, shape, dt, name=None):
        self.n += 1
        name = (name or "p") + "_%d_%d" % (id(self) % 100000, self.n)
        t = Tile(self.st.enter_context(self.cx.nc.psum_tensor(name, list(shape), dt)), name)
        t.res.psum = True
        return t


class RR:
    def __init__(self, items):
        self.items = items
        self.i = 0

    def next(self):
        x = self.items[self.i % len(self.items)]
        self.i += 1
        return x


def build(NE=NE_FULL, dbg=False, phases=("mod", "proj", "na", "df", "merge", "out", "moe", "fin")):
    nc = bass.Bass("TRN2", target_bir_lowering=False)
    st = ExitStack()
    with st:
        cx = Ctx(nc, st)

        def din(name, shape, dt=F32):
            return nc.dram_tensor(name, list(shape), dt, kind="ExternalInput").ap()

        def dscr(name, shape, dt=BF16):
            return nc.dram_tensor(name, list(shape), dt, kind="ExternalOutput" if dbg else "Internal").ap()

        xb = din("xb", [S, D])
        xo = din("xo", [TOK, D])
        xh = din("xh", [512, D])
        ctxi = din("ctx", [CTX, D])
        cc = din("cc", [128, 16, 2])
        w_ada = din("w_ada", [D, 6 * D])
        b_ada = din("b_ada", [1, 6 * D])
        w_in = din("w_in", [D, 10240])
        nab = din("nab", [NH, 128, 16, 64])
        navalid = din("navalid", [128, 16, 6])
        lamv = din("lamv", [1, 256])
        wsub = din("wsub", [128, 1])
        w_pn = din("w_proj_na", [1024, D])
        w_pd = din("w_proj_diff", [1024, D])
        w_out = din("w_out", [D, D])
        lnp = din("lnp", [4, D])
        w_r = din("w_router", [D, 32])
        b_r = din("b_router", [1, 32])
        w_g = din("w_gate", [NE, D, D])
        w_u = din("w_up", [NE, D, D])
        w_d = din("w_down", [NE, D, D])
        bgu = din("bgu", [NE, 128, 32])
        b_d = din("b_down", [NE, D])
        ropeK = din("ropeK", [2, 128, S])
        ropeQ = din("ropeQ", [2, 128, TOK])
        ident_in = din("ident", [128, 128])
        perm_in = din("perm", [128, 128])
        out = nc.dram_tensor("out", [TOK, D], F32, kind="ExternalOutput").ap()

        modv = dscr("modv", [2, 6 * D], F32)
        kdfT = dscr("kdfT", [NH, 128, NKEY_DF])
        vdf = dscr("vdf", [NKEY_DF, 1024])
        knaT = dscr("knaT", [NH, 128, NKEY_NA])
        vna = dscr("vna", [NKEY_NA, 1024])
        qnaT = dscr("qnaT", [NH, 128, TOK])
        qdfT = dscr("qdfT", [NH, 128, TOK])
        gnaT = dscr("gnaT", [16, 128, TOK])
        gdfT = dscr("gdfT", [16, 128, TOK])
        onaT = dscr("onaT", [NH, 128, TOK])
        odfT = dscr("odfT", [NH, 128, TOK])
        mrgT = dscr("mrgT", [16, 128, TOK])
        x1s = dscr("x1s", [TOK, D], F32)
        u2Ts = dscr("u2Ts", [16, 128, TOK])
        Gs = dscr("Gs", [TOK, 32], F32)
        y2s = dscr("y2s", [TOK, D], F32)

        PP = Phase(cx)
        PP.__enter__()
        ident_f = PP.sb([128, 128], F32, "identf")
        ident_b = PP.sb([128, 128], BF16, "identb")
        perm_b = PP.sb([128, 128], BF16, "permb")
        ones_b = PP.sb([128, 128], BF16, "onesb")
        ones_f = PP.sb([128, 128], F32, "onesf")
        cx.dma("sp", ident_f[:], ident_in[:, :], w=[ident_f])
        cx.dma("pool", ident_b[:], ident_in[:, :], w=[ident_b])
        cx.dma("pool", perm_b[:], perm_in[:, :], w=[perm_b])
        cx.op("dve", lambda e: e.memset(ones_b[:], 1.0), w=[ones_b])
        cx.op("dve", lambda e: e.memset(ones_f[:], 1.0), w=[ones_f])
        eps_ln = PP.sb([128, 1], F32, "epsln")
        eps_rms = PP.sb([128, 1], F32, "epsrms")
        cx.op("dve", lambda e: e.memset(eps_ln[:], LN_EPS), w=[eps_ln])
        cx.op("dve", lambda e: e.memset(eps_rms[:], RMS_EPS), w=[eps_rms])

        w_in_v = w_in.rearrange("(kc p) n -> p kc n", p=128)

        if "mod" in phases:
            with Phase(cx) as P:
                ccs = P.sb([128, 32], F32, "ccs")
                sg = P.sb([128, 32], F32, "sg")
                sil = P.sb([128, 16, 2], BF16, "sil")
                bsb = P.sb([2, 6 * D], F32, "bsb")
                msb = P.sb([2, 6 * D], F32, "msb")
                wt = [P.sb([128, 16, 512], BF16, "wada") for _ in range(3)]
                pb = [P.ps([128, 512], F32, "pm") for _ in range(2)]
                cx.dma("sp", ccs[:], cc.rearrange("p k t -> p (k t)"), w=[ccs])
                cx.dma("sp", bsb[0:1, :], b_ada[:, :], w=[bsb])
                cx.dma("sp", bsb[1:2, :], b_ada[:, :], w=[bsb])
                cx.op("act", lambda e: e.activation(out=sg[:], in_=ccs[:], func=AF.Sigmoid), r=[ccs], w=[sg])
                cx.op("dve", lambda e: e.tensor_tensor(out=sil[:].rearrange("p k t -> p (k t)"), in0=ccs[:], in1=sg[:], op=ALU.mult),
                      r=[ccs, sg], w=[sil])
                w_ada_v = w_ada.rearrange("(kc p) n -> p kc n", p=128)
                for n in range(24):
                    W = wt[n % 3]
                    cx.dma("pool", W[:], w_ada_v[:, :, n * 512:(n + 1) * 512], w=[W])
                    pp = pb[n % 2]
                    for kc in range(16):
                        cx.op("pe", lambda e, kc=kc: e.matmul(pp[0:2, :], sil[:, kc, :], W[:, kc, :], start=(kc == 0), stop=(kc == 15)),
                              r=[sil, W], w=[pp], sig=(kc == 15))
                    cx.op("dve", lambda e: e.tensor_tensor(out=msb[0:2, n * 512:(n + 1) * 512], in0=pp[0:2, :],
                                                          in1=bsb[0:2, n * 512:(n + 1) * 512], op=ALU.add),
                          r=[pp, bsb], w=[msb])
                cx.dma("sp", modv[:, :], msb[0:2, :], r=[msb])

        def mod_bc(row, idx):
            return modv[row:row + 1, idx * D:(idx + 1) * D].partition_broadcast(128)

        def load_bc(P, src_ap, name, plus1=False):
            t = P.sb([128, D], F32, name)
            cx.dma("sp", t[:], src_ap, w=[t])
            if plus1:
                cx.op("dve", lambda e: e.tensor_scalar(out=t[:], in0=t[:], scalar1=1.0, scalar2=None, op0=ALU.add), r=[t], w=[t])
            return t

        def ln_stats(P, src, tmp):
            stt, mv, rstd, nmr = tmp
            for c in range(4):
                cx.op("dve", lambda e, c=c: e.bn_stats(out=stt[:, c, :], in_=src[:, c * 512:(c + 1) * 512]), r=[src], w=[stt])
            cx.op("dve", lambda e: e.bn_aggr(out=mv[:], in_=stt[:].rearrange("p a b -> p (a b)")), r=[stt], w=[mv])
            cx.op("act", lambda e: e.activation(out=rstd[:], in_=mv[:, 1:2], func=AF.Sqrt, bias=eps_ln[:], scale=1.0), r=[mv, eps_ln], w=[rstd])
            cx.op("dve", lambda e: e.reciprocal(out=rstd[:], in_=rstd[:]), r=[rstd], w=[rstd])
            cx.op("dve", lambda e: e.tensor_scalar(out=nmr[:], in0=mv[:, 0:1], scalar1=rstd[:], scalar2=-1.0, op0=ALU.mult, op1=ALU.mult),
                  r=[mv, rstd], w=[nmr])

        def ln_tmp(P):
            return (P.sb([128, 4, 6], F32, "stt"), P.sb([128, 2], F32, "mv"), P.sb([128, 1], F32, "rstd"), P.sb([128, 1], F32, "nmr"))

        if "proj" in phases:
            with Phase(cx) as P:
                sc1_m = load_bc(P, mod_bc(0, 1), "sc1m", True)
                sh_m = load_bc(P, mod_bc(0, 0), "shm")
                sc1_c = load_bc(P, mod_bc(1, 1), "sc1c", True)
                sh_c = load_bc(P, mod_bc(1, 0), "shc")
                xt = [P.sb([128, D], F32, "xt") for _ in range(2)]
                zt = [P.sb([128, D], F32, "zt") for _ in range(2)]
                ut = [P.sb([128, D], BF16, "ut") for _ in range(2)]
                tmps = [ln_tmp(P) for _ in range(2)]
                uT = [P.sb([128, 16, 512], BF16, "uT") for _ in range(2)]
                wts = RR([P.sb([128, 16, 512], BF16, "win") for _ in range(3)])
                ptr = RR([P.ps([128, 1024], BF16, "ptr") for _ in range(2)])
                pmm = RR([P.ps([128, 512], F32, "pmm") for _ in range(4)])
                prp = pmm
                osb = RR([P.sb([128, 512], BF16, "osb") for _ in range(4)])
                xsb = RR([P.sb([128, 512], BF16, "xsb") for _ in range(2)])
                t1s = RR([P.sb([128, 512], F32, "t1s") for _ in range(2)])
                rC = RR([P.sb([128, 512], F32, "rC") for _ in range(2)])
                rS = RR([P.sb([128, 512], F32, "rS") for _ in range(2)])
                tcount = [0]

                def ln_gen(src_rows, ntok, sc1, sh, U):
                    for t in range(ntok // 128):
                        k = tcount[0] % 2
                        tcount[0] += 1
                        X, Z, Ub, tmp = xt[k], zt[k], ut[k], tmps[k]
                        cx.dma("sp", X[:], src_rows[t * 128:(t + 1) * 128, :], w=[X])
                        ln_stats(P, X, tmp)
                        cx.op("act", lambda e: e.activation(out=Z[:], in_=X[:], func=AF.Identity, scale=tmp[2][:], bias=tmp[3][:]),
                              r=[X, tmp[2], tmp[3]], w=[Z])
                        cx.op("dve", lambda e: e.tensor_tensor(out=Z[:], in0=Z[:], in1=sc1[:], op=ALU.mult), r=[Z, sc1], w=[Z])
                        cx.op("dve", lambda e: e.tensor_tensor(out=Ub[:], in0=Z[:], in1=sh[:], op=ALU.add), r=[Z, sh], w=[Ub])
                        yield 1
                        for g in range(2):
                            pt = ptr.next()
                            for q in range(8):
                                kc = g * 8 + q
                                cx.op("pe", lambda e, kc=kc, q=q: e.transpose(out=pt[:, q * 128:(q + 1) * 128], in_=Ub[:, kc * 128:(kc + 1) * 128],
                                                                            identity=ident_b[:]),
                                      r=[Ub, ident_b], w=[pt], sig=(q == 7))
                            cx.op("act", lambda e, g=g: e.copy(out=U[:, g * 8:(g + 1) * 8, t * 128:(t + 1) * 128],
                                                              in_=pt[:].rearrange("p (q n) -> p q n", q=8)),
                                  r=[pt], w=[U])
                        yield 2

                hk = [None]

                def step():
                    if hk[0] is not None:
                        if next(hk[0], None) is None:
                            hk[0] = None

                def drain():
                    while hk[0] is not None:
                        step()

                def proj(U, ntok, col0, ncols, mode, dest, rope=None):
                    for cb in range(ncols // 512):
                        W = wts.next()
                        c0 = col0 + cb * 512
                        cx.dma("pool", W[:], w_in_v[:, :, c0:c0 + 512], w=[W])
                        step()
                        if mode == "FM":
                            for sub in range(4):
                                pm = pmm.next()
                                for kc in range(16):
                                    cx.op("pe", lambda e, kc=kc: e.matmul(pm[:, 0:ntok], W[:, kc, sub * 128:(sub + 1) * 128], U[:, kc, 0:ntok],
                                                                         start=(kc == 0), stop=(kc == 15)),
                                          r=[W, U], w=[pm], sig=(kc == 15))
                                ci = cb * 4 + sub
                                if rope is None:
                                    O = osb.next()
                                    cx.op("act", lambda e: e.copy(out=O[:, 0:ntok], in_=pm[:, 0:ntok]), r=[pm], w=[O])
                                    dest(ci, O)
                                else:
                                    Cc, Ss = rope
                                    Xs = xsb.next()
                                    cx.op("act", lambda e: e.copy(out=Xs[:, 0:ntok], in_=pm[:, 0:ntok]), r=[pm], w=[Xs])
                                    pr = prp.next()
                                    cx.op("pe", lambda e: e.matmul(pr[:, 0:ntok], perm_b[:], Xs[:, 0:ntok], start=True, stop=True),
                                          r=[perm_b, Xs], w=[pr])
                                    T1 = t1s.next()
                                    cx.op("dve", lambda e: e.tensor_tensor(out=T1[:, 0:ntok], in0=Xs[:, 0:ntok], in1=Cc[:, 0:ntok], op=ALU.mult),
                                          r=[Xs, Cc], w=[T1])
                                    T2 = t1s.next()
                                    cx.op("dve", lambda e: e.tensor_tensor(out=T2[:, 0:ntok], in0=pr[:, 0:ntok], in1=Ss[:, 0:ntok], op=ALU.mult),
                                          r=[pr, Ss], w=[T2])
                                    O = osb.next()
                                    cx.op("dve", lambda e: e.tensor_tensor(out=O[:, 0:ntok], in0=T1[:, 0:ntok], in1=T2[:, 0:ntok], op=ALU.add),
                                          r=[T1, T2], w=[O])
                                    dest(ci, O)
                        else:
                            for t in range(ntok // 128):
                                pm = pmm.next()
                                for kc in range(16):
                                    cx.op("pe", lambda e, kc=kc: e.matmul(pm[:, :], U[:, kc, t * 128:(t + 1) * 128], W[:, kc, :],
                                                                         start=(kc == 0), stop=(kc == 15)),
                                          r=[W, U], w=[pm], sig=(kc == 15))
                                O = osb.next()
                                cx.op("act", lambda e: e.copy(out=O[:], in_=pm[:]), r=[pm], w=[O])
                                dest(t, cb, O)
                        step()

                def st_fm(dst, tok0, ntok):
                    return lambda ci, O: cx.dma("sp", dst[ci, :, tok0:tok0 + ntok], O[:, 0:ntok], r=[O])

                def st_tm(dst, row0):
                    return lambda t, cb, O: cx.dma("sp", dst[row0 + t * 128:row0 + (t + 1) * 128, cb * 512:(cb + 1) * 512], O[:], r=[O])

                ub = [0]

                def nextU():
                    ub[0] += 1
                    return uT[ub[0] % 2]

                def st_halo_fm(ci, O):
                    cx.dma("sp", knaT[ci, :, 0:256], O[:, 0:256], r=[O])
                    cx.dma("sp", knaT[ci, :, 1280:1536], O[:, 256:512], r=[O])

                def st_halo_tm(t, cb, O):
                    row0 = t * 128 if t < 2 else 1280 + (t - 2) * 128
                    cx.dma("sp", vna[row0:row0 + 128, cb * 512:(cb + 1) * 512], O[:], r=[O])

                blocks = []

                def s1_work(blk):
                    def f(U):
                        Cc, Ss = rC.next(), rS.next()
                        cx.dma("sp", Cc[:], ropeK[0, :, blk * 512:(blk + 1) * 512], w=[Cc])
                        cx.dma("sp", Ss[:], ropeK[1, :, blk * 512:(blk + 1) * 512], w=[Ss])
                        proj(U, 512, 2048, 1024, "FM", st_fm(kdfT, blk * 512, 512), rope=(Cc, Ss))
                        proj(U, 512, 3072, 1024, "TM", st_tm(vdf, blk * 512))
                    return f

                def s2_work(U):
                    proj(U, CTX, 0, 1024, "FM", st_fm(knaT, 1536, CTX))
                    proj(U, CTX, 1024, 1024, "TM", st_tm(vna, 1536))
                    proj(U, CTX, 2048, 1024, "FM", st_fm(kdfT, S, CTX))
                    proj(U, CTX, 3072, 1024, "TM", st_tm(vdf, S))

                def s3_work(U):
                    proj(U, 512, 0, 1024, "FM", st_halo_fm)
                    proj(U, 512, 1024, 1024, "TM", st_halo_tm)

                def s4_work(blk):
                    def f(U):
                        Cc, Ss = rC.next(), rS.next()
                        cx.dma("sp", Cc[:], ropeQ[0, :, blk * 512:(blk + 1) * 512], w=[Cc])
                        cx.dma("sp", Ss[:], ropeQ[1, :, blk * 512:(blk + 1) * 512], w=[Ss])
                        proj(U, 512, 0, 1024, "FM", st_fm(knaT, 256 + blk * 512, 512))
                        proj(U, 512, 1024, 1024, "TM", st_tm(vna, 256 + blk * 512))
                        proj(U, 512, 4096, 1024, "FM", st_fm(qnaT, blk * 512, 512))
                        proj(U, 512, 5120, 1024, "FM", st_fm(qdfT, blk * 512, 512), rope=(Cc, Ss))
                        proj(U, 512, 6144, 2048, "FM", st_fm(gnaT, blk * 512, 512))
                        proj(U, 512, 8192, 2048, "FM", st_fm(gdfT, blk * 512, 512))
                    return f

                for blk in range(S // 512):
                    blocks.append((xb[blk * 512:(blk + 1) * 512, :], 512, sc1_m, sh_m, s1_work(blk)))
                blocks.append((ctxi, CTX, sc1_c, sh_c, s2_work))
                blocks.append((xh, 512, sc1_m, sh_m, s3_work))
                for blk in range(2):
                    blocks.append((xo[blk * 512:(blk + 1) * 512, :], 512, sc1_m, sh_m, s4_work(blk)))
                hk[0] = ln_gen(blocks[0][0], blocks[0][1], blocks[0][2], blocks[0][3], uT[0])
                drain()
                for bi, (src, ntok, sc1, sh, work) in enumerate(blocks):
                    if bi + 1 < len(blocks):
                        nb_ = blocks[bi + 1]
                        hk[0] = ln_gen(nb_[0], nb_[1], nb_[2], nb_[3], uT[(bi + 1) % 2])
                    work(uT[bi % 2])
                    drain()

        if "na" in phases:
            with Phase(cx) as P:
                val = P.sb([128, 16, 6], F32, "val")
                cx.dma("sp", val[:], navalid[:, :, :], w=[val])
                kT = [P.sb([128, NKEY_NA], BF16, "kT") for _ in range(2)]
                vv = [P.sb([128, 14, 128], BF16, "vv") for _ in range(2)]
                qT = [P.sb([128, TOK], BF16, "qT") for _ in range(2)]
                nbf = [P.sb([128, 16, 64], F32, "nbf") for _ in range(2)]
                EB = [P.sb([128, 16, 64], BF16, "EB") for _ in range(2)]
                oT = [P.sb([128, TOK], BF16, "oT") for _ in range(2)]
                pS = RR([P.ps([128, 512], F32, "pS") for _ in range(2)])
                pO = RR([P.ps([128, 512], F32, "pO") for _ in range(2)])
                pZ = RR([P.ps([128, 512], F32, "pZ") for _ in range(2)])
                esb = RR([P.sb([128, 512], BF16, "esb") for _ in range(3)])
                e2 = RR([P.sb([128, 512], BF16, "e2") for _ in range(3)])
                rsb = RR([P.sb([128, 64], F32, "rsb") for _ in range(2)])
                scale = 128 ** -0.5
                def na_load(h):
                    K_, V_, Q_, NB_, EB_, O_ = kT[h % 2], vv[h % 2], qT[h % 2], nbf[h % 2], EB[h % 2], oT[h % 2]
                    cx.dma("sp", K_[:], knaT[h, :, :], w=[K_])
                    cx.dma("sp", V_[:], vna.rearrange("(c p) d -> p c d", p=128)[:, :, h * 128:(h + 1) * 128], w=[V_])
                    cx.dma("sp", Q_[:], qnaT[h, :, :], w=[Q_])
                    cx.dma("sp", NB_[:], nab[h, :, :, :], w=[NB_])
                    cx.op("act", lambda e: e.activation(out=EB_[:], in_=NB_[:], func=AF.Exp), r=[NB_], w=[EB_])

                def na_A(h, i):
                    K_, V_, Q_, NB_, EB_, O_ = kT[h % 2], vv[h % 2], qT[h % 2], nbf[h % 2], EB[h % 2], oT[h % 2]
                    lo, hi = min(i, 12), max(i + 8, 12)
                    ms = list(range(lo // 2, (hi + 1) // 2))
                    nl = len(ms)
                    chunks = ms + [12, 13]
                    ps = pS.next()
                    q = Q_[:, i * 64:(i + 1) * 64]
                    for ci, m in enumerate(chunks):
                        cx.op("pe", lambda e, ci=ci, m=m: e.matmul(ps[:, ci * 64:(ci + 1) * 64], K_[:, m * 128:(m + 1) * 128], q,
                                                                   start=True, stop=True),
                              r=[K_, Q_], w=[ps], sig=(ci == len(chunks) - 1))
                    nc_ = len(chunks)
                    E1 = esb.next()
                    cx.op("act", lambda e: e.activation(out=E1[:, 0:nc_ * 64], in_=ps[:, 0:nc_ * 64], func=AF.Exp, scale=scale), r=[ps], w=[E1])
                    idx0 = 2 * ms[0] - i - 4 + 8
                    E2 = e2.next()
                    cx.op("dve", lambda e: e.tensor_tensor(out=E2[:, 0:nl * 64].rearrange("p (c n) -> p c n", n=64),
                                                          in0=E1[:, 0:nl * 64].rearrange("p (c n) -> p c n", n=64),
                                                          in1=EB_[:, idx0:idx0 + 2 * nl - 1:2, :], op=ALU.mult),
                          r=[E1, EB_], w=[E2])
                    cx.op("dve", lambda e: e.tensor_tensor(out=E1[:, 0:nl * 64].rearrange("p (c n) -> p c n", n=64),
                                                          in0=E2[:, 0:nl * 64].rearrange("p (c n) -> p c n", n=64),
                                                          in1=val[:, i, 0:nl].unsqueeze(2).to_broadcast([128, nl, 64]), op=ALU.mult),
                          r=[E2, val], w=[E1])
                    return (chunks, E1)

                def na_B(h, i, st_):
                    K_, V_, Q_, NB_, EB_, O_ = kT[h % 2], vv[h % 2], qT[h % 2], nbf[h % 2], EB[h % 2], oT[h % 2]
                    chunks, E1 = st_
                    po, pz = pO.next(), pZ.next()
                    for ci, m in enumerate(chunks):
                        last = ci == len(chunks) - 1
                        cx.op("pe", lambda e, ci=ci, m=m: e.matmul(po[:, 0:64], V_[:, m, :], E1[:, ci * 64:(ci + 1) * 64],
                                                                   start=(ci == 0), stop=last), r=[V_, E1], w=[po], sig=False)
                        cx.op("pe", lambda e, ci=ci: e.matmul(pz[:, 0:64], ones_b[:], E1[:, ci * 64:(ci + 1) * 64],
                                                              start=(ci == 0), stop=last), r=[ones_b, E1], w=[pz], sig=last)
                    R_ = rsb.next()
                    cx.op("dve", lambda e: e.reciprocal(out=R_[:], in_=pz[:, 0:64]), r=[pz, po], w=[R_])
                    cx.op("dve", lambda e: e.tensor_tensor(out=O_[:, i * 64:(i + 1) * 64], in0=po[:, 0:64], in1=R_[:], op=ALU.mult),
                          r=[po, R_], w=[O_])
                    if i == 15:
                        cx.dma("sp", onaT[h, :, :], O_[:], r=[O_])

                its = [(h, i) for h in range(NH) for i in range(16)]
                pend = []
                for n_, (h, i) in enumerate(its):
                    if i == 0:
                        na_load(h)
                    pend.append((h, i, na_A(h, i)))
                    if len(pend) > 1:
                        na_B(*pend.pop(0))
                while pend:
                    na_B(*pend.pop(0))

        if "df" in phases:
            with Phase(cx) as P:
                lv = P.sb([128, 4, 64], F32, "lv")
                lp = P.sb([128, 2, 64], F32, "lp")
                ls = P.sb([128, 2], F32, "ls")
                le = P.sb([128, 2], F32, "le")
                nlam = P.sb([128, 1], F32, "nlam")
                wsc = P.sb([128, 1], F32, "wsc")
                cx.dma("sp", lv[:].rearrange("p a d -> p (a d)"), lamv[0:1, :].partition_broadcast(128), w=[lv])
                cx.dma("sp", wsc[:], wsub[:, :], w=[wsc])
                cx.op("dve", lambda e: e.tensor_tensor(out=lp[:, 0, :], in0=lv[:, 0, :], in1=lv[:, 1, :], op=ALU.mult), r=[lv], w=[lp])
                cx.op("dve", lambda e: e.tensor_tensor(out=lp[:, 1, :], in0=lv[:, 2, :], in1=lv[:, 3, :], op=ALU.mult), r=[lv], w=[lp])
                cx.op("dve", lambda e: e.reduce_sum(out=ls[:], in_=lp[:], axis=AX.X), r=[lp], w=[ls])
                cx.op("act", lambda e: e.activation(out=le[:], in_=ls[:], func=AF.Exp), r=[ls], w=[le])
                cx.op("dve", lambda e: e.tensor_scalar(out=nlam[:], in0=le[:, 1:2], scalar1=le[:, 0:1], scalar2=-LAM_INIT, op0=ALU.subtract, op1=ALU.add),
                      r=[le], w=[nlam])
                kT = [P.sb([128, NKEY_DF], BF16, "kT") for _ in range(2)]
                vv = [P.sb([128, 34, 128], BF16, "vv") for _ in range(2)]
                qT = [P.sb([128, TOK], BF16, "qT") for _ in range(2)]
                oT = [P.sb([128, TOK], BF16, "oT") for _ in range(2)]
                pS = RR([P.ps([128, 512], F32, "pS") for _ in range(3)])
                pO = [P.ps([128, 512], F32, "pO") for _ in range(2)]
                pZ = [P.ps([128, 512], F32, "pZ") for _ in range(2)]
                pS2 = P.ps([128, 512], F32, "pS2")
                esb = RR([P.sb([128, 512], BF16, "esb") for _ in range(4)])
                r0 = P.sb([128, 512], F32, "r0")
                r1 = P.sb([128, 512], F32, "r1")
                t0 = P.sb([128, 512], F32, "t0")
                t1 = P.sb([128, 512], F32, "t1")
                of = P.sb([128, 512], F32, "of")
                sq = P.sb([128, 512], F32, "sq")
                rs = P.sb([128, 512], F32, "rs")
                NKC = NKEY_DF // 128

                def df_load(h):
                    K_, V_, Q_, O_ = kT[h % 2], vv[h % 2], qT[h % 2], oT[h % 2]
                    cx.dma("sp", K_[:], kdfT[h, :, :], w=[K_])
                    cx.dma("sp", V_[:], vdf.rearrange("(c p) d -> p c d", p=128)[:, :, h * 128:(h + 1) * 128], w=[V_])
                    cx.dma("sp", Q_[:], qdfT[h, :, :], w=[Q_])

                def df_A(h, qb, kc, m):
                    K_, V_, Q_, O_ = kT[h % 2], vv[h % 2], qT[h % 2], oT[h % 2]
                    ps = pS.next()
                    cx.op("pe", lambda e: e.matmul(ps[:, :], K_[m * 64:(m + 1) * 64, kc * 128:(kc + 1) * 128],
                                                   Q_[m * 64:(m + 1) * 64, qb * 512:(qb + 1) * 512], start=True, stop=True),
                          r=[K_, Q_], w=[ps])
                    E1 = esb.next()
                    cx.op("act", lambda e: e.activation(out=E1[:], in_=ps[:], func=AF.Exp, scale=0.125), r=[ps], w=[E1])
                    return E1

                def df_B(h, qb, kc, m, E1):
                    K_, V_, Q_, O_ = kT[h % 2], vv[h % 2], qT[h % 2], oT[h % 2]
                    last = kc == NKC - 1
                    cx.op("pe", lambda e: e.matmul(pO[m][:, :], V_[:, kc, :], E1[:], start=(kc == 0), stop=last),
                          r=[V_, E1], w=[pO[m]], sig=False)
                    cx.op("pe", lambda e: e.matmul(pZ[m][:, :], ones_b[:], E1[:], start=(kc == 0), stop=last),
                          r=[ones_b, E1], w=[pZ[m]], sig=last)
                    if not (last and m == 1):
                        return
                    cx.op("dve", lambda e: e.reciprocal(out=r0[:], in_=pZ[0][:]), r=[pZ[0]], w=[r0])
                    cx.op("dve", lambda e: e.reciprocal(out=r1[:], in_=pZ[1][:]), r=[pZ[1]], w=[r1])
                    cx.op("dve", lambda e: e.tensor_scalar(out=r1[:], in0=r1[:], scalar1=nlam[:], scalar2=None, op0=ALU.mult), r=[r1, nlam], w=[r1])
                    cx.op("dve", lambda e: e.tensor_tensor(out=t0[:], in0=pO[0][:], in1=r0[:], op=ALU.mult), r=[pO[0], r0], w=[t0])
                    cx.op("dve", lambda e: e.tensor_tensor(out=t1[:], in0=pO[1][:], in1=r1[:], op=ALU.mult), r=[pO[1], r1], w=[t1])
                    cx.op("dve", lambda e: e.tensor_tensor(out=of[:], in0=t0[:], in1=t1[:], op=ALU.add), r=[t0, t1], w=[of])
                    cx.op("act", lambda e: e.activation(out=sq[:], in_=of[:], func=AF.Square), r=[of], w=[sq])
                    ps = pS2
                    cx.op("pe", lambda e: e.matmul(ps[:, :], ones_f[:], sq[:], start=True, stop=True), r=[ones_f, sq], w=[ps])
                    cx.op("act", lambda e: e.activation(out=rs[:], in_=ps[:], func=AF.Sqrt, bias=eps_rms[:], scale=1.0 / 128), r=[ps, eps_rms], w=[rs])
                    cx.op("dve", lambda e: e.reciprocal(out=rs[:], in_=rs[:]), r=[rs], w=[rs])
                    cx.op("dve", lambda e: e.tensor_tensor(out=of[:], in0=of[:], in1=rs[:], op=ALU.mult), r=[of, rs], w=[of])
                    cx.op("dve", lambda e: e.tensor_scalar(out=O_[:, qb * 512:(qb + 1) * 512], in0=of[:], scalar1=wsc[:], scalar2=1.0 - LAM_INIT,
                                                          op0=ALU.mult, op1=ALU.mult), r=[of, wsc], w=[O_])
                    if qb == 1:
                        cx.dma("sp", odfT[h, :, :], O_[:], r=[O_])

                its = [(h, qb, kc, m) for h in range(NH) for qb in range(2) for kc in range(NKC) for m in range(2)]
                pend = []
                for (h, qb, kc, m) in its:
                    if qb == 0 and kc == 0 and m == 0:
                        df_load(h)
                    pend.append((h, qb, kc, m, df_A(h, qb, kc, m)))
                    if len(pend) > 2:
                        df_B(*pend.pop(0))
                while pend:
                    df_B(*pend.pop(0))

        if "merge" in phases:
            with Phase(cx) as P:
                wpn = P.sb([128, 8, D], BF16, "wpn")
                wpd = P.sb([128, 8, D], BF16, "wpd")
                on = P.sb([128, 8, TOK], BF16, "on")
                od = P.sb([128, 8, TOK], BF16, "od")
                cx.dma("pool", wpn[:], w_pn.rearrange("(h p) n -> p h n", p=128), w=[wpn])
                cx.dma("pool", wpd[:], w_pd.rearrange("(h p) n -> p h n", p=128), w=[wpd])
                cx.dma("sp", on[:], onaT.rearrange("h p t -> p h t"), w=[on])
                cx.dma("sp", od[:], odfT.rearrange("h p t -> p h t"), w=[od])
                gn = RR([P.sb([128, TOK], BF16, "gn") for _ in range(2)])
                gd = RR([P.sb([128, TOK], BF16, "gd") for _ in range(2)])
                pA = RR([P.ps([128, 512], F32, "pA") for _ in range(2)])
                pD = RR([P.ps([128, 512], F32, "pD") for _ in range(2)])
                ta = RR([P.sb([128, 512], F32, "ta") for _ in range(2)])
                tb = RR([P.sb([128, 512], F32, "tb") for _ in range(2)])
                mo = RR([P.sb([128, TOK], BF16, "mo") for _ in range(2)])
                for fc in range(16):
                    Gn, Gd, Mo = gn.next(), gd.next(), mo.next()
                    cx.dma("sp", Gn[:], gnaT[fc, :, :], w=[Gn])
                    cx.dma("sp", Gd[:], gdfT[fc, :, :], w=[Gd])
                    cx.op("act", lambda e: e.activation(out=Gn[:], in_=Gn[:], func=AF.Sigmoid), r=[Gn], w=[Gn])
                    cx.op("act", lambda e: e.activation(out=Gd[:], in_=Gd[:], func=AF.Sigmoid), r=[Gd], w=[Gd])
                    for hf in range(2):
                        pa, pd = pA.next(), pD.next()
                        for h in range(8):
                            cx.op("pe", lambda e, h=h: e.matmul(pa[:, :], wpn[:, h, fc * 128:(fc + 1) * 128], on[:, h, hf * 512:(hf + 1) * 512],
                                                                start=(h == 0), stop=(h == 7)), r=[wpn, on], w=[pa], sig=(h == 7))
                        for h in range(8):
                            cx.op("pe", lambda e, h=h: e.matmul(pd[:, :], wpd[:, h, fc * 128:(fc + 1) * 128], od[:, h, hf * 512:(hf + 1) * 512],
                                                                start=(h == 0), stop=(h == 7)), r=[wpd, od], w=[pd], sig=(h == 7))
                        Ta, Tb = ta.next(), tb.next()
                        cx.op("dve", lambda e: e.tensor_tensor(out=Ta[:], in0=pa[:], in1=Gn[:, hf * 512:(hf + 1) * 512], op=ALU.mult), r=[pa, Gn], w=[Ta])
                        cx.op("dve", lambda e: e.tensor_tensor(out=Tb[:], in0=pd[:], in1=Gd[:, hf * 512:(hf + 1) * 512], op=ALU.mult), r=[pd, Gd], w=[Tb])
                        cx.op("dve", lambda e: e.tensor_tensor(out=Mo[:, hf * 512:(hf + 1) * 512], in0=Ta[:], in1=Tb[:], op=ALU.add), r=[Ta, Tb], w=[Mo])
                    cx.dma("sp", mrgT[fc, :, :], Mo[:], r=[Mo])

        if "out" in phases:
            with Phase(cx) as P:
                wo = P.sb([128, 16, D], BF16, "wo")
                cx.dma("pool", wo[:], w_out.rearrange("(kc p) n -> p kc n", p=128), w=[wo])
                gm = load_bc(P, mod_bc(0, 2), "gm")
                l1w = load_bc(P, lnp[0:1, :].partition_broadcast(128), "l1w")
                l1b = load_bc(P, lnp[1:2, :].partition_broadcast(128), "l1b")
                sc1f = load_bc(P, mod_bc(0, 4), "sc1f", True)
                shf = load_bc(P, mod_bc(0, 3), "shf")
                wr = P.sb([128, 16, 32], F32, "wr")
                cx.dma("sp", wr[:], w_r.rearrange("(kc p) e -> p kc e", p=128), w=[wr])
                brb = P.sb([128, 32], F32, "brb")
                cx.dma("sp", brb[:], b_r[0:1, :].partition_broadcast(128), w=[brb])
                mT = RR([P.sb([128, 16, 128], BF16, "mT") for _ in range(2)])
                xt = RR([P.sb([128, D], F32, "xt") for _ in range(2)])
                vt = RR([P.sb([128, D], F32, "vt") for _ in range(1)])
                ut = RR([P.sb([128, D], F32, "ut") for _ in range(1)])
                tmps = RR([ln_tmp(P) for _ in range(2)])
                py = RR([P.ps([128, 512], F32, "py") for _ in range(4)])
                ptr = RR([P.ps([128, 512], F32, "ptr") for _ in range(2)])
                plg = RR([P.ps([128, 512], F32, "plg") for _ in range(2)])
                uTf = RR([P.sb([128, 16, 128], F32, "uTf") for _ in range(2)])
                uTb = RR([P.sb([128, 16, 128], BF16, "uTb") for _ in range(2)])
                lg = RR([P.sb([128, 32], F32, "lg") for _ in range(2)])
                m8 = RR([P.sb([128, 8], F32, "m8") for _ in range(2)])
                nmx = RR([P.sb([128, 1], F32, "nmx") for _ in range(2)])
                msk = RR([P.sb([128, 32], F32, "msk") for _ in range(2)])
                ex = RR([P.sb([128, 32], F32, "ex") for _ in range(2)])
                sm = RR([P.sb([128, 1], F32, "sm") for _ in range(2)])
                gt = RR([P.sb([128, 32], F32, "gt") for _ in range(2)])
                for t in range(8):
                    M_ = mT.next()
                    cx.dma("sp", M_[:], mrgT.rearrange("k p t -> p k t")[:, :, t * 128:(t + 1) * 128], w=[M_])
                    X = xt.next()
                    cx.dma("sp", X[:], xo[t * 128:(t + 1) * 128, :], w=[X])
                    V = vt.next()
                    for cb in range(4):
                        pp = py.next()
                        for kc in range(16):
                            cx.op("pe", lambda e, kc=kc: e.matmul(pp[:, :], M_[:, kc, :], wo[:, kc, cb * 512:(cb + 1) * 512], start=(kc == 0), stop=(kc == 15)),
                                  r=[M_, wo], w=[pp], sig=(kc == 15))
                        cx.op("dve", lambda e: e.tensor_tensor(out=V[:, cb * 512:(cb + 1) * 512], in0=pp[:], in1=gm[:, cb * 512:(cb + 1) * 512], op=ALU.mult),
                              r=[pp, gm], w=[V])
                    cx.op("dve", lambda e: e.scalar_tensor_tensor(out=V[:], in0=X[:], scalar=ALPHA, in1=V[:], op0=ALU.mult, op1=ALU.add), r=[X, V], w=[V])
                    tmp = tmps.next()
                    ln_stats(P, V, tmp)
                    cx.op("act", lambda e: e.activation(out=V[:], in_=V[:], func=AF.Identity, scale=tmp[2][:], bias=tmp[3][:]), r=[V, tmp[2], tmp[3]], w=[V])
                    cx.op("dve", lambda e: e.tensor_tensor(out=V[:], in0=V[:], in1=l1w[:], op=ALU.mult), r=[V, l1w], w=[V])
                    cx.op("dve", lambda e: e.tensor_tensor(out=X[:], in0=V[:], in1=l1b[:], op=ALU.add), r=[V, l1b], w=[X])
                    cx.dma("sp", x1s[t * 128:(t + 1) * 128, :], X[:], r=[X])
                    ln_stats(P, X, tmp)
                    U = ut.next()
                    cx.op("act", lambda e: e.activation(out=U[:], in_=X[:], func=AF.Identity, scale=tmp[2][:], bias=tmp[3][:]), r=[X, tmp[2], tmp[3]], w=[U])
                    cx.op("dve", lambda e: e.tensor_tensor(out=U[:], in0=U[:], in1=sc1f[:], op=ALU.mult), r=[U, sc1f], w=[U])
                    cx.op("dve", lambda e: e.tensor_tensor(out=U[:], in0=U[:], in1=shf[:], op=ALU.add), r=[U, shf], w=[U])
                    UF, UB = uTf.next(), uTb.next()
                    for g in range(4):
                        pt = ptr.next()
                        for q in range(4):
                            kc = g * 4 + q
                            cx.op("pe", lambda e, kc=kc, q=q: e.transpose(out=pt[:, q * 128:(q + 1) * 128], in_=U[:, kc * 128:(kc + 1) * 128], identity=ident_f[:]),
                                  r=[U, ident_f], w=[pt], sig=(q == 3))
                        cx.op("act", lambda e, g=g: e.copy(out=UF[:, g * 4:(g + 1) * 4, :], in_=pt[:].rearrange("p (q n) -> p q n", q=4)), r=[pt], w=[UF])
                        cx.op("dve", lambda e, g=g: e.tensor_copy(out=UB[:, g * 4:(g + 1) * 4, :], in_=pt[:].rearrange("p (q n) -> p q n", q=4)), r=[pt], w=[UB])
                    cx.dma("sp", u2Ts.rearrange("k p t -> p k t")[:, :, t * 128:(t + 1) * 128], UB[:], r=[UB])
                    pl = plg.next()
                    for kc in range(16):
                        cx.op("pe", lambda e, kc=kc: e.matmul(pl[:, 0:32], UF[:, kc, :], wr[:, kc, :], start=(kc == 0), stop=(kc == 15)),
                              r=[UF, wr], w=[pl], sig=(kc == 15))
                    L, M8, NM, MK, EX, SM, GT = lg.next(), m8.next(), nmx.next(), msk.next(), ex.next(), sm.next(), gt.next()
                    cx.op("dve", lambda e: e.tensor_tensor(out=L[:], in0=pl[:, 0:32], in1=brb[:], op=ALU.add), r=[pl, brb], w=[L])
                    cx.op("dve", lambda e: e.max(out=M8[:], in_=L[:]), r=[L], w=[M8])
                    cx.op("dve", lambda e: e.tensor_scalar(out=NM[:], in0=M8[:, 0:1], scalar1=-1.0, scalar2=None, op0=ALU.mult), r=[M8], w=[NM])
                    cx.op("dve", lambda e: e.tensor_scalar(out=MK[:], in0=L[:], scalar1=M8[:, 3:4], scalar2=None, op0=ALU.is_ge), r=[L, M8], w=[MK])
                    cx.op("act", lambda e: e.activation(out=EX[:], in_=L[:], func=AF.Exp, bias=NM[:], scale=1.0), r=[L, NM], w=[EX])
                    cx.op("dve", lambda e: e.tensor_tensor(out=EX[:], in0=EX[:], in1=MK[:], op=ALU.mult), r=[EX, MK], w=[EX])
                    cx.op("dve", lambda e: e.reduce_sum(out=SM[:], in_=EX[:], axis=AX.X), r=[EX], w=[SM])
                    cx.op("dve", lambda e: e.reciprocal(out=SM[:], in_=SM[:]), r=[SM], w=[SM])
                    cx.op("dve", lambda e: e.tensor_scalar(out=GT[:], in0=EX[:], scalar1=SM[:], scalar2=None, op0=ALU.mult), r=[EX, SM], w=[GT])
                    cx.dma("sp", Gs[t * 128:(t + 1) * 128, :], GT[:], r=[GT])

        if "moe" in phases:
            with Phase(cx) as P:
                u2 = P.sb([128, 16, 512], BF16, "u2")
                G = P.sb([128, 4, 32], F32, "G")
                acc = P.sb([128, 4, D], F32, "acc")
                hT = [P.sb([128, 16, 512], BF16, "hT") for _ in range(1)]
                wgt = RR([P.sb([128, 16, 512], BF16, "wg") for _ in range(2)])
                wut = RR([P.sb([128, 16, 512], BF16, "wu") for _ in range(2)])
                wdt = RR([P.sb([128, 16, 512], BF16, "wd") for _ in range(2)])
                bg = RR([P.sb([128, 32], F32, "bg") for _ in range(2)])
                bd = RR([P.sb([1, D], BF16, "bd") for _ in range(2)])
                pg = RR([P.ps([128, 512], F32, "pg") for _ in range(2)])
                pu = RR([P.ps([128, 512], F32, "pu") for _ in range(2)])
                pyy = RR([P.ps([128, 512], F32, "pyy") for _ in range(2)])
                gs = RR([P.sb([128, 512], F32, "gs") for _ in range(2)])
                sgs = RR([P.sb([128, 512], F32, "sgs") for _ in range(2)])
                ls_ = RR([P.sb([128, 512], F32, "ls") for _ in range(2)])
                for hf in range(2):
                    cx.dma("sp", u2[:], u2Ts.rearrange("k p t -> p k t")[:, :, hf * 512:(hf + 1) * 512], w=[u2])
                    cx.dma("sp", G[:], Gs.rearrange("(t p) e -> p t e", p=128)[:, hf * 4:(hf + 1) * 4, :], w=[G])
                    cx.op("dve", lambda e: e.memset(acc[:], 0.0), w=[acc])
                    for ex_ in range(NE):
                        H = hT[0]
                        BG, BD = bg.next(), bd.next()
                        cx.dma("sp", BG[:], bgu[ex_, :, :], w=[BG])
                        cx.dma("pool", BD[:], b_d[ex_:ex_ + 1, :], w=[BD])
                        wg_v = w_g[ex_].rearrange("(kc p) f -> p kc f", p=128)
                        wu_v = w_u[ex_].rearrange("(kc p) f -> p kc f", p=128)
                        wd_v = w_d[ex_].rearrange("(kc p) f -> p kc f", p=128)
                        for fb in range(4):
                            WG, WU = wgt.next(), wut.next()
                            cx.dma("pool", WG[:], wg_v[:, :, fb * 512:(fb + 1) * 512], w=[WG])
                            cx.dma("pool", WU[:], wu_v[:, :, fb * 512:(fb + 1) * 512], w=[WU])
                            for sub in range(4):
                                fc = fb * 4 + sub
                                p1, p2 = pg.next(), pu.next()
                                for kc in range(16):
                                    cx.op("pe", lambda e, kc=kc: e.matmul(p1[:, :], WG[:, kc, sub * 128:(sub + 1) * 128], u2[:, kc, :], start=(kc == 0), stop=(kc == 15)),
                                          r=[WG, u2], w=[p1], sig=(kc == 15))
                                for kc in range(16):
                                    cx.op("pe", lambda e, kc=kc: e.matmul(p2[:, :], WU[:, kc, sub * 128:(sub + 1) * 128], u2[:, kc, :], start=(kc == 0), stop=(kc == 15)),
                                          r=[WU, u2], w=[p2], sig=(kc == 15))
                                GS, SG, LS = gs.next(), sgs.next(), ls_.next()
                                cx.op("dve", lambda e: e.tensor_scalar(out=GS[:], in0=p1[:], scalar1=BG[:, fc:fc + 1], scalar2=7.0, op0=ALU.add, op1=ALU.min),
                                      r=[p1, BG], w=[GS])
                                cx.op("act", lambda e: e.activation(out=SG[:], in_=GS[:], func=AF.Sigmoid, scale=1.702), r=[GS], w=[SG])
                                cx.op("dve", lambda e: e.tensor_scalar(out=LS[:], in0=p2[:], scalar1=BG[:, 16 + fc:17 + fc], scalar2=7.0, op0=ALU.add, op1=ALU.min),
                                      r=[p2, BG], w=[LS])
                                cx.op("dve", lambda e: e.tensor_scalar(out=LS[:], in0=LS[:], scalar1=-7.0, scalar2=1.0, op0=ALU.max, op1=ALU.add), r=[LS], w=[LS])
                                cx.op("dve", lambda e: e.tensor_tensor(out=GS[:], in0=GS[:], in1=SG[:], op=ALU.mult), r=[GS, SG], w=[GS])
                                cx.op("dve", lambda e: e.tensor_tensor(out=H[:, fc, :], in0=GS[:], in1=LS[:], op=ALU.mult), r=[GS, LS], w=[H])
                        for db in range(4):
                            WD = wdt.next()
                            cx.dma("pool", WD[:], wd_v[:, :, db * 512:(db + 1) * 512], w=[WD])
                            for t in range(4):
                                pp = pyy.next()
                                for fc in range(16):
                                    cx.op("pe", lambda e, fc=fc: e.matmul(pp[:, :], H[:, fc, t * 128:(t + 1) * 128], WD[:, fc, :], start=(fc == 0), stop=False),
                                          r=[H, WD], w=[pp], sig=False)
                                cx.op("pe", lambda e: e.matmul(pp[:, :], ones_b[0:1, :], BD[0:1, db * 512:(db + 1) * 512], start=False, stop=True),
                                      r=[ones_b, BD], w=[pp])
                                cx.op("dve", lambda e: e.scalar_tensor_tensor(out=acc[:, t, db * 512:(db + 1) * 512], in0=pp[:], scalar=G[:, t, ex_:ex_ + 1],
                                                                             in1=acc[:, t, db * 512:(db + 1) * 512], op0=ALU.mult, op1=ALU.add),
                                      r=[pp, G, acc], w=[acc])
                    for t in range(4):
                        row0 = hf * 512 + t * 128
                        cx.dma("sp", y2s[row0:row0 + 128, :], acc[:, t, :], r=[acc])

        if "fin" in phases:
            with Phase(cx) as P:
                gf = load_bc(P, mod_bc(0, 5), "gf")
                l2w = load_bc(P, lnp[2:3, :].partition_broadcast(128), "l2w")
                l2b = load_bc(P, lnp[3:4, :].partition_broadcast(128), "l2b")
                xt = RR([P.sb([128, D], F32, "xt") for _ in range(2)])
                yt = RR([P.sb([128, D], F32, "yt") for _ in range(2)])
                tmps = RR([ln_tmp(P) for _ in range(2)])
                for t in range(8):
                    X, Y = xt.next(), yt.next()
                    row0 = t * 128
                    cx.dma("sp", X[:], x1s[row0:row0 + 128, :], w=[X])
                    cx.dma("sp", Y[:], y2s[row0:row0 + 128, :], w=[Y])
                    cx.op("dve", lambda e: e.tensor_tensor(out=Y[:], in0=Y[:], in1=gf[:], op=ALU.mult), r=[Y, gf], w=[Y])
                    cx.op("dve", lambda e: e.scalar_tensor_tensor(out=X[:], in0=X[:], scalar=ALPHA, in1=Y[:], op0=ALU.mult, op1=ALU.add),
                          r=[X, Y], w=[X])
                    tmp = tmps.next()
                    ln_stats(P, X, tmp)
                    cx.op("act", lambda e: e.activation(out=X[:], in_=X[:], func=AF.Identity, scale=tmp[2][:], bias=tmp[3][:]), r=[X, tmp[2], tmp[3]], w=[X])
                    cx.op("dve", lambda e: e.tensor_tensor(out=X[:], in0=X[:], in1=l2w[:], op=ALU.mult), r=[X, l2w], w=[X])
                    cx.op("dve", lambda e: e.tensor_tensor(out=X[:], in0=X[:], in1=l2b[:], op=ALU.add), r=[X, l2b], w=[X])
                    cx.dma("sp", out[row0:row0 + 128, :], X[:], r=[X])

        cx.barrier()
        PP.__exit__(None, None, None)
        build.ninstr = cx.ninstr
    return nc


def _consts():
    f32 = np.float32
    ident = np.eye(128, dtype=f32)
    perm = np.zeros((128, 128), f32)
    f = np.arange(128)
    j = f % 32
    partner = f - j + (j + 16) % 32
    perm[partner, f] = 1.0
    inv_freq = (10000.0 ** (-np.arange(16, dtype=np.float32) / 16)).astype(f32)
    tok = np.arange(S)
    rowp = (tok // 64).astype(f32)
    colp = (tok % 64).astype(f32)
    i64 = f % 64
    half = i64 // 32
    pos = np.where(half[:, None] == 0, rowp[None, :], colp[None, :]).astype(f32)
    ang = (pos * inv_freq[j % 16][:, None]).astype(f32)
    C = np.cos(ang).astype(f32)
    Sn = np.sin(ang).astype(f32)
    Sn = np.where((j < 16)[:, None], -Sn, Sn).astype(f32)
    return ident, perm, np.stack([C, Sn]).astype(f32)


def _nab(rel_bias):
    p = np.arange(128)
    dr = p // 64
    ck = p % 64
    cq = np.arange(64)
    cstart = np.clip(cq - 8, 0, 48)
    colok = (ck[:, None] >= cstart[None, :]) & (ck[:, None] < cstart[None, :] + 16)
    coff = np.clip(ck[:, None] - cq[None, :] + 15, 0, 30)
    out = np.full((NH, 128, 16, 64), NEG, np.float32)
    for idx in range(16):
        d = idx - 8 + dr
        rowok = np.abs(d) <= 7
        ok = colok & rowok[:, None]
        vals = rel_bias[:, np.clip(d + 7, 0, 14)[:, None], coff]
        out[:, :, idx, :] = np.where(ok[None], vals, NEG)
    return out


def _navalid(j):
    v = np.zeros((128, 16, 6), np.float32)
    p = np.arange(128)
    for i in range(16):
        lo, hi = min(i, 12), max(i + 8, 12)
        ms = list(range(lo // 2, (hi + 1) // 2))
        r = 16 * j + i
        r0 = min(max(r - 4, 0), 56)
        for s_, m in enumerate(ms):
            g = 16 * j - 4 + 2 * m + p // 64
            v[:, i, s_] = ((g >= r0) & (g < r0 + 8)).astype(np.float32)
    return v


def prep_inputs(inp, NE=NE_FULL):
    ident, perm, rope = _consts()
    f32 = np.float32
    A = lambda a: np.ascontiguousarray(a, dtype=f32)
    nab = _nab(np.asarray(inp["na_rel_bias"][0], f32))
    lamv = A(np.concatenate([inp["lam_q1"], inp["lam_k1"], inp["lam_q2"], inp["lam_k2"]], axis=0)).reshape(1, 256)
    lnp = A(np.concatenate([inp["ln1_w"], inp["ln1_b"], inp["ln2_w"], inp["ln2_b"]], axis=0))
    bgu = A(np.concatenate([inp["b_gate"][0].reshape(32, 16, 128).transpose(0, 2, 1),
                            inp["b_up"][0].reshape(32, 16, 128).transpose(0, 2, 1)], axis=2))[:NE]
    shared = dict(
        w_ada=A(inp["w_ada"][0]), b_ada=A(inp["b_ada"]), w_in=A(inp["w_in"][0]), nab=A(nab), lamv=lamv,
        wsub=A(inp["diff_subln_w"][0].reshape(128, 1)), w_proj_na=A(inp["w_proj_na"][0]), w_proj_diff=A(inp["w_proj_diff"][0]),
        w_out=A(inp["w_out"][0]), lnp=lnp, w_router=A(inp["w_router"][0]), b_router=A(inp["b_router"]),
        w_gate=A(inp["w_gate"][0][:NE]), w_up=A(inp["w_up"][0][:NE]), w_down=A(inp["w_down"][0][:NE]), bgu=bgu,
        b_down=A(inp["b_down"][0][:NE]), ropeK=A(rope), ident=ident, perm=perm,
    )
    maps = []
    for i in range(NCORE):
        b, j = i // 4, i % 4
        x = np.asarray(inp["x"][b], f32)
        xh = np.zeros((512, D), f32)
        top0 = 1024 * j - 256
        if top0 >= 0:
            xh[0:256] = x[top0:top0 + 256]
        bot0 = 1024 * j + 1024
        if bot0 + 256 <= S:
            xh[256:512] = x[bot0:bot0 + 256]
        cc = np.stack([np.asarray(inp["c"][b], f32).reshape(16, 128).T, np.asarray(inp["c_ctx"], f32).reshape(16, 128).T], axis=2)
        m = dict(shared)
        m.update(xb=A(x), xo=A(x[1024 * j:1024 * j + 1024]), xh=xh, ctx=A(inp["ctx"][b]), cc=A(cc),
                 navalid=_navalid(j), ropeQ=A(rope[:, :, 1024 * j:1024 * j + 1024]))
        maps.append(m)
    return maps


def kernel(**inputs):
    nc = build()
    maps = prep_inputs(inputs)
    res = run_bass_kernel_spmd(nc, maps, core_ids=list(range(NCORE)))
    outp = np.empty((2, S, D), np.float32)
    for i in range(NCORE):
        b, j = i // 4, i % 4
        outp[b, 1024 * j:1024 * j + 1024] = res.results[i]["out"]
    return outp
```

```python
import math
from contextlib import ExitStack
import numpy as np
import concourse.bass as bass
import concourse.mybir as mybir
from concourse.bass_utils import run_bass_kernel_spmd

F32 = mybir.dt.float32
BF16 = mybir.dt.bfloat16
AF = mybir.ActivationFunctionType
ALU = mybir.AluOpType
AX = mybir.AxisListType

D = 2048
S = 4096
NCORE = 8
TOK = 1024
CTX = 256
NH = 8
NE_FULL = 32
LN_EPS = 1e-6
RMS_EPS = 1e-5
ALPHA = 2.0 ** 0.25
LAM_INIT = 0.8 - 0.6 * math.exp(0.0)
NKEY_DF = S + CTX
NKEY_NA = 24 * 64 + CTX
NSLOT = 6
NEG = -30000.0


class Res:
    __slots__ = ("name", "w", "r", "psum")

    def __init__(self, name):
        self.name = name
        self.w = None
        self.r = {}
        self.psum = False


class Tile:
    def __init__(self, t, name):
        self.t = t
        self.res = Res(name)

    def __getitem__(self, k):
        return self.t[k]


class Eng:
    def __init__(self, name, h, is_pe=False):
        self.name = name
        self.h = h
        self.sem = "e_" + name
        self.cnt = 0
        self.seen = {}
        self.dk = 0
        self.dvals = [0] * NSLOT
        self.is_pe = is_pe


class Ctx:
    def __init__(self, nc, stack):
        self.nc = nc
        self.stack = stack
        self.E = {
            "pe": Eng("pe", nc.tensor, True),
            "act": Eng("act", nc.scalar),
            "dve": Eng("dve", nc.vector),
            "pool": Eng("pool", nc.gpsimd),
            "sp": Eng("sp", nc.sync),
        }
        self.sems = {}
        for e in self.E.values():
            self.sems[e.sem] = stack.enter_context(nc.semaphore(e.sem))
        for q in ("sp", "pool", "act"):
            for k in range(NSLOT):
                n = "d_%s%d" % (q, k)
                self.sems[n] = stack.enter_context(nc.semaphore(n))
        self.ninstr = 0
        import os
        self.limit = int(os.environ.get("K_LIMIT", "0"))
        self.trace = int(os.environ.get("K_TRACE", "0"))

    def skip(self):
        return self.limit and self.ninstr >= self.limit

    def _waits(self, E, reads, writes, extra=()):
        if self.skip():
            return
        waits = {}

        def need(tok):
            if tok is None:
                return
            sem, val = tok
            if E.is_pe and sem == E.sem:
                return
            if E.seen.get(sem, 0) >= val:
                return
            if waits.get(sem, 0) < val:
                waits[sem] = val

        for R in reads:
            need(R.w)
        for R in writes:
            need(R.w)
            for s_, v_ in R.r.items():
                need((s_, v_))
        for t in extra:
            need(t)
        for sem, val in waits.items():
            E.seen[sem] = val
            E.h.wait_ge(self.sems[sem], val)
            self.ninstr += 1

    @staticmethod
    def _res(xs):
        return [x.res if isinstance(x, Tile) else x for x in xs]

    def op(self, eng, fn, r=(), w=(), sig=True):
        E = self.E[eng]
        if self.skip():
            return None
        r = self._res(r)
        w = self._res(w)
        w = w + [R for R in r if R.psum and R not in w]
        r = [R for R in r if not R.psum]
        self._waits(E, r, w)
        inst = fn(E.h)
        self.ninstr += 1
        if self.trace:
            print("OP", self.ninstr, eng, fn.__code__.co_firstlineno)
        if sig:
            E.cnt += 1
            inst.then_inc(self.sems[E.sem], 1)
            tok = (E.sem, E.cnt)
        else:
            tok = (E.sem, E.cnt + 1)
        for R in r:
            if R.r.get(tok[0], 0) < tok[1]:
                R.r[tok[0]] = tok[1]
        for R in w:
            R.w = tok
            R.r = {}
        return inst

    def dma(self, q, out, in_, r=(), w=(), **kw):
        E = self.E[q]
        if self.skip():
            return None
        r = self._res(r)
        w = self._res(w)
        slot = E.dk % NSLOT
        E.dk += 1
        sem = "d_%s%d" % (q, slot)
        prev = E.dvals[slot]
        extra = [(sem, prev)] if prev > 0 else []
        self._waits(E, r, w, extra)
        inst = E.h.dma_start(out=out, in_=in_, **kw)
        self.ninstr += 1
        if self.trace:
            import sys as _s
            print("DMA", self.ninstr, q, _s._getframe(1).f_lineno)
        E.dvals[slot] = prev + 16
        inst.then_inc(self.sems[sem], 16)
        tok = (sem, prev + 16)
        for R in r:
            if R.r.get(tok[0], 0) < tok[1]:
                R.r[tok[0]] = tok[1]
        for R in w:
            R.w = tok
            R.r = {}
        return inst

    def barrier(self):
        lim, self.limit = self.limit, 0
        self._barrier()
        self.limit = lim

    def _barrier(self):
        toks = []
        for e in self.E.values():
            if e.cnt > 0:
                toks.append((e.sem, e.cnt))
            for k in range(NSLOT):
                if e.dvals[k] > 0:
                    toks.append(("d_%s%d" % (e.name, k), e.dvals[k]))
        for e in self.E.values():
            self._waits(e, [], [], toks)


class Phase:
    def __init__(self, cx):
        self.cx = cx
        self.st = ExitStack()
        self.n = 0

    def __enter__(self):
        self.st.__enter__()
        return self

    def __exit__(self, *a):
        self.cx.barrier()
        return self.st.__exit__(*a)

    def sb(self, shape, dt, name=None):
        self.n += 1
        name = (name or "t") + "_%d_%d" % (id(self) % 100000, self.n)
        return Tile(self.st.enter_context(self.cx.nc.sbuf_tensor(name, list(shape), dt)), name)

    def ps(self, shape, dt, name=None):
        self.n += 1
        name = (name or "p") + "_%d_%d" % (id(self) % 100000, self.n)
        t = Tile(self.st.enter_context(self.cx.nc.psum_tensor(name, list(shape), dt)), name)
        t.res.psum = True
        return t


class RR:
    def __init__(self, items):
        self.items = items
        self.i = 0

    def next(self):
        x = self.items[self.i % len(self.items)]
        self.i += 1
        return x


def build(NE=NE_FULL, dbg=False, phases=("mod", "proj", "na", "df", "merge", "out", "moe", "fin")):
    nc = bass.Bass("TRN2", target_bir_lowering=False)
    st = ExitStack()
    with st:
        cx = Ctx(nc, st)

        def din(name, shape, dt=F32):
            return nc.dram_tensor(name, list(shape), dt, kind="ExternalInput").ap()

        def dscr(name, shape, dt=BF16):
            return nc.dram_tensor(name, list(shape), dt, kind="ExternalOutput" if dbg else "Internal").ap()

        xb = din("xb", [S, D])
        xo = din("xo", [TOK, D])
        xh = din("xh", [512, D])
        ctxi = din("ctx", [CTX, D])
        cc = din("cc", [128, 16, 2])
        w_ada = din("w_ada", [D, 6 * D])
        b_ada = din("b_ada", [1, 6 * D])
        w_in = din("w_in", [D, 10240])
        nab = din("nab", [NH, 128, 16, 64])
        navalid = din("navalid", [128, 16, 6])
        lamv = din("lamv", [1, 256])
        wsub = din("wsub", [128, 1])
        w_pn = din("w_proj_na", [1024, D])
        w_pd = din("w_proj_diff", [1024, D])
        w_out = din("w_out", [D, D])
        lnp = din("lnp", [4, D])
        w_r = din("w_router", [D, 32])
        b_r = din("b_router", [1, 32])
        w_g = din("w_gate", [NE, D, D])
        w_u = din("w_up", [NE, D, D])
        w_d = din("w_down", [NE, D, D])
        bgu = din("bgu", [NE, 128, 32])
        b_d = din("b_down", [NE, D])
        ropeK = din("ropeK", [2, 128, S])
        ropeQ = din("ropeQ", [2, 128, TOK])
        ident_in = din("ident", [128, 128])
        perm_in = din("perm", [128, 128])
        out = nc.dram_tensor("out", [TOK, D], F32, kind="ExternalOutput").ap()

        modv = dscr("modv", [2, 6 * D], F32)
        kdfT = dscr("kdfT", [NH, 128, NKEY_DF])
        vdf = dscr("vdf", [NKEY_DF, 1024])
        knaT = dscr("knaT", [NH, 128, NKEY_NA])
        vna = dscr("vna", [NKEY_NA, 1024])
        qnaT = dscr("qnaT", [NH, 128, TOK])
        qdfT = dscr("qdfT", [NH, 128, TOK])
        gnaT = dscr("gnaT", [16, 128, TOK])
        gdfT = dscr("gdfT", [16, 128, TOK])
        onaT = dscr("onaT", [NH, 128, TOK])
        odfT = dscr("odfT", [NH, 128, TOK])
        mrgT = dscr("mrgT", [16, 128, TOK])
        x1s = dscr("x1s", [TOK, D], F32)
        u2Ts = dscr("u2Ts", [16, 128, TOK])
        Gs = dscr("Gs", [TOK, 32], F32)
        y2s = dscr("y2s", [TOK, D], F32)

        PP = Phase(cx)
        PP.__enter__()
        ident_f = PP.sb([128, 128], F32, "identf")
        ident_b = PP.sb([128, 128], BF16, "identb")
        perm_b = PP.sb([128, 128], BF16, "permb")
        ones_b = PP.sb([128, 128], BF16, "onesb")
        ones_f = PP.sb([128, 128], F32, "onesf")
        cx.dma("sp", ident_f[:], ident_in[:, :], w=[ident_f])
        cx.dma("pool", ident_b[:], ident_in[:, :], w=[ident_b])
        cx.dma("pool", perm_b[:], perm_in[:, :], w=[perm_b])
        cx.op("dve", lambda e: e.memset(ones_b[:], 1.0), w=[ones_b])
        cx.op("dve", lambda e: e.memset(ones_f[:], 1.0), w=[ones_f])
        eps_ln = PP.sb([128, 1], F32, "epsln")
        eps_rms = PP.sb([128, 1], F32, "epsrms")
        cx.op("dve", lambda e: e.memset(eps_ln[:], LN_EPS), w=[eps_ln])
        cx.op("dve", lambda e: e.memset(eps_rms[:], RMS_EPS), w=[eps_rms])

        w_in_v = w_in.rearrange("(kc p) n -> p kc n", p=128)

        if "mod" in phases:
            with Phase(cx) as P:
                ccs = P.sb([128, 32], F32, "ccs")
                sg = P.sb([128, 32], F32, "sg")
                sil = P.sb([128, 16, 2], BF16, "sil")
                bsb = P.sb([2, 6 * D], F32, "bsb")
                msb = P.sb([2, 6 * D], F32, "msb")
                wt = [P.sb([128, 16, 512], BF16, "wada") for _ in range(3)]
                pb = [P.ps([128, 512], F32, "pm") for _ in range(2)]
                cx.dma("sp", ccs[:], cc.rearrange("p k t -> p (k t)"), w=[ccs])
                cx.dma("sp", bsb[0:1, :], b_ada[:, :], w=[bsb])
                cx.dma("sp", bsb[1:2, :], b_ada[:, :], w=[bsb])
                cx.op("act", lambda e: e.activation(out=sg[:], in_=ccs[:], func=AF.Sigmoid), r=[ccs], w=[sg])
                cx.op("dve", lambda e: e.tensor_tensor(out=sil[:].rearrange("p k t -> p (k t)"), in0=ccs[:], in1=sg[:], op=ALU.mult),
                      r=[ccs, sg], w=[sil])
                w_ada_v = w_ada.rearrange("(kc p) n -> p kc n", p=128)
                for n in range(24):
                    W = wt[n % 3]
                    cx.dma("pool", W[:], w_ada_v[:, :, n * 512:(n + 1) * 512], w=[W])
                    pp = pb[n % 2]
                    for kc in range(16):
                        cx.op("pe", lambda e, kc=kc: e.matmul(pp[0:2, :], sil[:, kc, :], W[:, kc, :], start=(kc == 0), stop=(kc == 15)),
                              r=[sil, W], w=[pp], sig=(kc == 15))
                    cx.op("dve", lambda e: e.tensor_tensor(out=msb[0:2, n * 512:(n + 1) * 512], in0=pp[0:2, :],
                                                          in1=bsb[0:2, n * 512:(n + 1) * 512], op=ALU.add),
                          r=[pp, bsb], w=[msb])
                cx.dma("sp", modv[:, :], msb[0:2, :], r=[msb])

        def mod_bc(row, idx):
            return modv[row:row + 1, idx * D:(idx + 1) * D].partition_broadcast(128)

        def load_bc(P, src_ap, name, plus1=False):
            t = P.sb([128, D], F32, name)
            cx.dma("sp", t[:], src_ap, w=[t])
            if plus1:
                cx.op("dve", lambda e: e.tensor_scalar(out=t[:], in0=t[:], scalar1=1.0, scalar2=None, op0=ALU.add), r=[t], w=[t])
            return t

        def ln_stats(P, src, tmp):
            stt, mv, rstd, nmr = tmp
            for c in range(4):
                cx.op("dve", lambda e, c=c: e.bn_stats(out=stt[:, c, :], in_=src[:, c * 512:(c + 1) * 512]), r=[src], w=[stt])
            cx.op("dve", lambda e: e.bn_aggr(out=mv[:], in_=stt[:].rearrange("p a b -> p (a b)")), r=[stt], w=[mv])
            cx.op("act", lambda e: e.activation(out=rstd[:], in_=mv[:, 1:2], func=AF.Sqrt, bias=eps_ln[:], scale=1.0), r=[mv, eps_ln], w=[rstd])
            cx.op("dve", lambda e: e.reciprocal(out=rstd[:], in_=rstd[:]), r=[rstd], w=[rstd])
            cx.op("dve", lambda e: e.tensor_scalar(out=nmr[:], in0=mv[:, 0:1], scalar1=rstd[:], scalar2=-1.0, op0=ALU.mult, op1=ALU.mult),
                  r=[mv, rstd], w=[nmr])

        def ln_tmp(P):
            return (P.sb([128, 4, 6], F32, "stt"), P.sb([128, 2], F32, "mv"), P.sb([128, 1], F32, "rstd"), P.sb([128, 1], F32, "nmr"))

        if "proj" in phases:
            with Phase(cx) as P:
                sc1_m = load_bc(P, mod_bc(0, 1), "sc1m", True)
                sh_m = load_bc(P, mod_bc(0, 0), "shm")
                sc1_c = load_bc(P, mod_bc(1, 1), "sc1c", True)
                sh_c = load_bc(P, mod_bc(1, 0), "shc")
                xt = [P.sb([128, D], F32, "xt") for _ in range(2)]
                zt = [P.sb([128, D], F32, "zt") for _ in range(2)]
                ut = [P.sb([128, D], BF16, "ut") for _ in range(2)]
                tmps = [ln_tmp(P) for _ in range(2)]
                uT = [P.sb([128, 16, 512], BF16, "uT") for _ in range(2)]
                wts = RR([P.sb([128, 16, 512], BF16, "win") for _ in range(3)])
                ptr = RR([P.ps([128, 1024], BF16, "ptr") for _ in range(2)])
                pmm = RR([P.ps([128, 512], F32, "pmm") for _ in range(4)])
                prp = pmm
                osb = RR([P.sb([128, 512], BF16, "osb") for _ in range(4)])
                xsb = RR([P.sb([128, 512], BF16, "xsb") for _ in range(2)])
                t1s = RR([P.sb([128, 512], F32, "t1s") for _ in range(2)])
                rC = RR([P.sb([128, 512], F32, "rC") for _ in range(2)])
                rS = RR([P.sb([128, 512], F32, "rS") for _ in range(2)])
                tcount = [0]

                def ln_gen(src_rows, ntok, sc1, sh, U):
                    for t in range(ntok // 128):
                        k = tcount[0] % 2
                        tcount[0] += 1
                        X, Z, Ub, tmp = xt[k], zt[k], ut[k], tmps[k]
                        cx.dma("sp", X[:], src_rows[t * 128:(t + 1) * 128, :], w=[X])
                        ln_stats(P, X, tmp)
                        cx.op("act", lambda e: e.activation(out=Z[:], in_=X[:], func=AF.Identity, scale=tmp[2][:], bias=tmp[3][:]),
                              r=[X, tmp[2], tmp[3]], w=[Z])
                        cx.op("dve", lambda e: e.tensor_tensor(out=Z[:], in0=Z[:], in1=sc1[:], op=ALU.mult), r=[Z, sc1], w=[Z])
                        cx.op("dve", lambda e: e.tensor_tensor(out=Ub[:], in0=Z[:], in1=sh[:], op=ALU.add), r=[Z, sh], w=[Ub])
                        yield 1
                        for g in range(2):
                            pt = ptr.next()
                            for q in range(8):
                                kc = g * 8 + q
                                cx.op("pe", lambda e, kc=kc, q=q: e.transpose(out=pt[:, q * 128:(q + 1) * 128], in_=Ub[:, kc * 128:(kc + 1) * 128],
                                                                            identity=ident_b[:]),
                                      r=[Ub, ident_b], w=[pt], sig=(q == 7))
                            cx.op("act", lambda e, g=g: e.copy(out=U[:, g * 8:(g + 1) * 8, t * 128:(t + 1) * 128],
                                                              in_=pt[:].rearrange("p (q n) -> p q n", q=8)),
                                  r=[pt], w=[U])
                        yield 2

                hk = [None]

                def step():
                    if hk[0] is not None:
                        if next(hk[0], None) is None:
                            hk[0] = None

                def drain():
                    while hk[0] is not None:
                        step()

                def proj(U, ntok, col0, ncols, mode, dest, rope=None):
                    for cb in range(ncols // 512):
                        W = wts.next()
                        c0 = col0 + cb * 512
                        cx.dma("pool", W[:], w_in_v[:, :, c0:c0 + 512], w=[W])
                        step()
                        if mode == "FM":
                            for sub in range(4):
                                pm = pmm.next()
                                for kc in range(16):
                                    cx.op("pe", lambda e, kc=kc: e.matmul(pm[:, 0:ntok], W[:, kc, sub * 128:(sub + 1) * 128], U[:, kc, 0:ntok],
                                                                         start=(kc == 0), stop=(kc == 15)),
                                          r=[W, U], w=[pm], sig=(kc == 15))
                                ci = cb * 4 + sub
                                if rope is None:
                                    O = osb.next()
                                    cx.op("act", lambda e: e.copy(out=O[:, 0:ntok], in_=pm[:, 0:ntok]), r=[pm], w=[O])
                                    dest(ci, O)
                                else:
                                    Cc, Ss = rope
                                    Xs = xsb.next()
                                    cx.op("act", lambda e: e.copy(out=Xs[:, 0:ntok], in_=pm[:, 0:ntok]), r=[pm], w=[Xs])
                                    pr = prp.next()
                                    cx.op("pe", lambda e: e.matmul(pr[:, 0:ntok], perm_b[:], Xs[:, 0:ntok], start=True, stop=True),
                                          r=[perm_b, Xs], w=[pr])
                                    T1 = t1s.next()
                                    cx.op("dve", lambda e: e.tensor_tensor(out=T1[:, 0:ntok], in0=Xs[:, 0:ntok], in1=Cc[:, 0:ntok], op=ALU.mult),
                                          r=[Xs, Cc], w=[T1])
                                    T2 = t1s.next()
                                    cx.op("dve", lambda e: e.tensor_tensor(out=T2[:, 0:ntok], in0=pr[:, 0:ntok], in1=Ss[:, 0:ntok], op=ALU.mult),
                                          r=[pr, Ss], w=[T2])
                                    O = osb.next()
                                    cx.op("dve", lambda e: e.tensor_tensor(out=O[:, 0:ntok], in0=T1[:, 0:ntok], in1=T2[:, 0:ntok], op=ALU.add),
                                          r=[T1, T2], w=[O])
                                    dest(ci, O)
                        else:
                            for t in range(ntok // 128):
                                pm = pmm.next()
                                for kc in range(16):
                                    cx.op("pe", lambda e, kc=kc: e.matmul(pm[:, :], U[:, kc, t * 128:(t + 1) * 128], W[:, kc, :],
                                                                         start=(kc == 0), stop=(kc == 15)),
                                          r=[W, U], w=[pm], sig=(kc == 15))
                                O = osb.next()
                                cx.op("act", lambda e: e.copy(out=O[:], in_=pm[:]), r=[pm], w=[O])
                                dest(t, cb, O)
                        step()

                def st_fm(dst, tok0, ntok):
                    return lambda ci, O: cx.dma("sp", dst[ci, :, tok0:tok0 + ntok], O[:, 0:ntok], r=[O])

                def st_tm(dst, row0):
                    return lambda t, cb, O: cx.dma("sp", dst[row0 + t * 128:row0 + (t + 1) * 128, cb * 512:(cb + 1) * 512], O[:], r=[O])

                ub = [0]

                def nextU():
                    ub[0] += 1
                    return uT[ub[0] % 2]

                def st_halo_fm(ci, O):
                    cx.dma("sp", knaT[ci, :, 0:256], O[:, 0:256], r=[O])
                    cx.dma("sp", knaT[ci, :, 1280:1536], O[:, 256:512], r=[O])

                def st_halo_tm(t, cb, O):
                    row0 = t * 128 if t < 2 else 1280 + (t - 2) * 128
                    cx.dma("sp", vna[row0:row0 + 128, cb * 512:(cb + 1) * 512], O[:], r=[O])

                blocks = []

                def s1_work(blk):
                    def f(U):
                        Cc, Ss = rC.next(), rS.next()
                        cx.dma("sp", Cc[:], ropeK[0, :, blk * 512:(blk + 1) * 512], w=[Cc])
                        cx.dma("sp", Ss[:], ropeK[1, :, blk * 512:(blk + 1) * 512], w=[Ss])
                        proj(U, 512, 2048, 1024, "FM", st_fm(kdfT, blk * 512, 512), rope=(Cc, Ss))
                        proj(U, 512, 3072, 1024, "TM", st_tm(vdf, blk * 512))
                    return f

                def s2_work(U):
                    proj(U, CTX, 0, 1024, "FM", st_fm(knaT, 1536, CTX))
                    proj(U, CTX, 1024, 1024, "TM", st_tm(vna, 1536))
                    proj(U, CTX, 2048, 1024, "FM", st_fm(kdfT, S, CTX))
                    proj(U, CTX, 3072, 1024, "TM", st_tm(vdf, S))

                def s3_work(U):
                    proj(U, 512, 0, 1024, "FM", st_halo_fm)
                    proj(U, 512, 1024, 1024, "TM", st_halo_tm)

                def s4_work(blk):
                    def f(U):
                        Cc, Ss = rC.next(), rS.next()
                        cx.dma("sp", Cc[:], ropeQ[0, :, blk * 512:(blk + 1) * 512], w=[Cc])
                        cx.dma("sp", Ss[:], ropeQ[1, :, blk * 512:(blk + 1) * 512], w=[Ss])
                        proj(U, 512, 0, 1024, "FM", st_fm(knaT, 256 + blk * 512, 512))
                        proj(U, 512, 1024, 1024, "TM", st_tm(vna, 256 + blk * 512))
                        proj(U, 512, 4096, 1024, "FM", st_fm(qnaT, blk * 512, 512))
                        proj(U, 512, 5120, 1024, "FM", st_fm(qdfT, blk * 512, 512), rope=(Cc, Ss))
                        proj(U, 512, 6144, 2048, "FM", st_fm(gnaT, blk * 512, 512))
                        proj(U, 512, 8192, 2048, "FM", st_fm(gdfT, blk * 512, 512))
                    return f

                for blk in range(S // 512):
                    blocks.append((xb[blk * 512:(blk + 1) * 512, :], 512, sc1_m, sh_m, s1_work(blk)))
                blocks.append((ctxi, CTX, sc1_c, sh_c, s2_work))
                blocks.append((xh, 512, sc1_m, sh_m, s3_work))
                for blk in range(2):
                    blocks.append((xo[blk * 512:(blk + 1) * 512, :], 512, sc1_m, sh_m, s4_work(blk)))
                hk[0] = ln_gen(blocks[0][0], blocks[0][1], blocks[0][2], blocks[0][3], uT[0])
                drain()
                for bi, (src, ntok, sc1, sh, work) in enumerate(blocks):
                    if bi + 1 < len(blocks):
                        nb_ = blocks[bi + 1]
                        hk[0] = ln_gen(nb_[0], nb_[1], nb_[2], nb_[3], uT[(bi + 1) % 2])
                    work(uT[bi % 2])
                    drain()

        if "na" in phases:
            with Phase(cx) as P:
                val = P.sb([128, 16, 6], F32, "val")
                cx.dma("sp", val[:], navalid[:, :, :], w=[val])
                kT = [P.sb([128, NKEY_NA], BF16, "kT") for _ in range(2)]
                vv = [P.sb([128, 14, 128], BF16, "vv") for _ in range(2)]
                qT = [P.sb([128, TOK], BF16, "qT") for _ in range(2)]
                nbf = [P.sb([128, 16, 64], F32, "nbf") for _ in range(2)]
                EB = [P.sb([128, 16, 64], BF16, "EB") for _ in range(2)]
                oT = [P.sb([128, TOK], BF16, "oT") for _ in range(2)]
                pS = RR([P.ps([128, 512], F32, "pS") for _ in range(2)])
                pO = RR([P.ps([128, 512], F32, "pO") for _ in range(2)])
                pZ = RR([P.ps([128, 512], F32, "pZ") for _ in range(2)])
                esb = RR([P.sb([128, 512], BF16, "esb") for _ in range(3)])
                e2 = RR([P.sb([128, 512], BF16, "e2") for _ in range(3)])
                rsb = RR([P.sb([128, 64], F32, "rsb") for _ in range(2)])
                scale = 128 ** -0.5
                def na_load(h):
                    K_, V_, Q_, NB_, EB_, O_ = kT[h % 2], vv[h % 2], qT[h % 2], nbf[h % 2], EB[h % 2], oT[h % 2]
                    cx.dma("sp", K_[:], knaT[h, :, :], w=[K_])
                    cx.dma("sp", V_[:], vna.rearrange("(c p) d -> p c d", p=128)[:, :, h * 128:(h + 1) * 128], w=[V_])
                    cx.dma("sp", Q_[:], qnaT[h, :, :], w=[Q_])
                    cx.dma("sp", NB_[:], nab[h, :, :, :], w=[NB_])
                    cx.op("act", lambda e: e.activation(out=EB_[:], in_=NB_[:], func=AF.Exp), r=[NB_], w=[EB_])

                def na_A(h, i):
                    K_, V_, Q_, NB_, EB_, O_ = kT[h % 2], vv[h % 2], qT[h % 2], nbf[h % 2], EB[h % 2], oT[h % 2]
                    lo, hi = min(i, 12), max(i + 8, 12)
                    ms = list(range(lo // 2, (hi + 1) // 2))
                    nl = len(ms)
                    chunks = ms + [12, 13]
                    ps = pS.next()
                    q = Q_[:, i * 64:(i + 1) * 64]
                    for ci, m in enumerate(chunks):
                        cx.op("pe", lambda e, ci=ci, m=m: e.matmul(ps[:, ci * 64:(ci + 1) * 64], K_[:, m * 128:(m + 1) * 128], q,
                                                                   start=True, stop=True),
                              r=[K_, Q_], w=[ps], sig=(ci == len(chunks) - 1))
                    nc_ = len(chunks)
                    E1 = esb.next()
                    cx.op("act", lambda e: e.activation(out=E1[:, 0:nc_ * 64], in_=ps[:, 0:nc_ * 64], func=AF.Exp, scale=scale), r=[ps], w=[E1])
                    idx0 = 2 * ms[0] - i - 4 + 8
                    E2 = e2.next()
                    cx.op("dve", lambda e: e.tensor_tensor(out=E2[:, 0:nl * 64].rearrange("p (c n) -> p c n", n=64),
                                                          in0=E1[:, 0:nl * 64].rearrange("p (c n) -> p c n", n=64),
                                                          in1=EB_[:, idx0:idx0 + 2 * nl - 1:2, :], op=ALU.mult),
                          r=[E1, EB_], w=[E2])
                    cx.op("dve", lambda e: e.tensor_tensor(out=E1[:, 0:nl * 64].rearrange("p (c n) -> p c n", n=64),
                                                          in0=E2[:, 0:nl * 64].rearrange("p (c n) -> p c n", n=64),
                                                          in1=val[:, i, 0:nl].unsqueeze(2).to_broadcast([128, nl, 64]), op=ALU.mult),
                          r=[E2, val], w=[E1])
                    return (chunks, E1)

                def na_B(h, i, st_):
                    K_, V_, Q_, NB_, EB_, O_ = kT[h % 2], vv[h % 2], qT[h % 2], nbf[h % 2], EB[h % 2], oT[h % 2]
                    chunks, E1 = st_
                    po, pz = pO.next(), pZ.next()
                    for ci, m in enumerate(chunks):
                        last = ci == len(chunks) - 1
                        cx.op("pe", lambda e, ci=ci, m=m: e.matmul(po[:, 0:64], V_[:, m, :], E1[:, ci * 64:(ci + 1) * 64],
                                                                   start=(ci == 0), stop=last), r=[V_, E1], w=[po], sig=False)
                        cx.op("pe", lambda e, ci=ci: e.matmul(pz[:, 0:64], ones_b[:], E1[:, ci * 64:(ci + 1) * 64],
                                                              start=(ci == 0), stop=last), r=[ones_b, E1], w=[pz], sig=last)
                    R_ = rsb.next()
                    cx.op("dve", lambda e: e.reciprocal(out=R_[:], in_=pz[:, 0:64]), r=[pz, po], w=[R_])
                    cx.op("dve", lambda e: e.tensor_tensor(out=O_[:, i * 64:(i + 1) * 64], in0=po[:, 0:64], in1=R_[:], op=ALU.mult),
                          r=[po, R_], w=[O_])
                    if i == 15:
                        cx.dma("sp", onaT[h, :, :], O_[:], r=[O_])

                its = [(h, i) for h in range(NH) for i in range(16)]
                pend = []
                for n_, (h, i) in enumerate(its):
                    if i == 0:
                        na_load(h)
                    pend.append((h, i, na_A(h, i)))
                    if len(pend) > 1:
                        na_B(*pend.pop(0))
                while pend:
                    na_B(*pend.pop(0))

        if "df" in phases:
            with Phase(cx) as P:
                lv = P.sb([128, 4, 64], F32, "lv")
                lp = P.sb([128, 2, 64], F32, "lp")
                ls = P.sb([128, 2], F32, "ls")
                le = P.sb([128, 2], F32, "le")
                nlam = P.sb([128, 1], F32, "nlam")
                wsc = P.sb([128, 1], F32, "wsc")
                cx.dma("sp", lv[:].rearrange("p a d -> p (a d)"), lamv[0:1, :].partition_broadcast(128), w=[lv])
                cx.dma("sp", wsc[:], wsub[:, :], w=[wsc])
                cx.op("dve", lambda e: e.tensor_tensor(out=lp[:, 0, :], in0=lv[:, 0, :], in1=lv[:, 1, :], op=ALU.mult), r=[lv], w=[lp])
                cx.op("dve", lambda e: e.tensor_tensor(out=lp[:, 1, :], in0=lv[:, 2, :], in1=lv[:, 3, :], op=ALU.mult), r=[lv], w=[lp])
                cx.op("dve", lambda e: e.reduce_sum(out=ls[:], in_=lp[:], axis=AX.X), r=[lp], w=[ls])
                cx.op("act", lambda e: e.activation(out=le[:], in_=ls[:], func=AF.Exp), r=[ls], w=[le])
                cx.op("dve", lambda e: e.tensor_scalar(out=nlam[:], in0=le[:, 1:2], scalar1=le[:, 0:1], scalar2=-LAM_INIT, op0=ALU.subtract, op1=ALU.add),
                      r=[le], w=[nlam])
                kT = [P.sb([128, NKEY_DF], BF16, "kT") for _ in range(2)]
                vv = [P.sb([128, 34, 128], BF16, "vv") for _ in range(2)]
                qT = [P.sb([128, TOK], BF16, "qT") for _ in range(2)]
                oT = [P.sb([128, TOK], BF16, "oT") for _ in range(2)]
                pS = RR([P.ps([128, 512], F32, "pS") for _ in range(3)])
                pO = [P.ps([128, 512], F32, "pO") for _ in range(2)]
                pZ = [P.ps([128, 512], F32, "pZ") for _ in range(2)]
                pS2 = P.ps([128, 512], F32, "pS2")
                esb = RR([P.sb([128, 512], BF16, "esb") for _ in range(4)])
                r0 = P.sb([128, 512], F32, "r0")
                r1 = P.sb([128, 512], F32, "r1")
                t0 = P.sb([128, 512], F32, "t0")
                t1 = P.sb([128, 512], F32, "t1")
                of = P.sb([128, 512], F32, "of")
                sq = P.sb([128, 512], F32, "sq")
                rs = P.sb([128, 512], F32, "rs")
                NKC = NKEY_DF // 128

                def df_load(h):
                    K_, V_, Q_, O_ = kT[h % 2], vv[h % 2], qT[h % 2], oT[h % 2]
                    cx.dma("sp", K_[:], kdfT[h, :, :], w=[K_])
                    cx.dma("sp", V_[:], vdf.rearrange("(c p) d -> p c d", p=128)[:, :, h * 128:(h + 1) * 128], w=[V_])
                    cx.dma("sp", Q_[:], qdfT[h, :, :], w=[Q_])

                def df_A(h, qb, kc, m):
                    K_, V_, Q_, O_ = kT[h % 2], vv[h % 2], qT[h % 2], oT[h % 2]
                    ps = pS.next()
                    cx.op("pe", lambda e: e.matmul(ps[:, :], K_[m * 64:(m + 1) * 64, kc * 128:(kc + 1) * 128],
                                                   Q_[m * 64:(m + 1) * 64, qb * 512:(qb + 1) * 512], start=True, stop=True),
                          r=[K_, Q_], w=[ps])
                    E1 = esb.next()
                    cx.op("act", lambda e: e.activation(out=E1[:], in_=ps[:], func=AF.Exp, scale=0.125), r=[ps], w=[E1])
                    return E1

                def df_B(h, qb, kc, m, E1):
                    K_, V_, Q_, O_ = kT[h % 2], vv[h % 2], qT[h % 2], oT[h % 2]
                    last = kc == NKC - 1
                    cx.op("pe", lambda e: e.matmul(pO[m][:, :], V_[:, kc, :], E1[:], start=(kc == 0), stop=last),
                          r=[V_, E1], w=[pO[m]], sig=False)
                    cx.op("pe", lambda e: e.matmul(pZ[m][:, :], ones_b[:], E1[:], start=(kc == 0), stop=last),
                          r=[ones_b, E1], w=[pZ[m]], sig=last)
                    if not (last and m == 1):
                        return
                    cx.op("dve", lambda e: e.reciprocal(out=r0[:], in_=pZ[0][:]), r=[pZ[0]], w=[r0])
                    cx.op("dve", lambda e: e.reciprocal(out=r1[:], in_=pZ[1][:]), r=[pZ[1]], w=[r1])
                    cx.op("dve", lambda e: e.tensor_scalar(out=r1[:], in0=r1[:], scalar1=nlam[:], scalar2=None, op0=ALU.mult), r=[r1, nlam], w=[r1])
                    cx.op("dve", lambda e: e.tensor_tensor(out=t0[:], in0=pO[0][:], in1=r0[:], op=ALU.mult), r=[pO[0], r0], w=[t0])
                    cx.op("dve", lambda e: e.tensor_tensor(out=t1[:], in0=pO[1][:], in1=r1[:], op=ALU.mult), r=[pO[1], r1], w=[t1])
                    cx.op("dve", lambda e: e.tensor_tensor(out=of[:], in0=t0[:], in1=t1[:], op=ALU.add), r=[t0, t1], w=[of])
                    cx.op("act", lambda e: e.activation(out=sq[:], in_=of[:], func=AF.Square), r=[of], w=[sq])
                    ps = pS2
                    cx.op("pe", lambda e: e.matmul(ps[:, :], ones_f[:], sq[:], start=True, stop=True), r=[ones_f, sq], w=[ps])
                    cx.op("act", lambda e: e.activation(out=rs[:], in_=ps[:], func=AF.Sqrt, bias=eps_rms[:], scale=1.0 / 128), r=[ps, eps_rms], w=[rs])
                    cx.op("dve", lambda e: e.reciprocal(out=rs[:], in_=rs[:]), r=[rs], w=[rs])
                    cx.op("dve", lambda e: e.tensor_tensor(out=of[:], in0=of[:], in1=rs[:], op=ALU.mult), r=[of, rs], w=[of])
                    cx.op("dve", lambda e: e.tensor_scalar(out=O_[:, qb * 512:(qb + 1) * 512], in0=of[:], scalar1=wsc[:], scalar2=1.0 - LAM_INIT,
                                                          op0=ALU.mult, op1=ALU.mult), r=[of, wsc], w=[O_])
                    if qb == 1:
                        cx.dma("sp", odfT[h, :, :], O_[:], r=[O_])

                its = [(h, qb, kc, m) for h in range(NH) for qb in range(2) for kc in range(NKC) for m in range(2)]
                pend = []
                for (h, qb, kc, m) in its:
                    if qb == 0 and kc == 0 and m == 0:
                        df_load(h)
                    pend.append((h, qb, kc, m, df_A(h, qb, kc, m)))
                    if len(pend) > 2:
                        df_B(*pend.pop(0))
                while pend:
                    df_B(*pend.pop(0))

        if "merge" in phases:
            with Phase(cx) as P:
                wpn = P.sb([128, 8, D], BF16, "wpn")
                wpd = P.sb([128, 8, D], BF16, "wpd")
                on = P.sb([128, 8, TOK], BF16, "on")
                od = P.sb([128, 8, TOK], BF16, "od")
                cx.dma("pool", wpn[:], w_pn.rearrange("(h p) n -> p h n", p=128), w=[wpn])
                cx.dma("pool", wpd[:], w_pd.rearrange("(h p) n -> p h n", p=128), w=[wpd])
                cx.dma("sp", on[:], onaT.rearrange("h p t -> p h t"), w=[on])
                cx.dma("sp", od[:], odfT.rearrange("h p t -> p h t"), w=[od])
                gn = RR([P.sb([128, TOK], BF16, "gn") for _ in range(2)])
                gd = RR([P.sb([128, TOK], BF16, "gd") for _ in range(2)])
                pA = RR([P.ps([128, 512], F32, "pA") for _ in range(2)])
                pD = RR([P.ps([128, 512], F32, "pD") for _ in range(2)])
                ta = RR([P.sb([128, 512], F32, "ta") for _ in range(2)])
                tb = RR([P.sb([128, 512], F32, "tb") for _ in range(2)])
                mo = RR([P.sb([128, TOK], BF16, "mo") for _ in range(2)])
                for fc in range(16):
                    Gn, Gd, Mo = gn.next(), gd.next(), mo.next()
                    cx.dma("sp", Gn[:], gnaT[fc, :, :], w=[Gn])
                    cx.dma("sp", Gd[:], gdfT[fc, :, :], w=[Gd])
                    cx.op("act", lambda e: e.activation(out=Gn[:], in_=Gn[:], func=AF.Sigmoid), r=[Gn], w=[Gn])
                    cx.op("act", lambda e: e.activation(out=Gd[:], in_=Gd[:], func=AF.Sigmoid), r=[Gd], w=[Gd])
                    for hf in range(2):
                        pa, pd = pA.next(), pD.next()
                        for h in range(8):
                            cx.op("pe", lambda e, h=h: e.matmul(pa[:, :], wpn[:, h, fc * 128:(fc + 1) * 128], on[:, h, hf * 512:(hf + 1) * 512],
                                                                start=(h == 0), stop=(h == 7)), r=[wpn, on], w=[pa], sig=(h == 7))
                        for h in range(8):
                            cx.op("pe", lambda e, h=h: e.matmul(pd[:, :], wpd[:, h, fc * 128:(fc + 1) * 128], od[:, h, hf * 512:(hf + 1) * 512],
                                                                start=(h == 0), stop=(h == 7)), r=[wpd, od], w=[pd], sig=(h == 7))
                        Ta, Tb = ta.next(), tb.next()
                        cx.op("dve", lambda e: e.tensor_tensor(out=Ta[:], in0=pa[:], in1=Gn[:, hf * 512:(hf + 1) * 512], op=ALU.mult), r=[pa, Gn], w=[Ta])
                        cx.op("dve", lambda e: e.tensor_tensor(out=Tb[:], in0=pd[:], in1=Gd[:, hf * 512:(hf + 1) * 512], op=ALU.mult), r=[pd, Gd], w=[Tb])
                        cx.op("dve", lambda e: e.tensor_tensor(out=Mo[:, hf * 512:(hf + 1) * 512], in0=Ta[:], in1=Tb[:], op=ALU.add), r=[Ta, Tb], w=[Mo])
                    cx.dma("sp", mrgT[fc, :, :], Mo[:], r=[Mo])

        if "out" in phases:
            with Phase(cx) as P:
                wo = P.sb([128, 16, D], BF16, "wo")
                cx.dma("pool", wo[:], w_out.rearrange("(kc p) n -> p kc n", p=128), w=[wo])
                gm = load_bc(P, mod_bc(0, 2), "gm")
                l1w = load_bc(P, lnp[0:1, :].partition_broadcast(128), "l1w")
                l1b = load_bc(P, lnp[1:2, :].partition_broadcast(128), "l1b")
                sc1f = load_bc(P, mod_bc(0, 4), "sc1f", True)
                shf = load_bc(P, mod_bc(0, 3), "shf")
                wr = P.sb([128, 16, 32], F32, "wr")
                cx.dma("sp", wr[:], w_r.rearrange("(kc p) e -> p kc e", p=128), w=[wr])
                brb = P.sb([128, 32], F32, "brb")
                cx.dma("sp", brb[:], b_r[0:1, :].partition_broadcast(128), w=[brb])
                mT = RR([P.sb([128, 16, 128], BF16, "mT") for _ in range(2)])
                xt = RR([P.sb([128, D], F32, "xt") for _ in range(2)])
                vt = RR([P.sb([128, D], F32, "vt") for _ in range(1)])
                ut = RR([P.sb([128, D], F32, "ut") for _ in range(1)])
                tmps = RR([ln_tmp(P) for _ in range(2)])
                py = RR([P.ps([128, 512], F32, "py") for _ in range(4)])
                ptr = RR([P.ps([128, 512], F32, "ptr") for _ in range(2)])
                plg = RR([P.ps([128, 512], F32, "plg") for _ in range(2)])
                uTf = RR([P.sb([128, 16, 128], F32, "uTf") for _ in range(2)])
                uTb = RR([P.sb([128, 16, 128], BF16, "uTb") for _ in range(2)])
                lg = RR([P.sb([128, 32], F32, "lg") for _ in range(2)])
                m8 = RR([P.sb([128, 8], F32, "m8") for _ in range(2)])
                nmx = RR([P.sb([128, 1], F32, "nmx") for _ in range(2)])
                msk = RR([P.sb([128, 32], F32, "msk") for _ in range(2)])
                ex = RR([P.sb([128, 32], F32, "ex") for _ in range(2)])
                sm = RR([P.sb([128, 1], F32, "sm") for _ in range(2)])
                gt = RR([P.sb([128, 32], F32, "gt") for _ in range(2)])
                for t in range(8):
                    M_ = mT.next()
                    cx.dma("sp", M_[:], mrgT.rearrange("k p t -> p k t")[:, :, t * 128:(t + 1) * 128], w=[M_])
                    X = xt.next()
                    cx.dma("sp", X[:], xo[t * 128:(t + 1) * 128, :], w=[X])
                    V = vt.next()
                    for cb in range(4):
                        pp = py.next()
                        for kc in range(16):
                            cx.op("pe", lambda e, kc=kc: e.matmul(pp[:, :], M_[:, kc, :], wo[:, kc, cb * 512:(cb + 1) * 512], start=(kc == 0), stop=(kc == 15)),
                                  r=[M_, wo], w=[pp], sig=(kc == 15))
                        cx.op("dve", lambda e: e.tensor_tensor(out=V[:, cb * 512:(cb + 1) * 512], in0=pp[:], in1=gm[:, cb * 512:(cb + 1) * 512], op=ALU.mult),
                              r=[pp, gm], w=[V])
                    cx.op("dve", lambda e: e.scalar_tensor_tensor(out=V[:], in0=X[:], scalar=ALPHA, in1=V[:], op0=ALU.mult, op1=ALU.add), r=[X, V], w=[V])
                    tmp = tmps.next()
                    ln_stats(P, V, tmp)
                    cx.op("act", lambda e: e.activation(out=V[:], in_=V[:], func=AF.Identity, scale=tmp[2][:], bias=tmp[3][:]), r=[V, tmp[2], tmp[3]], w=[V])
                    cx.op("dve", lambda e: e.tensor_tensor(out=V[:], in0=V[:], in1=l1w[:], op=ALU.mult), r=[V, l1w], w=[V])
                    cx.op("dve", lambda e: e.tensor_tensor(out=X[:], in0=V[:], in1=l1b[:], op=ALU.add), r=[V, l1b], w=[X])
                    cx.dma("sp", x1s[t * 128:(t + 1) * 128, :], X[:], r=[X])
                    ln_stats(P, X, tmp)
                    U = ut.next()
                    cx.op("act", lambda e: e.activation(out=U[:], in_=X[:], func=AF.Identity, scale=tmp[2][:], bias=tmp[3][:]), r=[X, tmp[2], tmp[3]], w=[U])
                    cx.op("dve", lambda e: e.tensor_tensor(out=U[:], in0=U[:], in1=sc1f[:], op=ALU.mult), r=[U, sc1f], w=[U])
                    cx.op("dve", lambda e: e.tensor_tensor(out=U[:], in0=U[:], in1=shf[:], op=ALU.add), r=[U, shf], w=[U])
                    UF, UB = uTf.next(), uTb.next()
                    for g in range(4):
                        pt = ptr.next()
                        for q in range(4):
                            kc = g * 4 + q
                            cx.op("pe", lambda e, kc=kc, q=q: e.transpose(out=pt[:, q * 128:(q + 1) * 128], in_=U[:, kc * 128:(kc + 1) * 128], identity=ident_f[:]),
                                  r=[U, ident_f], w=[pt], sig=(q == 3))
                        cx.op("act", lambda e, g=g: e.copy(out=UF[:, g * 4:(g + 1) * 4, :], in_=pt[:].rearrange("p (q n) -> p q n", q=4)), r=[pt], w=[UF])
                        cx.op("dve", lambda e, g=g: e.tensor_copy(out=UB[:, g * 4:(g + 1) * 4, :], in_=pt[:].rearrange("p (q n) -> p q n", q=4)), r=[pt], w=[UB])
                    cx.dma("sp", u2Ts.rearrange("k p t -> p k t")[:, :, t * 128:(t + 1) * 128], UB[:], r=[UB])
                    pl = plg.next()
                    for kc in range(16):
                        cx.op("pe", lambda e, kc=kc: e.matmul(pl[:, 0:32], UF[:, kc, :], wr[:, kc, :], start=(kc == 0), stop=(kc == 15)),
                              r=[UF, wr], w=[pl], sig=(kc == 15))
                    L, M8, NM, MK, EX, SM, GT = lg.next(), m8.next(), nmx.next(), msk.next(), ex.next(), sm.next(), gt.next()
                    cx.op("dve", lambda e: e.tensor_tensor(out=L[:], in0=pl[:, 0:32], in1=brb[:], op=ALU.add), r=[pl, brb], w=[L])
                    cx.op("dve", lambda e: e.max(out=M8[:], in_=L[:]), r=[L], w=[M8])
                    cx.op("dve", lambda e: e.tensor_scalar(out=NM[:], in0=M8[:, 0:1], scalar1=-1.0, scalar2=None, op0=ALU.mult), r=[M8], w=[NM])
                    cx.op("dve", lambda e: e.tensor_scalar(out=MK[:], in0=L[:], scalar1=M8[:, 3:4], scalar2=None, op0=ALU.is_ge), r=[L, M8], w=[MK])
                    cx.op("act", lambda e: e.activation(out=EX[:], in_=L[:], func=AF.Exp, bias=NM[:], scale=1.0), r=[L, NM], w=[EX])
                    cx.op("dve", lambda e: e.tensor_tensor(out=EX[:], in0=EX[:], in1=MK[:], op=ALU.mult), r=[EX, MK], w=[EX])
                    cx.op("dve", lambda e: e.reduce_sum(out=SM[:], in_=EX[:], axis=AX.X), r=[EX], w=[SM])
                    cx.op("dve", lambda e: e.reciprocal(out=SM[:], in_=SM[:]), r=[SM], w=[SM])
                    cx.op("dve", lambda e: e.tensor_scalar(out=GT[:], in0=EX[:], scalar1=SM[:], scalar2=None, op0=ALU.mult), r=[EX, SM], w=[GT])
                    cx.dma("sp", Gs[t * 128:(t + 1) * 128, :], GT[:], r=[GT])

        if "moe" in phases:
            with Phase(cx) as P:
                u2 = P.sb([128, 16, 512], BF16, "u2")
                G = P.sb([128, 4, 32], F32, "G")
                GT = P.sb([32, 512], BF16, "GT")
                bdall = P.sb([32, D], BF16, "bdall")
                acc = P.sb([128, 4, D], F32, "acc")
                hT = [P.sb([128, 16, 512], BF16, "hT") for _ in range(2)]
                wgt = RR([P.sb([128, 16, 512], BF16, "wg") for _ in range(2)])
                wut = RR([P.sb([128, 16, 512], BF16, "wu") for _ in range(2)])
                wdt = RR([P.sb([128, 16, 512], BF16, "wd") for _ in range(2)])
                bg = [P.sb([128, 32], F32, "bg") for _ in range(3)]
                pg = RR([P.ps([128, 512], F32, "pg") for _ in range(2)])
                pu = RR([P.ps([128, 512], F32, "pu") for _ in range(2)])
                pyy = RR([P.ps([128, 512], F32, "pyy") for _ in range(2)])
                gs = RR([P.sb([128, 512], F32, "gs") for _ in range(2)])
                sgs = RR([P.sb([128, 512], F32, "sgs") for _ in range(2)])
                ls_ = RR([P.sb([128, 512], F32, "ls") for _ in range(2)])
                cx.dma("pool", bdall[0:NE, :], b_d[0:NE, :], w=[bdall])

                def GU(ex_, fb):
                    H = hT[ex_ % 2]
                    BG = bg[ex_ % 3]
                    if fb == 0:
                        cx.dma("sp", BG[:], bgu[ex_, :, :], w=[BG])
                    wg_v = w_g[ex_].rearrange("(kc p) f -> p kc f", p=128)
                    wu_v = w_u[ex_].rearrange("(kc p) f -> p kc f", p=128)
                    WG, WU = wgt.next(), wut.next()
                    cx.dma("pool", WG[:], wg_v[:, :, fb * 512:(fb + 1) * 512], w=[WG])
                    cx.dma("pool", WU[:], wu_v[:, :, fb * 512:(fb + 1) * 512], w=[WU])
                    for sub in range(4):
                        fc = fb * 4 + sub
                        p1, p2 = pg.next(), pu.next()
                        for kc in range(16):
                            cx.op("pe", lambda e, kc=kc: e.matmul(p1[:, :], WG[:, kc, sub * 128:(sub + 1) * 128], u2[:, kc, :], start=(kc == 0), stop=(kc == 15)),
                                  r=[WG, u2], w=[p1], sig=(kc == 15))
                        for kc in range(16):
                            cx.op("pe", lambda e, kc=kc: e.matmul(p2[:, :], WU[:, kc, sub * 128:(sub + 1) * 128], u2[:, kc, :], start=(kc == 0), stop=(kc == 15)),
                                  r=[WU, u2], w=[p2], sig=(kc == 15))
                        GS, SG, LS = gs.next(), sgs.next(), ls_.next()
                        cx.op("dve", lambda e: e.tensor_scalar(out=GS[:], in0=p1[:], scalar1=BG[:, fc:fc + 1], scalar2=7.0, op0=ALU.add, op1=ALU.min),
                              r=[p1, BG], w=[GS])
                        cx.op("act", lambda e: e.activation(out=SG[:], in_=GS[:], func=AF.Sigmoid, scale=1.702), r=[GS], w=[SG])
                        cx.op("dve", lambda e: e.tensor_scalar(out=LS[:], in0=p2[:], scalar1=BG[:, 16 + fc:17 + fc], scalar2=7.0, op0=ALU.add, op1=ALU.min),
                              r=[p2, BG], w=[LS])
                        cx.op("dve", lambda e: e.tensor_scalar(out=LS[:], in0=LS[:], scalar1=-7.0, scalar2=1.0, op0=ALU.max, op1=ALU.add), r=[LS], w=[LS])
                        cx.op("dve", lambda e: e.tensor_tensor(out=GS[:], in0=GS[:], in1=SG[:], op=ALU.mult), r=[GS, SG], w=[GS])
                        cx.op("dve", lambda e: e.tensor_tensor(out=H[:, fc, :], in0=GS[:], in1=LS[:], op=ALU.mult), r=[GS, LS], w=[H])

                def DN(ex_):
                    H = hT[ex_ % 2]
                    wd_v = w_d[ex_].rearrange("(kc p) f -> p kc f", p=128)
                    for db in range(4):
                        WD = wdt.next()
                        cx.dma("pool", WD[:], wd_v[:, :, db * 512:(db + 1) * 512], w=[WD])
                        for t in range(4):
                            pp = pyy.next()
                            for fc in range(16):
                                cx.op("pe", lambda e, fc=fc: e.matmul(pp[:, :], H[:, fc, t * 128:(t + 1) * 128], WD[:, fc, :], start=(fc == 0), stop=(fc == 15)),
                                      r=[H, WD], w=[pp], sig=(fc == 15))
                            cx.op("dve", lambda e: e.scalar_tensor_tensor(out=acc[:, t, db * 512:(db + 1) * 512], in0=pp[:], scalar=G[:, t, ex_:ex_ + 1],
                                                                         in1=acc[:, t, db * 512:(db + 1) * 512], op0=ALU.mult, op1=ALU.add),
                                  r=[pp, G, acc], w=[acc])

                for hf in range(2):
                    cx.dma("sp", u2[:], u2Ts.rearrange("k p t -> p k t")[:, :, hf * 512:(hf + 1) * 512], w=[u2])
                    cx.dma("sp", G[:], Gs.rearrange("(t p) e -> p t e", p=128)[:, hf * 4:(hf + 1) * 4, :], w=[G])
                    ptg = pg.next()
                    for t in range(4):
                        cx.op("pe", lambda e, t=t: e.transpose(out=ptg[0:NE, t * 128:(t + 1) * 128], in_=G[:, t, 0:NE], identity=ident_f[:]),
                              r=[G, ident_f], w=[ptg], sig=(t == 3))
                    cx.op("act", lambda e: e.copy(out=GT[0:NE, :], in_=ptg[0:NE, :]), r=[ptg], w=[GT])
                    for t in range(4):
                        for db in range(4):
                            pp = pyy.next()
                            cx.op("pe", lambda e: e.matmul(pp[:, :], GT[0:NE, t * 128:(t + 1) * 128], bdall[0:NE, db * 512:(db + 1) * 512], start=True, stop=True),
                                  r=[GT, bdall], w=[pp])
                            cx.op("dve", lambda e: e.tensor_copy(out=acc[:, t, db * 512:(db + 1) * 512], in_=pp[:]), r=[pp], w=[acc])
                    for fb in range(4):
                        GU(0, fb)
                    for ex_ in range(NE):
                        if ex_ + 1 < NE:
                            GU(ex_ + 1, 0)
                        DN(ex_)
                        if ex_ + 1 < NE:
                            for fb in range(1, 4):
                                GU(ex_ + 1, fb)
                    for t in range(4):
                        row0 = hf * 512 + t * 128
                        cx.dma("sp", y2s[row0:row0 + 128, :], acc[:, t, :], r=[acc])

        if "fin" in phases:
            with Phase(cx) as P:
                gf = load_bc(P, mod_bc(0, 5), "gf")
                l2w = load_bc(P, lnp[2:3, :].partition_broadcast(128), "l2w")
                l2b = load_bc(P, lnp[3:4, :].partition_broadcast(128), "l2b")
                xt = RR([P.sb([128, D], F32, "xt") for _ in range(2)])
                yt = RR([P.sb([128, D], F32, "yt") for _ in range(2)])
                tmps = RR([ln_tmp(P) for _ in range(2)])
                for t in range(8):
                    X, Y = xt.next(), yt.next()
                    row0 = t * 128
                    cx.dma("sp", X[:], x1s[row0:row0 + 128, :], w=[X])
                    cx.dma("sp", Y[:], y2s[row0:row0 + 128, :], w=[Y])
                    cx.op("dve", lambda e: e.tensor_tensor(out=Y[:], in0=Y[:], in1=gf[:], op=ALU.mult), r=[Y, gf], w=[Y])
                    cx.op("dve", lambda e: e.scalar_tensor_tensor(out=X[:], in0=X[:], scalar=ALPHA, in1=Y[:], op0=ALU.mult, op1=ALU.add),
                          r=[X, Y], w=[X])
                    tmp = tmps.next()
                    ln_stats(P, X, tmp)
                    cx.op("act", lambda e: e.activation(out=X[:], in_=X[:], func=AF.Identity, scale=tmp[2][:], bias=tmp[3][:]), r=[X, tmp[2], tmp[3]], w=[X])
                    cx.op("dve", lambda e: e.tensor_tensor(out=X[:], in0=X[:], in1=l2w[:], op=ALU.mult), r=[X, l2w], w=[X])
                    cx.op("dve", lambda e: e.tensor_tensor(out=X[:], in0=X[:], in1=l2b[:], op=ALU.add), r=[X, l2b], w=[X])
                    cx.dma("sp", out[row0:row0 + 128, :], X[:], r=[X])

        cx.barrier()
        PP.__exit__(None, None, None)
        build.ninstr = cx.ninstr
    return nc


def _consts():
    f32 = np.float32
    ident = np.eye(128, dtype=f32)
    perm = np.zeros((128, 128), f32)
    f = np.arange(128)
    j = f % 32
    partner = f - j + (j + 16) % 32
    perm[partner, f] = 1.0
    inv_freq = (10000.0 ** (-np.arange(16, dtype=np.float32) / 16)).astype(f32)
    tok = np.arange(S)
    rowp = (tok // 64).astype(f32)
    colp = (tok % 64).astype(f32)
    i64 = f % 64
    half = i64 // 32
    pos = np.where(half[:, None] == 0, rowp[None, :], colp[None, :]).astype(f32)
    ang = (pos * inv_freq[j % 16][:, None]).astype(f32)
    C = np.cos(ang).astype(f32)
    Sn = np.sin(ang).astype(f32)
    Sn = np.where((j < 16)[:, None], -Sn, Sn).astype(f32)
    return ident, perm, np.stack([C, Sn]).astype(f32)


def _nab(rel_bias):
    p = np.arange(128)
    dr = p // 64
    ck = p % 64
    cq = np.arange(64)
    cstart = np.clip(cq - 8, 0, 48)
    colok = (ck[:, None] >= cstart[None, :]) & (ck[:, None] < cstart[None, :] + 16)
    coff = np.clip(ck[:, None] - cq[None, :] + 15, 0, 30)
    out = np.full((NH, 128, 16, 64), NEG, np.float32)
    for idx in range(16):
        d = idx - 8 + dr
        rowok = np.abs(d) <= 7
        ok = colok & rowok[:, None]
        vals = rel_bias[:, np.clip(d + 7, 0, 14)[:, None], coff]
        out[:, :, idx, :] = np.where(ok[None], vals, NEG)
    return out


def _navalid(j):
    v = np.zeros((128, 16, 6), np.float32)
    p = np.arange(128)
    for i in range(16):
        lo, hi = min(i, 12), max(i + 8, 12)
        ms = list(range(lo // 2, (hi + 1) // 2))
        r = 16 * j + i
        r0 = min(max(r - 4, 0), 56)
        for s_, m in enumerate(ms):
            g = 16 * j - 4 + 2 * m + p // 64
            v[:, i, s_] = ((g >= r0) & (g < r0 + 8)).astype(np.float32)
    return v


def prep_inputs(inp, NE=NE_FULL):
    ident, perm, rope = _consts()
    f32 = np.float32
    A = lambda a: np.ascontiguousarray(a, dtype=f32)
    nab = _nab(np.asarray(inp["na_rel_bias"][0], f32))
    lamv = A(np.concatenate([inp["lam_q1"], inp["lam_k1"], inp["lam_q2"], inp["lam_k2"]], axis=0)).reshape(1, 256)
    lnp = A(np.concatenate([inp["ln1_w"], inp["ln1_b"], inp["ln2_w"], inp["ln2_b"]], axis=0))
    bgu = A(np.concatenate([inp["b_gate"][0].reshape(32, 16, 128).transpose(0, 2, 1),
                            inp["b_up"][0].reshape(32, 16, 128).transpose(0, 2, 1)], axis=2))[:NE]
    shared = dict(
        w_ada=A(inp["w_ada"][0]), b_ada=A(inp["b_ada"]), w_in=A(inp["w_in"][0]), nab=A(nab), lamv=lamv,
        wsub=A(inp["diff_subln_w"][0].reshape(128, 1)), w_proj_na=A(inp["w_proj_na"][0]), w_proj_diff=A(inp["w_proj_diff"][0]),
        w_out=A(inp["w_out"][0]), lnp=lnp, w_router=A(inp["w_router"][0]), b_router=A(inp["b_router"]),
        w_gate=A(inp["w_gate"][0][:NE]), w_up=A(inp["w_up"][0][:NE]), w_down=A(inp["w_down"][0][:NE]), bgu=bgu,
        b_down=A(inp["b_down"][0][:NE]), ropeK=A(rope), ident=ident, perm=perm,
    )
    maps = []
    for i in range(NCORE):
        b, j = i // 4, i % 4
        x = np.asarray(inp["x"][b], f32)
        xh = np.zeros((512, D), f32)
        top0 = 1024 * j - 256
        if top0 >= 0:
            xh[0:256] = x[top0:top0 + 256]
        bot0 = 1024 * j + 1024
        if bot0 + 256 <= S:
            xh[256:512] = x[bot0:bot0 + 256]
        cc = np.stack([np.asarray(inp["c"][b], f32).reshape(16, 128).T, np.asarray(inp["c_ctx"], f32).reshape(16, 128).T], axis=2)
        m = dict(shared)
        m.update(xb=A(x), xo=A(x[1024 * j:1024 * j + 1024]), xh=xh, ctx=A(inp["ctx"][b]), cc=A(cc),
                 navalid=_navalid(j), ropeQ=A(rope[:, :, 1024 * j:1024 * j + 1024]))
        maps.append(m)
    return maps


def kernel(**inputs):
    nc = build()
    maps = prep_inputs(inputs)
    res = run_bass_kernel_spmd(nc, maps, core_ids=list(range(NCORE)))
    outp = np.empty((2, S, D), np.float32)
    for i in range(NCORE):
        b, j = i // 4, i % 4
        outp[b, 1024 * j:1024 * j + 1024] = res.results[i]["out"]
    return outp
```

```python
import math
from contextlib import ExitStack
import numpy as np
import concourse.bass as bass
import concourse.mybir as mybir
from concourse.bass_utils import run_bass_kernel_spmd

F32 = mybir.dt.float32
BF16 = mybir.dt.bfloat16
AF = mybir.ActivationFunctionType
ALU = mybir.AluOpType
AX = mybir.AxisListType

D = 2048
S = 4096
NCORE = 8
TOK = 1024
CTX = 256
NH = 8
NE_FULL = 32
LN_EPS = 1e-6
RMS_EPS = 1e-5
ALPHA = 2.0 ** 0.25
LAM_INIT = 0.8 - 0.6 * math.exp(0.0)
NKEY_DF = S + CTX
NKEY_NA = 24 * 64 + CTX
NSLOT = 6
NEG = -30000.0


class Res:
    __slots__ = ("name", "w", "r", "psum")

    def __init__(self, name):
        self.name = name
        self.w = None
        self.r = {}
        self.psum = False


class Tile:
    def __init__(self, t, name):
        self.t = t
        self.res = Res(name)

    def __getitem__(self, k):
        return self.t[k]


class Eng:
    def __init__(self, name, h, is_pe=False):
        self.name = name
        self.h = h
        self.sem = "e_" + name
        self.cnt = 0
        self.seen = {}
        self.dk = 0
        self.dvals = [0] * NSLOT
        self.is_pe = is_pe


class Ctx:
    def __init__(self, nc, stack):
        self.nc = nc
        self.stack = stack
        self.E = {
            "pe": Eng("pe", nc.tensor, True),
            "act": Eng("act", nc.scalar),
            "dve": Eng("dve", nc.vector),
            "pool": Eng("pool", nc.gpsimd),
            "sp": Eng("sp", nc.sync),
        }
        self.sems = {}
        for e in self.E.values():
            self.sems[e.sem] = stack.enter_context(nc.semaphore(e.sem))
        for q in ("sp", "pool", "act"):
            for k in range(NSLOT):
                n = "d_%s%d" % (q, k)
                self.sems[n] = stack.enter_context(nc.semaphore(n))
        self.ninstr = 0
        import os
        self.limit = int(os.environ.get("K_LIMIT", "0"))
        self.trace = int(os.environ.get("K_TRACE", "0"))

    def skip(self):
        return self.limit and self.ninstr >= self.limit

    def _waits(self, E, reads, writes, extra=()):
        if self.skip():
            return
        waits = {}

        def need(tok):
            if tok is None:
                return
            sem, val = tok
            if E.is_pe and sem == E.sem:
                return
            if E.seen.get(sem, 0) >= val:
                return
            if waits.get(sem, 0) < val:
                waits[sem] = val

        for R in reads:
            need(R.w)
        for R in writes:
            need(R.w)
            for s_, v_ in R.r.items():
                need((s_, v_))
        for t in extra:
            need(t)
        for sem, val in waits.items():
            E.seen[sem] = val
            E.h.wait_ge(self.sems[sem], val)
            self.ninstr += 1

    @staticmethod
    def _res(xs):
        return [x.res if isinstance(x, Tile) else x for x in xs]

    def op(self, eng, fn, r=(), w=(), sig=True):
        E = self.E[eng]
        if self.skip():
            return None
        r = self._res(r)
        w = self._res(w)
        w = w + [R for R in r if R.psum and R not in w]
        r = [R for R in r if not R.psum]
        self._waits(E, r, w)
        inst = fn(E.h)
        self.ninstr += 1
        if self.trace:
            print("OP", self.ninstr, eng, fn.__code__.co_firstlineno)
        if sig:
            E.cnt += 1
            inst.then_inc(self.sems[E.sem], 1)
            tok = (E.sem, E.cnt)
        else:
            tok = (E.sem, E.cnt + 1)
        for R in r:
            if R.r.get(tok[0], 0) < tok[1]:
                R.r[tok[0]] = tok[1]
        for R in w:
            R.w = tok
            R.r = {}
        return inst

    def dma(self, q, out, in_, r=(), w=(), **kw):
        E = self.E[q]
        if self.skip():
            return None
        r = self._res(r)
        w = self._res(w)
        slot = E.dk % NSLOT
        E.dk += 1
        sem = "d_%s%d" % (q, slot)
        prev = E.dvals[slot]
        extra = [(sem, prev)] if prev > 0 else []
        self._waits(E, r, w, extra)
        inst = E.h.dma_start(out=out, in_=in_, **kw)
        self.ninstr += 1
        if self.trace:
            import sys as _s
            print("DMA", self.ninstr, q, _s._getframe(1).f_lineno)
        E.dvals[slot] = prev + 16
        inst.then_inc(self.sems[sem], 16)
        tok = (sem, prev + 16)
        for R in r:
            if R.r.get(tok[0], 0) < tok[1]:
                R.r[tok[0]] = tok[1]
        for R in w:
            R.w = tok
            R.r = {}
        return inst

    def barrier(self):
        lim, self.limit = self.limit, 0
        self._barrier()
        self.limit = lim

    def _barrier(self):
        toks = []
        for e in self.E.values():
            if e.cnt > 0:
                toks.append((e.sem, e.cnt))
            for k in range(NSLOT):
                if e.dvals[k] > 0:
                    toks.append(("d_%s%d" % (e.name, k), e.dvals[k]))
        for e in self.E.values():
            self._waits(e, [], [], toks)


class Phase:
    def __init__(self, cx):
        self.cx = cx
        self.st = ExitStack()
        self.n = 0

    def __enter__(self):
        self.st.__enter__()
        return self

    def __exit__(self, *a):
        self.cx.barrier()
        return self.st.__exit__(*a)

    def sb(self, shape, dt, name=None):
        self.n += 1
        name = (name or "t") + "_%d_%d" % (id(self) % 100000, self.n)
        return Tile(self.st.enter_context(self.cx.nc.sbuf_tensor(name, list(shape), dt)), name)

    def ps(self, shape, dt, name=None):
        self.n += 1
        name = (name or "p") + "_%d_%d" % (id(self) % 100000, self.n)
        t = Tile(self.st.enter_context(self.cx.nc.psum_tensor(name, list(shape), dt)), name)
        t.res.psum = True
        return t


class RR:
    def __init__(self, items):
        self.items = items
        self.i = 0

    def next(self):
        x = self.items[self.i % len(self.items)]
        self.i += 1
        return x


def build(NE=NE_FULL, dbg=False, phases=("mod", "proj", "na", "df", "merge", "out", "moe", "fin")):
    nc = bass.Bass("TRN2", target_bir_lowering=False)
    st = ExitStack()
    with st:
        cx = Ctx(nc, st)

        def din(name, shape, dt=F32):
            return nc.dram_tensor(name, list(shape), dt, kind="ExternalInput").ap()

        def dscr(name, shape, dt=BF16):
            return nc.dram_tensor(name, list(shape), dt, kind="ExternalOutput" if dbg else "Internal").ap()

        xb = din("xb", [S, D])
        xo = din("xo", [TOK, D])
        xh = din("xh", [512, D])
        ctxi = din("ctx", [CTX, D])
        cc = din("cc", [128, 16, 2])
        w_ada = din("w_ada", [D, 6 * D])
        b_ada = din("b_ada", [1, 6 * D])
        w_in = din("w_in", [D, 10240])
        nab = din("nab", [NH, 128, 16, 64])
        navalid = din("navalid", [128, 16, 6])
        lamv = din("lamv", [1, 256])
        wsub = din("wsub", [128, 1])
        w_pn = din("w_proj_na", [1024, D])
        w_pd = din("w_proj_diff", [1024, D])
        w_out = din("w_out", [D, D])
        lnp = din("lnp", [4, D])
        w_r = din("w_router", [D, 32])
        b_r = din("b_router", [1, 32])
        w_g = din("w_gate", [NE, D, D])
        w_u = din("w_up", [NE, D, D])
        w_d = din("w_down", [NE, D, D])
        bgu = din("bgu", [NE, 128, 32])
        b_d = din("b_down", [NE, D])
        ropeK = din("ropeK", [2, 128, S])
        ropeQ = din("ropeQ", [2, 128, TOK])
        ident_in = din("ident", [128, 128])
        perm_in = din("perm", [128, 128])
        out = nc.dram_tensor("out", [TOK, D], F32, kind="ExternalOutput").ap()

        modv = dscr("modv", [2, 6 * D], F32)
        kdfT = dscr("kdfT", [NH, 128, NKEY_DF])
        vdf = dscr("vdf", [NKEY_DF, 1024])
        knaT = dscr("knaT", [NH, 128, NKEY_NA])
        vna = dscr("vna", [NKEY_NA, 1024])
        qnaT = dscr("qnaT", [NH, 128, TOK])
        qdfT = dscr("qdfT", [NH, 128, TOK])
        gnaT = dscr("gnaT", [16, 128, TOK])
        gdfT = dscr("gdfT", [16, 128, TOK])
        onaT = dscr("onaT", [NH, 128, TOK])
        odfT = dscr("odfT", [NH, 128, TOK])
        mrgT = dscr("mrgT", [16, 128, TOK])
        x1s = dscr("x1s", [TOK, D], F32)
        u2Ts = dscr("u2Ts", [16, 128, TOK])
        Gs = dscr("Gs", [TOK, 32], F32)
        y2s = dscr("y2s", [TOK, D], F32)

        PP = Phase(cx)
        PP.__enter__()
        ident_f = PP.sb([128, 128], F32, "identf")
        ident_b = PP.sb([128, 128], BF16, "identb")
        perm_b = PP.sb([128, 128], BF16, "permb")
        ones_b = PP.sb([128, 128], BF16, "onesb")
        ones_f = PP.sb([128, 128], F32, "onesf")
        cx.dma("sp", ident_f[:], ident_in[:, :], w=[ident_f])
        cx.dma("pool", ident_b[:], ident_in[:, :], w=[ident_b])
        cx.dma("pool", perm_b[:], perm_in[:, :], w=[perm_b])
        cx.op("dve", lambda e: e.memset(ones_b[:], 1.0), w=[ones_b])
        cx.op("dve", lambda e: e.memset(ones_f[:], 1.0), w=[ones_f])
        eps_ln = PP.sb([128, 1], F32, "epsln")
        eps_rms = PP.sb([128, 1], F32, "epsrms")
        cx.op("dve", lambda e: e.memset(eps_ln[:], LN_EPS), w=[eps_ln])
        cx.op("dve", lambda e: e.memset(eps_rms[:], RMS_EPS), w=[eps_rms])

        w_in_v = w_in.rearrange("(kc p) n -> p kc n", p=128)

        if "mod" in phases:
            with Phase(cx) as P:
                ccs = P.sb([128, 32], F32, "ccs")
                sg = P.sb([128, 32], F32, "sg")
                sil = P.sb([128, 16, 2], BF16, "sil")
                bsb = P.sb([2, 6 * D], F32, "bsb")
                msb = P.sb([2, 6 * D], F32, "msb")
                wt = [P.sb([128, 16, 512], BF16, "wada") for _ in range(3)]
                pb = [P.ps([128, 512], F32, "pm") for _ in range(2)]
                cx.dma("sp", ccs[:], cc.rearrange("p k t -> p (k t)"), w=[ccs])
                cx.dma("sp", bsb[0:1, :], b_ada[:, :], w=[bsb])
                cx.dma("sp", bsb[1:2, :], b_ada[:, :], w=[bsb])
                cx.op("act", lambda e: e.activation(out=sg[:], in_=ccs[:], func=AF.Sigmoid), r=[ccs], w=[sg])
                cx.op("dve", lambda e: e.tensor_tensor(out=sil[:].rearrange("p k t -> p (k t)"), in0=ccs[:], in1=sg[:], op=ALU.mult),
                      r=[ccs, sg], w=[sil])
                w_ada_v = w_ada.rearrange("(kc p) n -> p kc n", p=128)
                for n in range(24):
                    W = wt[n % 3]
                    cx.dma("pool", W[:], w_ada_v[:, :, n * 512:(n + 1) * 512], w=[W])
                    pp = pb[n % 2]
                    for kc in range(16):
                        cx.op("pe", lambda e, kc=kc: e.matmul(pp[0:2, :], sil[:, kc, :], W[:, kc, :], start=(kc == 0), stop=(kc == 15)),
                              r=[sil, W], w=[pp], sig=(kc == 15))
                    cx.op("dve", lambda e: e.tensor_tensor(out=msb[0:2, n * 512:(n + 1) * 512], in0=pp[0:2, :],
                                                          in1=bsb[0:2, n * 512:(n + 1) * 512], op=ALU.add),
                          r=[pp, bsb], w=[msb])
                cx.dma("sp", modv[:, :], msb[0:2, :], r=[msb])

        def mod_bc(row, idx):
            return modv[row:row + 1, idx * D:(idx + 1) * D].partition_broadcast(128)

        def load_bc(P, src_ap, name, plus1=False):
            t = P.sb([128, D], F32, name)
            cx.dma("sp", t[:], src_ap, w=[t])
            if plus1:
                cx.op("dve", lambda e: e.tensor_scalar(out=t[:], in0=t[:], scalar1=1.0, scalar2=None, op0=ALU.add), r=[t], w=[t])
            return t

        def ln_stats(P, src, tmp):
            stt, mv, rstd, nmr = tmp
            for c in range(4):
                cx.op("dve", lambda e, c=c: e.bn_stats(out=stt[:, c, :], in_=src[:, c * 512:(c + 1) * 512]), r=[src], w=[stt])
            cx.op("dve", lambda e: e.bn_aggr(out=mv[:], in_=stt[:].rearrange("p a b -> p (a b)")), r=[stt], w=[mv])
            cx.op("act", lambda e: e.activation(out=rstd[:], in_=mv[:, 1:2], func=AF.Sqrt, bias=eps_ln[:], scale=1.0), r=[mv, eps_ln], w=[rstd])
            cx.op("dve", lambda e: e.reciprocal(out=rstd[:], in_=rstd[:]), r=[rstd], w=[rstd])
            cx.op("dve", lambda e: e.tensor_scalar(out=nmr[:], in0=mv[:, 0:1], scalar1=rstd[:], scalar2=-1.0, op0=ALU.mult, op1=ALU.mult),
                  r=[mv, rstd], w=[nmr])

        def ln_tmp(P):
            return (P.sb([128, 4, 6], F32, "stt"), P.sb([128, 2], F32, "mv"), P.sb([128, 1], F32, "rstd"), P.sb([128, 1], F32, "nmr"))

        if "proj" in phases:
            with Phase(cx) as P:
                sc1_m = load_bc(P, mod_bc(0, 1), "sc1m", True)
                sh_m = load_bc(P, mod_bc(0, 0), "shm")
                sc1_c = load_bc(P, mod_bc(1, 1), "sc1c", True)
                sh_c = load_bc(P, mod_bc(1, 0), "shc")
                xt = [P.sb([128, D], F32, "xt") for _ in range(2)]
                zt = [P.sb([128, D], F32, "zt") for _ in range(2)]
                ut = [P.sb([128, D], BF16, "ut") for _ in range(2)]
                tmps = [ln_tmp(P) for _ in range(2)]
                uT = [P.sb([128, 16, 512], BF16, "uT") for _ in range(2)]
                wts = RR([P.sb([128, 16, 512], BF16, "win") for _ in range(3)])
                ptr = RR([P.ps([128, 1024], BF16, "ptr") for _ in range(2)])
                pmm = RR([P.ps([128, 512], F32, "pmm") for _ in range(4)])
                prp = pmm
                osb = RR([P.sb([128, 512], BF16, "osb") for _ in range(4)])
                xsb = RR([P.sb([128, 512], BF16, "xsb") for _ in range(2)])
                t1s = RR([P.sb([128, 512], F32, "t1s") for _ in range(2)])
                rC = RR([P.sb([128, 512], F32, "rC") for _ in range(2)])
                rS = RR([P.sb([128, 512], F32, "rS") for _ in range(2)])
                tcount = [0]

                def ln_gen(src_rows, ntok, sc1, sh, U):
                    for t in range(ntok // 128):
                        k = tcount[0] % 2
                        tcount[0] += 1
                        X, Z, Ub, tmp = xt[k], zt[k], ut[k], tmps[k]
                        cx.dma("sp", X[:], src_rows[t * 128:(t + 1) * 128, :], w=[X])
                        ln_stats(P, X, tmp)
                        cx.op("act", lambda e: e.activation(out=Z[:], in_=X[:], func=AF.Identity, scale=tmp[2][:], bias=tmp[3][:]),
                              r=[X, tmp[2], tmp[3]], w=[Z])
                        cx.op("dve", lambda e: e.tensor_tensor(out=Z[:], in0=Z[:], in1=sc1[:], op=ALU.mult), r=[Z, sc1], w=[Z])
                        cx.op("dve", lambda e: e.tensor_tensor(out=Ub[:], in0=Z[:], in1=sh[:], op=ALU.add), r=[Z, sh], w=[Ub])
                        yield 1
                        for g in range(2):
                            pt = ptr.next()
                            for q in range(8):
                                kc = g * 8 + q
                                cx.op("pe", lambda e, kc=kc, q=q: e.transpose(out=pt[:, q * 128:(q + 1) * 128], in_=Ub[:, kc * 128:(kc + 1) * 128],
                                                                            identity=ident_b[:]),
                                      r=[Ub, ident_b], w=[pt], sig=(q == 7))
                            cx.op("act", lambda e, g=g: e.copy(out=U[:, g * 8:(g + 1) * 8, t * 128:(t + 1) * 128],
                                                              in_=pt[:].rearrange("p (q n) -> p q n", q=8)),
                                  r=[pt], w=[U])
                        yield 2

                hk = [None]

                def step():
                    if hk[0] is not None:
                        if next(hk[0], None) is None:
                            hk[0] = None

                def drain():
                    while hk[0] is not None:
                        step()

                def proj(U, ntok, col0, ncols, mode, dest, rope=None):
                    for cb in range(ncols // 512):
                        W = wts.next()
                        c0 = col0 + cb * 512
                        cx.dma("pool", W[:], w_in_v[:, :, c0:c0 + 512], w=[W])
                        step()
                        if mode == "FM":
                            for sub in range(4):
                                pm = pmm.next()
                                for kc in range(16):
                                    cx.op("pe", lambda e, kc=kc: e.matmul(pm[:, 0:ntok], W[:, kc, sub * 128:(sub + 1) * 128], U[:, kc, 0:ntok],
                                                                         start=(kc == 0), stop=(kc == 15)),
                                          r=[W, U], w=[pm], sig=(kc == 15))
                                ci = cb * 4 + sub
                                if rope is None:
                                    O = osb.next()
                                    cx.op("act", lambda e: e.copy(out=O[:, 0:ntok], in_=pm[:, 0:ntok]), r=[pm], w=[O])
                                    dest(ci, O)
                                else:
                                    Cc, Ss = rope
                                    Xs = xsb.next()
                                    cx.op("act", lambda e: e.copy(out=Xs[:, 0:ntok], in_=pm[:, 0:ntok]), r=[pm], w=[Xs])
                                    pr = prp.next()
                                    cx.op("pe", lambda e: e.matmul(pr[:, 0:ntok], perm_b[:], Xs[:, 0:ntok], start=True, stop=True),
                                          r=[perm_b, Xs], w=[pr])
                                    T1 = t1s.next()
                                    cx.op("dve", lambda e: e.tensor_tensor(out=T1[:, 0:ntok], in0=Xs[:, 0:ntok], in1=Cc[:, 0:ntok], op=ALU.mult),
                                          r=[Xs, Cc], w=[T1])
                                    T2 = t1s.next()
                                    cx.op("dve", lambda e: e.tensor_tensor(out=T2[:, 0:ntok], in0=pr[:, 0:ntok], in1=Ss[:, 0:ntok], op=ALU.mult),
                                          r=[pr, Ss], w=[T2])
                                    O = osb.next()
                                    cx.op("dve", lambda e: e.tensor_tensor(out=O[:, 0:ntok], in0=T1[:, 0:ntok], in1=T2[:, 0:ntok], op=ALU.add),
                                          r=[T1, T2], w=[O])
                                    dest(ci, O)
                        else:
                            for t in range(ntok // 128):
                                pm = pmm.next()
                                for kc in range(16):
                                    cx.op("pe", lambda e, kc=kc: e.matmul(pm[:, :], U[:, kc, t * 128:(t + 1) * 128], W[:, kc, :],
                                                                         start=(kc == 0), stop=(kc == 15)),
                                          r=[W, U], w=[pm], sig=(kc == 15))
                                O = osb.next()
                                cx.op("act", lambda e: e.copy(out=O[:], in_=pm[:]), r=[pm], w=[O])
                                dest(t, cb, O)
                        step()

                def st_fm(dst, tok0, ntok):
                    return lambda ci, O: cx.dma("sp", dst[ci, :, tok0:tok0 + ntok], O[:, 0:ntok], r=[O])

                def st_tm(dst, row0):
                    return lambda t, cb, O: cx.dma("sp", dst[row0 + t * 128:row0 + (t + 1) * 128, cb * 512:(cb + 1) * 512], O[:], r=[O])

                ub = [0]

                def nextU():
                    ub[0] += 1
                    return uT[ub[0] % 2]

                def st_halo_fm(ci, O):
                    cx.dma("sp", knaT[ci, :, 0:256], O[:, 0:256], r=[O])
                    cx.dma("sp", knaT[ci, :, 1280:1536], O[:, 256:512], r=[O])

                def st_halo_tm(t, cb, O):
                    row0 = t * 128 if t < 2 else 1280 + (t - 2) * 128
                    cx.dma("sp", vna[row0:row0 + 128, cb * 512:(cb + 1) * 512], O[:], r=[O])

                blocks = []

                def s1_work(blk):
                    def f(U):
                        Cc, Ss = rC.next(), rS.next()
                        cx.dma("sp", Cc[:], ropeK[0, :, blk * 512:(blk + 1) * 512], w=[Cc])
                        cx.dma("sp", Ss[:], ropeK[1, :, blk * 512:(blk + 1) * 512], w=[Ss])
                        proj(U, 512, 2048, 1024, "FM", st_fm(kdfT, blk * 512, 512), rope=(Cc, Ss))
                        proj(U, 512, 3072, 1024, "TM", st_tm(vdf, blk * 512))
                    return f

                def s2_work(U):
                    proj(U, CTX, 0, 1024, "FM", st_fm(knaT, 1536, CTX))
                    proj(U, CTX, 1024, 1024, "TM", st_tm(vna, 1536))
                    proj(U, CTX, 2048, 1024, "FM", st_fm(kdfT, S, CTX))
                    proj(U, CTX, 3072, 1024, "TM", st_tm(vdf, S))

                def s3_work(U):
                    proj(U, 512, 0, 1024, "FM", st_halo_fm)
                    proj(U, 512, 1024, 1024, "TM", st_halo_tm)

                def s4_work(blk):
                    def f(U):
                        Cc, Ss = rC.next(), rS.next()
                        cx.dma("sp", Cc[:], ropeQ[0, :, blk * 512:(blk + 1) * 512], w=[Cc])
                        cx.dma("sp", Ss[:], ropeQ[1, :, blk * 512:(blk + 1) * 512], w=[Ss])
                        proj(U, 512, 0, 1024, "FM", st_fm(knaT, 256 + blk * 512, 512))
                        proj(U, 512, 1024, 1024, "TM", st_tm(vna, 256 + blk * 512))
                        proj(U, 512, 4096, 1024, "FM", st_fm(qnaT, blk * 512, 512))
                        proj(U, 512, 5120, 1024, "FM", st_fm(qdfT, blk * 512, 512), rope=(Cc, Ss))
                        proj(U, 512, 6144, 2048, "FM", st_fm(gnaT, blk * 512, 512))
                        proj(U, 512, 8192, 2048, "FM", st_fm(gdfT, blk * 512, 512))
                    return f

                for blk in range(S // 512):
                    blocks.append((xb[blk * 512:(blk + 1) * 512, :], 512, sc1_m, sh_m, s1_work(blk)))
                blocks.append((ctxi, CTX, sc1_c, sh_c, s2_work))
                blocks.append((xh, 512, sc1_m, sh_m, s3_work))
                for blk in range(2):
                    blocks.append((xo[blk * 512:(blk + 1) * 512, :], 512, sc1_m, sh_m, s4_work(blk)))
                hk[0] = ln_gen(blocks[0][0], blocks[0][1], blocks[0][2], blocks[0][3], uT[0])
                drain()
                for bi, (src, ntok, sc1, sh, work) in enumerate(blocks):
                    if bi + 1 < len(blocks):
                        nb_ = blocks[bi + 1]
                        hk[0] = ln_gen(nb_[0], nb_[1], nb_[2], nb_[3], uT[(bi + 1) % 2])
                    work(uT[bi % 2])
                    drain()

        PW = Phase(cx)
        PW.__enter__()
        pre = {}
        if "out" in phases:
            pre["wo"] = PW.sb([128, 16, D], BF16, "wo")
            cx.dma("pool", pre["wo"][:], w_out.rearrange("(kc p) n -> p kc n", p=128), w=[pre["wo"]])
        PW2 = Phase(cx)
        PW2.__enter__()
        if "merge" in phases:
            pre["wpn"] = PW2.sb([128, 8, D], BF16, "wpn")
            pre["wpd"] = PW2.sb([128, 8, D], BF16, "wpd")
            cx.dma("pool", pre["wpn"][:], w_pn.rearrange("(h p) n -> p h n", p=128), w=[pre["wpn"]])
            cx.dma("pool", pre["wpd"][:], w_pd.rearrange("(h p) n -> p h n", p=128), w=[pre["wpd"]])

        if "na" in phases:
            with Phase(cx) as P:
                val = P.sb([128, 16, 6], F32, "val")
                cx.dma("sp", val[:], navalid[:, :, :], w=[val])
                kT = [P.sb([128, NKEY_NA], BF16, "kT") for _ in range(2)]
                vv = [P.sb([128, 14, 128], BF16, "vv") for _ in range(2)]
                qT = [P.sb([128, TOK], BF16, "qT") for _ in range(2)]
                nbf = [P.sb([128, 16, 64], F32, "nbf") for _ in range(2)]
                EB = [P.sb([128, 16, 64], BF16, "EB") for _ in range(2)]
                oT = [P.sb([128, TOK], BF16, "oT") for _ in range(2)]
                pS = RR([P.ps([128, 512], F32, "pS") for _ in range(2)])
                pO = RR([P.ps([128, 512], F32, "pO") for _ in range(2)])
                pZ = RR([P.ps([128, 512], F32, "pZ") for _ in range(2)])
                esb = RR([P.sb([128, 512], BF16, "esb") for _ in range(3)])
                e2 = RR([P.sb([128, 512], BF16, "e2") for _ in range(3)])
                rsb = RR([P.sb([128, 64], F32, "rsb") for _ in range(2)])
                scale = 128 ** -0.5
                def na_load(h):
                    K_, V_, Q_, NB_, EB_, O_ = kT[h % 2], vv[h % 2], qT[h % 2], nbf[h % 2], EB[h % 2], oT[h % 2]
                    cx.dma("sp", K_[:], knaT[h, :, :], w=[K_])
                    cx.dma("sp", V_[:], vna.rearrange("(c p) d -> p c d", p=128)[:, :, h * 128:(h + 1) * 128], w=[V_])
                    cx.dma("sp", Q_[:], qnaT[h, :, :], w=[Q_])
                    cx.dma("sp", NB_[:], nab[h, :, :, :], w=[NB_])
                    cx.op("act", lambda e: e.activation(out=EB_[:], in_=NB_[:], func=AF.Exp), r=[NB_], w=[EB_])

                def na_A(h, i):
                    K_, V_, Q_, NB_, EB_, O_ = kT[h % 2], vv[h % 2], qT[h % 2], nbf[h % 2], EB[h % 2], oT[h % 2]
                    lo, hi = min(i, 12), max(i + 8, 12)
                    ms = list(range(lo // 2, (hi + 1) // 2))
                    nl = len(ms)
                    chunks = ms + [12, 13]
                    ps = pS.next()
                    q = Q_[:, i * 64:(i + 1) * 64]
                    for ci, m in enumerate(chunks):
                        cx.op("pe", lambda e, ci=ci, m=m: e.matmul(ps[:, ci * 64:(ci + 1) * 64], K_[:, m * 128:(m + 1) * 128], q,
                                                                   start=True, stop=True),
                              r=[K_, Q_], w=[ps], sig=(ci == len(chunks) - 1))
                    nc_ = len(chunks)
                    E1 = esb.next()
                    cx.op("act", lambda e: e.activation(out=E1[:, 0:nc_ * 64], in_=ps[:, 0:nc_ * 64], func=AF.Exp, scale=scale), r=[ps], w=[E1])
                    idx0 = 2 * ms[0] - i - 4 + 8
                    E2 = e2.next()
                    cx.op("dve", lambda e: e.tensor_tensor(out=E2[:, 0:nl * 64].rearrange("p (c n) -> p c n", n=64),
                                                          in0=E1[:, 0:nl * 64].rearrange("p (c n) -> p c n", n=64),
                                                          in1=EB_[:, idx0:idx0 + 2 * nl - 1:2, :], op=ALU.mult),
                          r=[E1, EB_], w=[E2])
                    cx.op("dve", lambda e: e.tensor_tensor(out=E1[:, 0:nl * 64].rearrange("p (c n) -> p c n", n=64),
                                                          in0=E2[:, 0:nl * 64].rearrange("p (c n) -> p c n", n=64),
                                                          in1=val[:, i, 0:nl].unsqueeze(2).to_broadcast([128, nl, 64]), op=ALU.mult),
                          r=[E2, val], w=[E1])
                    return (chunks, E1)

                def na_B(h, i, st_):
                    K_, V_, Q_, NB_, EB_, O_ = kT[h % 2], vv[h % 2], qT[h % 2], nbf[h % 2], EB[h % 2], oT[h % 2]
                    chunks, E1 = st_
                    po, pz = pO.next(), pZ.next()
                    for ci, m in enumerate(chunks):
                        last = ci == len(chunks) - 1
                        cx.op("pe", lambda e, ci=ci, m=m: e.matmul(po[:, 0:64], V_[:, m, :], E1[:, ci * 64:(ci + 1) * 64],
                                                                   start=(ci == 0), stop=last), r=[V_, E1], w=[po], sig=False)
                        cx.op("pe", lambda e, ci=ci: e.matmul(pz[:, 0:64], ones_b[:], E1[:, ci * 64:(ci + 1) * 64],
                                                              start=(ci == 0), stop=last), r=[ones_b, E1], w=[pz], sig=last)
                    R_ = rsb.next()
                    cx.op("dve", lambda e: e.reciprocal(out=R_[:], in_=pz[:, 0:64]), r=[pz, po], w=[R_])
                    cx.op("dve", lambda e: e.tensor_tensor(out=O_[:, i * 64:(i + 1) * 64], in0=po[:, 0:64], in1=R_[:], op=ALU.mult),
                          r=[po, R_], w=[O_])
                    if i == 15:
                        cx.dma("sp", onaT[h, :, :], O_[:], r=[O_])

                its = [(h, i) for h in range(NH) for i in range(16)]
                pend = []
                for n_, (h, i) in enumerate(its):
                    if i == 0:
                        na_load(h)
                    pend.append((h, i, na_A(h, i)))
                    if len(pend) > 1:
                        na_B(*pend.pop(0))
                while pend:
                    na_B(*pend.pop(0))

        if "df" in phases:
            with Phase(cx) as P:
                lv = P.sb([128, 4, 64], F32, "lv")
                lp = P.sb([128, 2, 64], F32, "lp")
                ls = P.sb([128, 2], F32, "ls")
                le = P.sb([128, 2], F32, "le")
                nlam = P.sb([128, 1], F32, "nlam")
                wsc = P.sb([128, 1], F32, "wsc")
                cx.dma("sp", lv[:].rearrange("p a d -> p (a d)"), lamv[0:1, :].partition_broadcast(128), w=[lv])
                cx.dma("sp", wsc[:], wsub[:, :], w=[wsc])
                cx.op("dve", lambda e: e.tensor_tensor(out=lp[:, 0, :], in0=lv[:, 0, :], in1=lv[:, 1, :], op=ALU.mult), r=[lv], w=[lp])
                cx.op("dve", lambda e: e.tensor_tensor(out=lp[:, 1, :], in0=lv[:, 2, :], in1=lv[:, 3, :], op=ALU.mult), r=[lv], w=[lp])
                cx.op("dve", lambda e: e.reduce_sum(out=ls[:], in_=lp[:], axis=AX.X), r=[lp], w=[ls])
                cx.op("act", lambda e: e.activation(out=le[:], in_=ls[:], func=AF.Exp), r=[ls], w=[le])
                cx.op("dve", lambda e: e.tensor_scalar(out=nlam[:], in0=le[:, 1:2], scalar1=le[:, 0:1], scalar2=-LAM_INIT, op0=ALU.subtract, op1=ALU.add),
                      r=[le], w=[nlam])
                kT = [P.sb([128, NKEY_DF], BF16, "kT") for _ in range(2)]
                vv = [P.sb([128, 34, 128], BF16, "vv") for _ in range(2)]
                qT = [P.sb([128, TOK], BF16, "qT") for _ in range(2)]
                oT = [P.sb([128, TOK], BF16, "oT") for _ in range(2)]
                pS = RR([P.ps([128, 512], F32, "pS") for _ in range(4)])
                pO = [P.ps([128, 512], F32, "pO") for _ in range(2)]
                pZ = [P.ps([128, 512], F32, "pZ") for _ in range(2)]
                esb = RR([P.sb([128, 512], BF16, "esb") for _ in range(4)])
                r0 = P.sb([128, 512], F32, "r0")
                r1 = P.sb([128, 512], F32, "r1")
                t0 = P.sb([128, 512], F32, "t0")
                t1 = P.sb([128, 512], F32, "t1")
                of = P.sb([128, 512], F32, "of")
                sq = P.sb([128, 512], F32, "sq")
                rs = P.sb([128, 512], F32, "rs")
                NKC = NKEY_DF // 128

                def df_load(h):
                    K_, V_, Q_, O_ = kT[h % 2], vv[h % 2], qT[h % 2], oT[h % 2]
                    cx.dma("sp", K_[:], kdfT[h, :, :], w=[K_])
                    cx.dma("sp", V_[:], vdf.rearrange("(c p) d -> p c d", p=128)[:, :, h * 128:(h + 1) * 128], w=[V_])
                    cx.dma("sp", Q_[:], qdfT[h, :, :], w=[Q_])

                def df_A(h, qb, kc):
                    K_, V_, Q_, O_ = kT[h % 2], vv[h % 2], qT[h % 2], oT[h % 2]
                    pss = [pS.next(), pS.next()]
                    for m in range(2):
                        cx.op("pe", lambda e, m=m: e.matmul(pss[m][:, :], K_[m * 64:(m + 1) * 64, kc * 128:(kc + 1) * 128],
                                                            Q_[m * 64:(m + 1) * 64, qb * 512:(qb + 1) * 512], start=True, stop=True),
                              r=[K_, Q_], w=[pss[m]])
                    Es = [esb.next(), esb.next()]
                    for m in range(2):
                        cx.op("act", lambda e, m=m: e.activation(out=Es[m][:], in_=pss[m][:], func=AF.Exp, scale=0.125), r=[pss[m]], w=[Es[m]])
                    return Es

                def df_B(h, qb, kc, m, E1):
                    K_, V_, Q_, O_ = kT[h % 2], vv[h % 2], qT[h % 2], oT[h % 2]
                    last = kc == NKC - 1
                    cx.op("pe", lambda e: e.matmul(pO[m][:, :], V_[:, kc, :], E1[:], start=(kc == 0), stop=last),
                          r=[V_, E1], w=[pO[m]], sig=False)
                    cx.op("pe", lambda e: e.matmul(pZ[m][:, :], ones_b[:], E1[:], start=(kc == 0), stop=last),
                          r=[ones_b, E1], w=[pZ[m]], sig=last)
                    if not (last and m == 1):
                        return
                    cx.op("dve", lambda e: e.reciprocal(out=r0[:], in_=pZ[0][:]), r=[pZ[0]], w=[r0])
                    cx.op("dve", lambda e: e.reciprocal(out=r1[:], in_=pZ[1][:]), r=[pZ[1]], w=[r1])
                    cx.op("dve", lambda e: e.tensor_scalar(out=r1[:], in0=r1[:], scalar1=nlam[:], scalar2=None, op0=ALU.mult), r=[r1, nlam], w=[r1])
                    cx.op("dve", lambda e: e.tensor_tensor(out=t0[:], in0=pO[0][:], in1=r0[:], op=ALU.mult), r=[pO[0], r0], w=[t0])
                    cx.op("dve", lambda e: e.tensor_tensor(out=t1[:], in0=pO[1][:], in1=r1[:], op=ALU.mult), r=[pO[1], r1], w=[t1])
                    cx.op("dve", lambda e: e.tensor_tensor(out=of[:], in0=t0[:], in1=t1[:], op=ALU.add), r=[t0, t1], w=[of])
                    cx.op("act", lambda e: e.activation(out=sq[:], in_=of[:], func=AF.Square), r=[of], w=[sq])
                    ps = pS.next()
                    cx.op("pe", lambda e: e.matmul(ps[:, :], ones_f[:], sq[:], start=True, stop=True), r=[ones_f, sq], w=[ps])
                    cx.op("act", lambda e: e.activation(out=rs[:], in_=ps[:], func=AF.Sqrt, bias=eps_rms[:], scale=1.0 / 128), r=[ps, eps_rms], w=[rs])
                    cx.op("dve", lambda e: e.reciprocal(out=rs[:], in_=rs[:]), r=[rs], w=[rs])
                    cx.op("dve", lambda e: e.tensor_tensor(out=of[:], in0=of[:], in1=rs[:], op=ALU.mult), r=[of, rs], w=[of])
                    cx.op("dve", lambda e: e.tensor_scalar(out=O_[:, qb * 512:(qb + 1) * 512], in0=of[:], scalar1=wsc[:], scalar2=1.0 - LAM_INIT,
                                                          op0=ALU.mult, op1=ALU.mult), r=[of, wsc], w=[O_])
                    if qb == 1:
                        cx.dma("sp", odfT[h, :, :], O_[:], r=[O_])

                its = [(h, qb, kc) for h in range(NH) for qb in range(2) for kc in range(NKC)]
                pend = []

                def df_Bp(h, qb, kc, Es):
                    for m in range(2):
                        df_B(h, qb, kc, m, Es[m])

                for (h, qb, kc) in its:
                    if qb == 0 and kc == 0:
                        df_load(h)
                    pend.append((h, qb, kc, df_A(h, qb, kc)))
                    if len(pend) > 1:
                        df_Bp(*pend.pop(0))
                while pend:
                    df_Bp(*pend.pop(0))

        if "merge" in phases:
            with Phase(cx) as P:
                wpn, wpd = pre["wpn"], pre["wpd"]
                on = P.sb([128, 8, TOK], BF16, "on")
                od = P.sb([128, 8, TOK], BF16, "od")
                cx.dma("sp", on[:], onaT.rearrange("h p t -> p h t"), w=[on])
                cx.dma("sp", od[:], odfT.rearrange("h p t -> p h t"), w=[od])
                gn = RR([P.sb([128, TOK], BF16, "gn") for _ in range(2)])
                gd = RR([P.sb([128, TOK], BF16, "gd") for _ in range(2)])
                pA = RR([P.ps([128, 512], F32, "pA") for _ in range(2)])
                pD = RR([P.ps([128, 512], F32, "pD") for _ in range(2)])
                ta = RR([P.sb([128, 512], F32, "ta") for _ in range(2)])
                tb = RR([P.sb([128, 512], F32, "tb") for _ in range(2)])
                mo = RR([P.sb([128, TOK], BF16, "mo") for _ in range(2)])
                for fc in range(16):
                    Gn, Gd, Mo = gn.next(), gd.next(), mo.next()
                    cx.dma("sp", Gn[:], gnaT[fc, :, :], w=[Gn])
                    cx.dma("sp", Gd[:], gdfT[fc, :, :], w=[Gd])
                    cx.op("act", lambda e: e.activation(out=Gn[:], in_=Gn[:], func=AF.Sigmoid), r=[Gn], w=[Gn])
                    cx.op("act", lambda e: e.activation(out=Gd[:], in_=Gd[:], func=AF.Sigmoid), r=[Gd], w=[Gd])
                    for hf in range(2):
                        pa, pd = pA.next(), pD.next()
                        for h in range(8):
                            cx.op("pe", lambda e, h=h: e.matmul(pa[:, :], wpn[:, h, fc * 128:(fc + 1) * 128], on[:, h, hf * 512:(hf + 1) * 512],
                                                                start=(h == 0), stop=(h == 7)), r=[wpn, on], w=[pa], sig=(h == 7))
                        for h in range(8):
                            cx.op("pe", lambda e, h=h: e.matmul(pd[:, :], wpd[:, h, fc * 128:(fc + 1) * 128], od[:, h, hf * 512:(hf + 1) * 512],
                                                                start=(h == 0), stop=(h == 7)), r=[wpd, od], w=[pd], sig=(h == 7))
                        Ta, Tb = ta.next(), tb.next()
                        cx.op("dve", lambda e: e.tensor_tensor(out=Ta[:], in0=pa[:], in1=Gn[:, hf * 512:(hf + 1) * 512], op=ALU.mult), r=[pa, Gn], w=[Ta])
                        cx.op("dve", lambda e: e.tensor_tensor(out=Tb[:], in0=pd[:], in1=Gd[:, hf * 512:(hf + 1) * 512], op=ALU.mult), r=[pd, Gd], w=[Tb])
                        cx.op("dve", lambda e: e.tensor_tensor(out=Mo[:, hf * 512:(hf + 1) * 512], in0=Ta[:], in1=Tb[:], op=ALU.add), r=[Ta, Tb], w=[Mo])
                    cx.dma("sp", mrgT[fc, :, :], Mo[:], r=[Mo])

        PW2.__exit__(None, None, None)

        if "out" in phases:
            with Phase(cx) as P:
                wo = pre["wo"]
                gm = load_bc(P, mod_bc(0, 2), "gm")
                l1w = load_bc(P, lnp[0:1, :].partition_broadcast(128), "l1w")
                l1b = load_bc(P, lnp[1:2, :].partition_broadcast(128), "l1b")
                sc1f = load_bc(P, mod_bc(0, 4), "sc1f", True)
                shf = load_bc(P, mod_bc(0, 3), "shf")
                wr = P.sb([128, 16, 32], F32, "wr")
                cx.dma("sp", wr[:], w_r.rearrange("(kc p) e -> p kc e", p=128), w=[wr])
                brb = P.sb([128, 32], F32, "brb")
                cx.dma("sp", brb[:], b_r[0:1, :].partition_broadcast(128), w=[brb])
                mT = RR([P.sb([128, 16, 128], BF16, "mT") for _ in range(2)])
                xt = RR([P.sb([128, D], F32, "xt") for _ in range(2)])
                vt = RR([P.sb([128, D], F32, "vt") for _ in range(1)])
                ut = RR([P.sb([128, D], F32, "ut") for _ in range(1)])
                tmps = RR([ln_tmp(P) for _ in range(2)])
                py = RR([P.ps([128, 512], F32, "py") for _ in range(4)])
                ptr = RR([P.ps([128, 512], F32, "ptr") for _ in range(2)])
                plg = RR([P.ps([128, 512], F32, "plg") for _ in range(2)])
                uTf = RR([P.sb([128, 16, 128], F32, "uTf") for _ in range(2)])
                uTb = RR([P.sb([128, 16, 128], BF16, "uTb") for _ in range(2)])
                lg = RR([P.sb([128, 32], F32, "lg") for _ in range(2)])
                m8 = RR([P.sb([128, 8], F32, "m8") for _ in range(2)])
                nmx = RR([P.sb([128, 1], F32, "nmx") for _ in range(2)])
                msk = RR([P.sb([128, 32], F32, "msk") for _ in range(2)])
                ex = RR([P.sb([128, 32], F32, "ex") for _ in range(2)])
                sm = RR([P.sb([128, 1], F32, "sm") for _ in range(2)])
                gt = RR([P.sb([128, 32], F32, "gt") for _ in range(2)])
                for t in range(8):
                    M_ = mT.next()
                    cx.dma("sp", M_[:], mrgT.rearrange("k p t -> p k t")[:, :, t * 128:(t + 1) * 128], w=[M_])
                    X = xt.next()
                    cx.dma("sp", X[:], xo[t * 128:(t + 1) * 128, :], w=[X])
                    V = vt.next()
                    for cb in range(4):
                        pp = py.next()
                        for kc in range(16):
                            cx.op("pe", lambda e, kc=kc: e.matmul(pp[:, :], M_[:, kc, :], wo[:, kc, cb * 512:(cb + 1) * 512], start=(kc == 0), stop=(kc == 15)),
                                  r=[M_, wo], w=[pp], sig=(kc == 15))
                        cx.op("dve", lambda e: e.tensor_tensor(out=V[:, cb * 512:(cb + 1) * 512], in0=pp[:], in1=gm[:, cb * 512:(cb + 1) * 512], op=ALU.mult),
                              r=[pp, gm], w=[V])
                    cx.op("dve", lambda e: e.scalar_tensor_tensor(out=V[:], in0=X[:], scalar=ALPHA, in1=V[:], op0=ALU.mult, op1=ALU.add), r=[X, V], w=[V])
                    tmp = tmps.next()
                    ln_stats(P, V, tmp)
                    cx.op("act", lambda e: e.activation(out=V[:], in_=V[:], func=AF.Identity, scale=tmp[2][:], bias=tmp[3][:]), r=[V, tmp[2], tmp[3]], w=[V])
                    cx.op("dve", lambda e: e.tensor_tensor(out=V[:], in0=V[:], in1=l1w[:], op=ALU.mult), r=[V, l1w], w=[V])
                    cx.op("dve", lambda e: e.tensor_tensor(out=X[:], in0=V[:], in1=l1b[:], op=ALU.add), r=[V, l1b], w=[X])
                    cx.dma("sp", x1s[t * 128:(t + 1) * 128, :], X[:], r=[X])
                    ln_stats(P, X, tmp)
                    U = ut.next()
                    cx.op("act", lambda e: e.activation(out=U[:], in_=X[:], func=AF.Identity, scale=tmp[2][:], bias=tmp[3][:]), r=[X, tmp[2], tmp[3]], w=[U])
                    cx.op("dve", lambda e: e.tensor_tensor(out=U[:], in0=U[:], in1=sc1f[:], op=ALU.mult), r=[U, sc1f], w=[U])
                    cx.op("dve", lambda e: e.tensor_tensor(out=U[:], in0=U[:], in1=shf[:], op=ALU.add), r=[U, shf], w=[U])
                    UF, UB = uTf.next(), uTb.next()
                    for g in range(4):
                        pt = ptr.next()
                        for q in range(4):
                            kc = g * 4 + q
                            cx.op("pe", lambda e, kc=kc, q=q: e.transpose(out=pt[:, q * 128:(q + 1) * 128], in_=U[:, kc * 128:(kc + 1) * 128], identity=ident_f[:]),
                                  r=[U, ident_f], w=[pt], sig=(q == 3))
                        cx.op("act", lambda e, g=g: e.copy(out=UF[:, g * 4:(g + 1) * 4, :], in_=pt[:].rearrange("p (q n) -> p q n", q=4)), r=[pt], w=[UF])
                        cx.op("dve", lambda e, g=g: e.tensor_copy(out=UB[:, g * 4:(g + 1) * 4, :], in_=pt[:].rearrange("p (q n) -> p q n", q=4)), r=[pt], w=[UB])
                    cx.dma("sp", u2Ts.rearrange("k p t -> p k t")[:, :, t * 128:(t + 1) * 128], UB[:], r=[UB])
                    pl = plg.next()
                    for kc in range(16):
                        cx.op("pe", lambda e, kc=kc: e.matmul(pl[:, 0:32], UF[:, kc, :], wr[:, kc, :], start=(kc == 0), stop=(kc == 15)),
                              r=[UF, wr], w=[pl], sig=(kc == 15))
                    L, M8, NM, MK, EX, SM, GT = lg.next(), m8.next(), nmx.next(), msk.next(), ex.next(), sm.next(), gt.next()
                    cx.op("dve", lambda e: e.tensor_tensor(out=L[:], in0=pl[:, 0:32], in1=brb[:], op=ALU.add), r=[pl, brb], w=[L])
                    cx.op("dve", lambda e: e.max(out=M8[:], in_=L[:]), r=[L], w=[M8])
                    cx.op("dve", lambda e: e.tensor_scalar(out=NM[:], in0=M8[:, 0:1], scalar1=-1.0, scalar2=None, op0=ALU.mult), r=[M8], w=[NM])
                    cx.op("dve", lambda e: e.tensor_scalar(out=MK[:], in0=L[:], scalar1=M8[:, 3:4], scalar2=None, op0=ALU.is_ge), r=[L, M8], w=[MK])
                    cx.op("act", lambda e: e.activation(out=EX[:], in_=L[:], func=AF.Exp, bias=NM[:], scale=1.0), r=[L, NM], w=[EX])
                    cx.op("dve", lambda e: e.tensor_tensor(out=EX[:], in0=EX[:], in1=MK[:], op=ALU.mult), r=[EX, MK], w=[EX])
                    cx.op("dve", lambda e: e.reduce_sum(out=SM[:], in_=EX[:], axis=AX.X), r=[EX], w=[SM])
                    cx.op("dve", lambda e: e.reciprocal(out=SM[:], in_=SM[:]), r=[SM], w=[SM])
                    cx.op("dve", lambda e: e.tensor_scalar(out=GT[:], in0=EX[:], scalar1=SM[:], scalar2=None, op0=ALU.mult), r=[EX, SM], w=[GT])
                    cx.dma("sp", Gs[t * 128:(t + 1) * 128, :], GT[:], r=[GT])

        PW.__exit__(None, None, None)

        if "moe" in phases:
            with Phase(cx) as P:
                u2 = P.sb([128, 16, 512], BF16, "u2")
                G = P.sb([128, 4, 32], F32, "G")
                acc = P.sb([128, 4, D], F32, "acc")
                hT = [P.sb([128, 16, 512], BF16, "hT") for _ in range(1)]
                wgt = RR([P.sb([128, 16, 512], BF16, "wg") for _ in range(2)])
                wut = RR([P.sb([128, 16, 512], BF16, "wu") for _ in range(2)])
                wdt = RR([P.sb([128, 16, 512], BF16, "wd") for _ in range(2)])
                bg = RR([P.sb([128, 32], F32, "bg") for _ in range(2)])
                bd = RR([P.sb([1, D], BF16, "bd") for _ in range(2)])
                pg = RR([P.ps([128, 512], F32, "pg") for _ in range(2)])
                pu = RR([P.ps([128, 512], F32, "pu") for _ in range(2)])
                pyy = RR([P.ps([128, 512], F32, "pyy") for _ in range(2)])
                gs = RR([P.sb([128, 512], F32, "gs") for _ in range(2)])
                sgs = RR([P.sb([128, 512], F32, "sgs") for _ in range(2)])
                ls_ = RR([P.sb([128, 512], F32, "ls") for _ in range(2)])
                for hf in range(2):
                    cx.dma("sp", u2[:], u2Ts.rearrange("k p t -> p k t")[:, :, hf * 512:(hf + 1) * 512], w=[u2])
                    cx.dma("sp", G[:], Gs.rearrange("(t p) e -> p t e", p=128)[:, hf * 4:(hf + 1) * 4, :], w=[G])
                    cx.op("dve", lambda e: e.memset(acc[:], 0.0), w=[acc])
                    for ex_ in range(NE):
                        H = hT[0]
                        BG, BD = bg.next(), bd.next()
                        cx.dma("sp", BG[:], bgu[ex_, :, :], w=[BG])
                        cx.dma("pool", BD[:], b_d[ex_:ex_ + 1, :], w=[BD])
                        wg_v = w_g[ex_].rearrange("(kc p) f -> p kc f", p=128)
                        wu_v = w_u[ex_].rearrange("(kc p) f -> p kc f", p=128)
                        wd_v = w_d[ex_].rearrange("(kc p) f -> p kc f", p=128)
                        for fb in range(4):
                            WG, WU = wgt.next(), wut.next()
                            cx.dma("pool", WG[:], wg_v[:, :, fb * 512:(fb + 1) * 512], w=[WG])
                            cx.dma("pool", WU[:], wu_v[:, :, fb * 512:(fb + 1) * 512], w=[WU])
                            for sub in range(4):
                                fc = fb * 4 + sub
                                p1, p2 = pg.next(), pu.next()
                                for kc in range(16):
                                    cx.op("pe", lambda e, kc=kc: e.matmul(p1[:, :], WG[:, kc, sub * 128:(sub + 1) * 128], u2[:, kc, :], start=(kc == 0), stop=(kc == 15)),
                                          r=[WG, u2], w=[p1], sig=(kc == 15))
                                for kc in range(16):
                                    cx.op("pe", lambda e, kc=kc: e.matmul(p2[:, :], WU[:, kc, sub * 128:(sub + 1) * 128], u2[:, kc, :], start=(kc == 0), stop=(kc == 15)),
                                          r=[WU, u2], w=[p2], sig=(kc == 15))
                                GS, SG, LS = gs.next(), sgs.next(), ls_.next()
                                cx.op("dve", lambda e: e.tensor_scalar(out=GS[:], in0=p1[:], scalar1=BG[:, fc:fc + 1], scalar2=7.0, op0=ALU.add, op1=ALU.min),
                                      r=[p1, BG], w=[GS])
                                cx.op("act", lambda e: e.activation(out=SG[:], in_=GS[:], func=AF.Sigmoid, scale=1.702), r=[GS], w=[SG])
                                cx.op("dve", lambda e: e.tensor_scalar(out=LS[:], in0=p2[:], scalar1=BG[:, 16 + fc:17 + fc], scalar2=7.0, op0=ALU.add, op1=ALU.min),
                                      r=[p2, BG], w=[LS])
                                cx.op("dve", lambda e: e.tensor_scalar(out=LS[:], in0=LS[:], scalar1=-7.0, scalar2=1.0, op0=ALU.max, op1=ALU.add), r=[LS], w=[LS])
                                cx.op("dve", lambda e: e.tensor_tensor(out=GS[:], in0=GS[:], in1=SG[:], op=ALU.mult), r=[GS, SG], w=[GS])
                                cx.op("dve", lambda e: e.tensor_tensor(out=H[:, fc, :], in0=GS[:], in1=LS[:], op=ALU.mult), r=[GS, LS], w=[H])
                        for db in range(4):
                            WD = wdt.next()
                            cx.dma("pool", WD[:], wd_v[:, :, db * 512:(db + 1) * 512], w=[WD])
                            for t in range(4):
                                pp = pyy.next()
                                for fc in range(16):
                                    cx.op("pe", lambda e, fc=fc: e.matmul(pp[:, :], H[:, fc, t * 128:(t + 1) * 128], WD[:, fc, :], start=(fc == 0), stop=False),
                                          r=[H, WD], w=[pp], sig=False)
                                cx.op("pe", lambda e: e.matmul(pp[:, :], ones_b[0:1, :], BD[0:1, db * 512:(db + 1) * 512], start=False, stop=True),
                                      r=[ones_b, BD], w=[pp])
                                cx.op("dve", lambda e: e.scalar_tensor_tensor(out=acc[:, t, db * 512:(db + 1) * 512], in0=pp[:], scalar=G[:, t, ex_:ex_ + 1],
                                                                             in1=acc[:, t, db * 512:(db + 1) * 512], op0=ALU.mult, op1=ALU.add),
                                      r=[pp, G, acc], w=[acc])
                    for t in range(4):
                        row0 = hf * 512 + t * 128
                        cx.dma("sp", y2s[row0:row0 + 128, :], acc[:, t, :], r=[acc])

        if "fin" in phases:
            with Phase(cx) as P:
                gf = load_bc(P, mod_bc(0, 5), "gf")
                l2w = load_bc(P, lnp[2:3, :].partition_broadcast(128), "l2w")
                l2b = load_bc(P, lnp[3:4, :].partition_broadcast(128), "l2b")
                xt = RR([P.sb([128, D], F32, "xt") for _ in range(2)])
                yt = RR([P.sb([128, D], F32, "yt") for _ in range(2)])
                tmps = RR([ln_tmp(P) for _ in range(2)])
                for t in range(8):
                    X, Y = xt.next(), yt.next()
                    row0 = t * 128
                    cx.dma("sp", X[:], x1s[row0:row0 + 128, :], w=[X])
                    cx.dma("sp", Y[:], y2s[row0:row0 + 128, :], w=[Y])
                    cx.op("dve", lambda e: e.tensor_tensor(out=Y[:], in0=Y[:], in1=gf[:], op=ALU.mult), r=[Y, gf], w=[Y])
                    cx.op("dve", lambda e: e.scalar_tensor_tensor(out=X[:], in0=X[:], scalar=ALPHA, in1=Y[:], op0=ALU.mult, op1=ALU.add),
                          r=[X, Y], w=[X])
                    tmp = tmps.next()
                    ln_stats(P, X, tmp)
                    cx.op("act", lambda e: e.activation(out=X[:], in_=X[:], func=AF.Identity, scale=tmp[2][:], bias=tmp[3][:]), r=[X, tmp[2], tmp[3]], w=[X])
                    cx.op("dve", lambda e: e.tensor_tensor(out=X[:], in0=X[:], in1=l2w[:], op=ALU.mult), r=[X, l2w], w=[X])
                    cx.op("dve", lambda e: e.tensor_tensor(out=X[:], in0=X[:], in1=l2b[:], op=ALU.add), r=[X, l2b], w=[X])
                    cx.dma("sp", out[row0:row0 + 128, :], X[:], r=[X])

        cx.barrier()
        PP.__exit__(None, None, None)
        build.ninstr = cx.ninstr
    return nc


def _consts():
    f32 = np.float32
    ident = np.eye(128, dtype=f32)
    perm = np.zeros((128, 128), f32)
    f = np.arange(128)
    j = f % 32
    partner = f - j + (j + 16) % 32
    perm[partner, f] = 1.0
    inv_freq = (10000.0 ** (-np.arange(16, dtype=np.float32) / 16)).astype(f32)
    tok = np.arange(S)
    rowp = (tok // 64).astype(f32)
    colp = (tok % 64).astype(f32)
    i64 = f % 64
    half = i64 // 32
    pos = np.where(half[:, None] == 0, rowp[None, :], colp[None, :]).astype(f32)
    ang = (pos * inv_freq[j % 16][:, None]).astype(f32)
    C = np.cos(ang).astype(f32)
    Sn = np.sin(ang).astype(f32)
    Sn = np.where((j < 16)[:, None], -Sn, Sn).astype(f32)
    return ident, perm, np.stack([C, Sn]).astype(f32)


def _nab(rel_bias):
    p = np.arange(128)
    dr = p // 64
    ck = p % 64
    cq = np.arange(64)
    cstart = np.clip(cq - 8, 0, 48)
    colok = (ck[:, None] >= cstart[None, :]) & (ck[:, None] < cstart[None, :] + 16)
    coff = np.clip(ck[:, None] - cq[None, :] + 15, 0, 30)
    out = np.full((NH, 128, 16, 64), NEG, np.float32)
    for idx in range(16):
        d = idx - 8 + dr
        rowok = np.abs(d) <= 7
        ok = colok & rowok[:, None]
        vals = rel_bias[:, np.clip(d + 7, 0, 14)[:, None], coff]
        out[:, :, idx, :] = np.where(ok[None], vals, NEG)
    return out


def _navalid(j):
    v = np.zeros((128, 16, 6), np.float32)
    p = np.arange(128)
    for i in range(16):
        lo, hi = min(i, 12), max(i + 8, 12)
        ms = list(range(lo // 2, (hi + 1) // 2))
        r = 16 * j + i
        r0 = min(max(r - 4, 0), 56)
        for s_, m in enumerate(ms):
            g = 16 * j - 4 + 2 * m + p // 64
            v[:, i, s_] = ((g >= r0) & (g < r0 + 8)).astype(np.float32)
    return v


def prep_inputs(inp, NE=NE_FULL):
    ident, perm, rope = _consts()
    f32 = np.float32
    A = lambda a: np.ascontiguousarray(a, dtype=f32)
    nab = _nab(np.asarray(inp["na_rel_bias"][0], f32))
    lamv = A(np.concatenate([inp["lam_q1"], inp["lam_k1"], inp["lam_q2"], inp["lam_k2"]], axis=0)).reshape(1, 256)
    lnp = A(np.concatenate([inp["ln1_w"], inp["ln1_b"], inp["ln2_w"], inp["ln2_b"]], axis=0))
    bgu = A(np.concatenate([inp["b_gate"][0].reshape(32, 16, 128).transpose(0, 2, 1),
                            inp["b_up"][0].reshape(32, 16, 128).transpose(0, 2, 1)], axis=2))[:NE]
    shared = dict(
        w_ada=A(inp["w_ada"][0]), b_ada=A(inp["b_ada"]), w_in=A(inp["w_in"][0]), nab=A(nab), lamv=lamv,
        wsub=A(inp["diff_subln_w"][0].reshape(128, 1)), w_proj_na=A(inp["w_proj_na"][0]), w_proj_diff=A(inp["w_proj_diff"][0]),
        w_out=A(inp["w_out"][0]), lnp=lnp, w_router=A(inp["w_router"][0]), b_router=A(inp["b_router"]),
        w_gate=A(inp["w_gate"][0][:NE]), w_up=A(inp["w_up"][0][:NE]), w_down=A(inp["w_down"][0][:NE]), bgu=bgu,
        b_down=A(inp["b_down"][0][:NE]), ropeK=A(rope), ident=ident, perm=perm,
    )
    maps = []
    for i in range(NCORE):
        b, j = i // 4, i % 4
        x = np.asarray(inp["x"][b], f32)
        xh = np.zeros((512, D), f32)
        top0 = 1024 * j - 256
        if top0 >= 0:
            xh[0:256] = x[top0:top0 + 256]
        bot0 = 1024 * j + 1024
        if bot0 + 256 <= S:
            xh[256:512] = x[bot0:bot0 + 256]
        cc = np.stack([np.asarray(inp["c"][b], f32).reshape(16, 128).T, np.asarray(inp["c_ctx"], f32).reshape(16, 128).T], axis=2)
        m = dict(shared)
        m.update(xb=A(x), xo=A(x[1024 * j:1024 * j + 1024]), xh=xh, ctx=A(inp["ctx"][b]), cc=A(cc),
                 navalid=_navalid(j), ropeQ=A(rope[:, :, 1024 * j:1024 * j + 1024]))
        maps.append(m)
    return maps


def kernel(**inputs):
    nc = build()
    maps = prep_inputs(inputs)
    res = run_bass_kernel_spmd(nc, maps, core_ids=list(range(NCORE)))
    outp = np.empty((2, S, D), np.float32)
    for i in range(NCORE):
        b, j = i // 4, i % 4
        outp[b, 1024 * j:1024 * j + 1024] = res.results[i]["out"]
    return outp
```
